# Optimizing a Trainium2 kernel written in Bass

```python
import math
import jax, jax.numpy as jnp
from jax import lax
import numpy as np

D_MODEL = 1024
BATCH = 8
SEQ = 2048
DEPTH = 4

N_MIXERS = 2
RMS_EPS = 1e-6
GN_EPS = 1e-6
NEG_INF = -1e30

NSA_HEADS = 16
NSA_HEAD_DIM = 64
NSA_KV_GROUPS = 4
NSA_Q_PER_GROUP = NSA_HEADS // NSA_KV_GROUPS
NSA_WIDTH = NSA_HEADS * NSA_HEAD_DIM
NSA_KV_WIDTH = NSA_KV_GROUPS * NSA_HEAD_DIM
CMP_BLOCK = 32
CMP_STRIDE = 16
CMP_HIDDEN = 256
SLC_BLOCK = 64
SLC_TOPN = 16
SLC_Q_BLOCK = 32
WIN_SIZE = 512
WIN_Q_BLOCK = 128
FORCED_SCORE = 1e3
NSA_PROJ = NSA_WIDTH + 6 * NSA_KV_WIDTH + 3 * NSA_HEADS + 3 * NSA_WIDTH

REL_BUCKETS = 32
REL_MAX_DIST = 128

RET_HEADS = 4
RET_QK_DIM = 256
RET_V_DIM = 512
RET_QK_WIDTH = RET_HEADS * RET_QK_DIM
RET_WIDTH = RET_HEADS * RET_V_DIM
RET_CHUNK = 128
ROPE_BASE = 10000.0
RET_PROJ = 2 * RET_QK_WIDTH + 2 * RET_WIDTH

kernel_name = "hybrid_nsa_retention_sandwich"


def rms_norm(x, gain):
    xf = x.astype(jnp.float32)
    y = xf * lax.rsqrt(jnp.mean(xf * xf, axis=-1, keepdims=True) + RMS_EPS)
    return (y * gain.astype(jnp.float32)).astype(x.dtype)


def split_cols(a, sizes):
    return jnp.split(a, list(np.cumsum(sizes)[:-1]), axis=-1)


def masked_softmax(logits, mask):
    p = jax.nn.softmax(jnp.where(mask, logits, NEG_INF), axis=-1)
    return jnp.where(mask, p, 0.0)


def t5_bucket(dist):
    n = jnp.maximum(dist, 0)
    max_exact = REL_BUCKETS // 2
    nf = jnp.maximum(n, 1).astype(jnp.float32)
    large = max_exact + (jnp.log(nf / max_exact) / math.log(REL_MAX_DIST / max_exact)
                         * (REL_BUCKETS - max_exact)).astype(jnp.int32)
    large = jnp.minimum(large, REL_BUCKETS - 1)
    return jnp.where(n < max_exact, n, large)


def compress_kv(kv, pos, w1, w2):
    B, S, G, dh = kv.shape
    n_cmp = (S - CMP_BLOCK) // CMP_STRIDE + 1
    idx = jnp.arange(n_cmp)[:, None] * CMP_STRIDE + jnp.arange(CMP_BLOCK)[None, :]
    blocks = kv[:, idx] + pos[None, None, :, None, :].astype(kv.dtype)
    flat = blocks.transpose(0, 1, 3, 2, 4).reshape(B, n_cmp, G, CMP_BLOCK * dh)
    return jax.nn.silu(flat @ w1) @ w2


def nsa_compressed(q, kc, vc, table):
    B, S, H, dh = q.shape
    n_cmp = kc.shape[1]
    qg = q.reshape(B, S, NSA_KV_GROUPS, NSA_Q_PER_GROUP, dh)
    logits = jnp.einsum('bsgrd,bcgd->bgrsc', qg, kc).astype(jnp.float32) * (dh ** -0.5)
    t = jnp.arange(S)
    end = jnp.arange(n_cmp) * CMP_STRIDE + CMP_BLOCK - 1
    dist = t[:, None] - end[None, :]
    bias = table[t5_bucket(dist)].astype(jnp.float32)
    bias = bias.reshape(S, n_cmp, NSA_KV_GROUPS, NSA_Q_PER_GROUP).transpose(2, 3, 0, 1)
    p = masked_softmax(logits + bias, dist >= 0)
    o = jnp.einsum('bgrsc,bcgd->bsgrd', p.astype(vc.dtype), vc).reshape(B, S, H, dh)
    return o, p


def select_blocks(p_cmp, S):
    n_cmp = p_cmp.shape[-1]
    n_sel = S // SLC_BLOCK
    cs = jnp.arange(n_cmp) * CMP_STRIDE
    j = jnp.arange(n_sel)
    overlap = ((cs[:, None] < (j[None, :] + 1) * SLC_BLOCK)
               & (cs[:, None] + CMP_BLOCK > j[None, :] * SLC_BLOCK)).astype(jnp.float32)
    imp = jnp.einsum('bgrsc,cj->bgsj', p_cmp, overlap)
    t = jnp.arange(S)
    cur = t // SLC_BLOCK
    valid = j[None, :] * SLC_BLOCK <= t[:, None]
    forced = (j[None, :] == 0) | (j[None, :] == cur[:, None]) | (j[None, :] == cur[:, None] - 1)
    score = jnp.where(valid, imp + jnp.where(forced, FORCED_SCORE, 0.0), NEG_INF)
    _, sel = lax.top_k(score, min(SLC_TOPN, n_sel))
    return sel


def nsa_selected(q, k, v, sel, table):
    B, S, H, dh = q.shape
    G, R = NSA_KV_GROUPS, NSA_Q_PER_GROUP
    n_sel = S // SLC_BLOCK
    n_top = sel.shape[-1]
    n_keys = n_top * SLC_BLOCK
    kb = k.reshape(B, n_sel, SLC_BLOCK, G, dh).transpose(0, 3, 1, 2, 4)
    vb = v.reshape(B, n_sel, SLC_BLOCK, G, dh).transpose(0, 3, 1, 2, 4)
    nq = S // SLC_Q_BLOCK
    q_blk = q.reshape(B, nq, SLC_Q_BLOCK, G, R, dh).transpose(1, 0, 2, 3, 4, 5)
    sel_blk = sel.reshape(B, G, nq, SLC_Q_BLOCK, n_top).transpose(2, 0, 1, 3, 4)
    bi = jnp.arange(B)[:, None, None, None]
    gi = jnp.arange(G)[None, :, None, None]
    tbl = table.reshape(REL_BUCKETS, G, R)

    def one(args):
        qb, sb, blk = args
        t = blk * SLC_Q_BLOCK + jnp.arange(SLC_Q_BLOCK)
        kg = kb[bi, gi, sb].reshape(B, G, SLC_Q_BLOCK, n_keys, dh)
        vg = vb[bi, gi, sb].reshape(B, G, SLC_Q_BLOCK, n_keys, dh)
        pos = (sb[..., None] * SLC_BLOCK + jnp.arange(SLC_BLOCK)).reshape(B, G, SLC_Q_BLOCK, n_keys)
        dist = t[None, None, :, None] - pos
        bias = tbl[t5_bucket(dist), gi].astype(jnp.float32).transpose(0, 1, 4, 2, 3)
        logits = jnp.einsum('bqgrd,bgqkd->bgrqk', qb, kg).astype(jnp.float32) * (dh ** -0.5)
        p = masked_softmax(logits + bias, (dist >= 0)[:, :, None])
        return jnp.einsum('bgrqk,bgqkd->bqgrd', p.astype(vg.dtype), vg)

    out = lax.map(one, (q_blk, sel_blk, jnp.arange(nq)))
    return out.transpose(1, 0, 2, 3, 4, 5).reshape(B, S, H, dh)


def nsa_window(q, k, v, table):
    B, S, H, dh = q.shape
    G, R = NSA_KV_GROUPS, NSA_Q_PER_GROUP
    nq = S // WIN_Q_BLOCK
    span = WIN_SIZE + WIN_Q_BLOCK
    kp = jnp.pad(k, ((0, 0), (WIN_SIZE, 0), (0, 0), (0, 0)))
    vp = jnp.pad(v, ((0, 0), (WIN_SIZE, 0), (0, 0), (0, 0)))
    q_blk = q.reshape(B, nq, WIN_Q_BLOCK, G, R, dh).transpose(1, 0, 2, 3, 4, 5)

    def one(args):
        qb, blk = args
        start = blk * WIN_Q_BLOCK
        kw = lax.dynamic_slice_in_dim(kp, start, span, axis=1)
        vw = lax.dynamic_slice_in_dim(vp, start, span, axis=1)
        t = start + jnp.arange(WIN_Q_BLOCK)
        pos = start - WIN_SIZE + jnp.arange(span)
        dist = t[:, None] - pos[None, :]
        mask = (dist >= 0) & (dist < WIN_SIZE) & (pos[None, :] >= 0)
        bias = table[t5_bucket(dist)].astype(jnp.float32)
        bias = bias.reshape(WIN_Q_BLOCK, span, G, R).transpose(2, 3, 0, 1)
        logits = jnp.einsum('bqgrd,bkgd->bgrqk', qb, kw).astype(jnp.float32) * (dh ** -0.5)
        p = masked_softmax(logits + bias, mask)
        return jnp.einsum('bgrqk,bkgd->bqgrd', p.astype(vw.dtype), vw)

    out = lax.map(one, (q_blk, jnp.arange(nq)))
    return out.transpose(1, 0, 2, 3, 4, 5).reshape(B, S, H, dh)


def nsa_mixer(h, w_in, w_out, k_pos, k_w1, k_w2, v_pos, v_w1, v_w2, table):
    B, S, _ = h.shape
    H, dh, G = NSA_HEADS, NSA_HEAD_DIM, NSA_KV_GROUPS
    q, kv, g, z = split_cols(h @ w_in, [NSA_WIDTH, 6 * NSA_KV_WIDTH, 3 * H, 3 * NSA_WIDTH])
    q = q.reshape(B, S, H, dh)
    k_c, v_c, k_s, v_s, k_w, v_w = [a.reshape(B, S, G, dh) for a in jnp.split(kv, 6, axis=-1)]
    gates = jax.nn.sigmoid(g).reshape(B, S, 3, H, 1)
    zs = jax.nn.silu(z).reshape(B, S, 3, H, dh)
    kc = compress_kv(k_c, k_pos, k_w1, k_w2)
    vc = compress_kv(v_c, v_pos, v_w1, v_w2)
    o_cmp, p_cmp = nsa_compressed(q, kc, vc, table)
    sel = select_blocks(p_cmp, S)
    o_slc = nsa_selected(q, k_s, v_s, sel, table)
    o_win = nsa_window(q, k_w, v_w, table)
    o = jnp.stack([o_cmp, o_slc, o_win], axis=2)
    y = jnp.sum(gates * o * zs, axis=2).reshape(B, S, NSA_WIDTH)
    return y @ w_out


def rotary(x):
    S, d = x.shape[1], x.shape[-1]
    inv = 1.0 / (ROPE_BASE ** jnp.linspace(0.0, 1.0, d // 2, dtype=jnp.float32))
    ang = jnp.arange(S, dtype=jnp.float32)[:, None] * inv[None, :]
    cos = jnp.cos(ang)[None, :, None, :].astype(x.dtype)
    sin = jnp.sin(ang)[None, :, None, :].astype(x.dtype)
    x1, x2 = x[..., :d // 2], x[..., d // 2:]
    return jnp.concatenate([x1 * cos - x2 * sin, x1 * sin + x2 * cos], axis=-1)


def chunkwise_retention(q, k, v):
    B, S, H, dk = q.shape
    dv = v.shape[-1]
    C = RET_CHUNK
    nc = S // C
    dt = q.dtype
    log_g = jnp.log(1.0 - 2.0 ** (-5.0 - jnp.arange(H, dtype=jnp.float32)))
    i = jnp.arange(C, dtype=jnp.float32)
    diff = i[:, None] - i[None, :]
    inner = jnp.where(diff >= 0, jnp.exp(diff[None] * log_g[:, None, None]), 0.0).astype(dt)
    xi = jnp.exp((i + 1.0)[None, :] * log_g[:, None]).astype(dt)
    zeta = jnp.exp((C - 1.0 - i)[None, :] * log_g[:, None]).astype(dt)
    chunk_decay = jnp.exp(C * log_g).astype(dt)
    qc = q.reshape(B, nc, C, H, dk).transpose(1, 0, 3, 2, 4)
    kc = k.reshape(B, nc, C, H, dk).transpose(1, 0, 3, 2, 4)
    vc = v.reshape(B, nc, C, H, dv).transpose(1, 0, 3, 2, 4)

    def step(state, inp):
        qi, ki, vi = inp
        attn = jnp.einsum('bhid,bhjd->bhij', qi, ki) * inner[None]
        o = (jnp.einsum('bhij,bhjv->bhiv', attn, vi)
             + jnp.einsum('bhid,bhdv->bhiv', qi, state) * xi[None, :, :, None])
        state = (state * chunk_decay[None, :, None, None]
                 + jnp.einsum('bhjd,bhjv->bhdv', ki * zeta[None, :, :, None], vi))
        return state, o

    state0 = jnp.zeros((B, H, dk, dv), dt)
    _, o = lax.scan(step, state0, (qc, kc, vc))
    return o.transpose(1, 0, 3, 2, 4).reshape(B, S, H, dv)


def retention_mixer(h, w_in, w_out, gn_gain):
    B, S, _ = h.shape
    q, k, v, z = split_cols(h @ w_in, [RET_QK_WIDTH, RET_QK_WIDTH, RET_WIDTH, RET_WIDTH])
    q = rotary(q.reshape(B, S, RET_HEADS, RET_QK_DIM))
    k = rotary(k.reshape(B, S, RET_HEADS, RET_QK_DIM)) * (RET_QK_DIM ** -0.5)
    v = v.reshape(B, S, RET_HEADS, RET_V_DIM)
    o = chunkwise_retention(q, k, v).astype(jnp.float32)
    mu = jnp.mean(o, axis=-1, keepdims=True)
    var = jnp.mean(jnp.square(o - mu), axis=-1, keepdims=True)
    o = (o - mu) * lax.rsqrt(var + GN_EPS) * gn_gain.reshape(RET_HEADS, RET_V_DIM).astype(jnp.float32)
    o = o.astype(h.dtype).reshape(B, S, RET_WIDTH)
    return (o * jax.nn.silu(z)) @ w_out


def setup_inputs(seed: int = 0) -> dict:
    key = jax.random.key(seed)
    ks = jax.random.split(key, 16)
    n_nsa = (DEPTH + N_MIXERS - 1) // N_MIXERS
    n_ret = DEPTH // N_MIXERS

    def nrm(k, shape, scale):
        return jax.random.normal(k, shape, jnp.float32) * scale

    return {
        "x": nrm(ks[0], (BATCH, SEQ, D_MODEL), 1.0),
        "pre_norm_gain": 1.0 + nrm(ks[1], (DEPTH, D_MODEL), 0.1),
        "post_norm_gain": 1.0 + nrm(ks[2], (DEPTH, D_MODEL), 0.1),
        "rel_bias_table": nrm(ks[3], (REL_BUCKETS, NSA_HEADS), 0.5),
        "nsa_w_in": nrm(ks[4], (n_nsa, D_MODEL, NSA_PROJ), D_MODEL ** -0.5),
        "nsa_w_out": nrm(ks[5], (n_nsa, NSA_WIDTH, D_MODEL), NSA_WIDTH ** -0.5),
        "nsa_cmp_k_pos": nrm(ks[6], (n_nsa, CMP_BLOCK, NSA_HEAD_DIM), 0.5),
        "nsa_cmp_k_w1": nrm(ks[7], (n_nsa, CMP_BLOCK * NSA_HEAD_DIM, CMP_HIDDEN), (CMP_BLOCK * NSA_HEAD_DIM) ** -0.5),
        "nsa_cmp_k_w2": nrm(ks[8], (n_nsa, CMP_HIDDEN, NSA_HEAD_DIM), CMP_HIDDEN ** -0.5),
        "nsa_cmp_v_pos": nrm(ks[9], (n_nsa, CMP_BLOCK, NSA_HEAD_DIM), 0.5),
        "nsa_cmp_v_w1": nrm(ks[10], (n_nsa, CMP_BLOCK * NSA_HEAD_DIM, CMP_HIDDEN), (CMP_BLOCK * NSA_HEAD_DIM) ** -0.5),
        "nsa_cmp_v_w2": nrm(ks[11], (n_nsa, CMP_HIDDEN, NSA_HEAD_DIM), CMP_HIDDEN ** -0.5),
        "ret_w_in": nrm(ks[12], (n_ret, D_MODEL, RET_PROJ), D_MODEL ** -0.5),
        "ret_w_out": nrm(ks[13], (n_ret, RET_WIDTH, D_MODEL), RET_WIDTH ** -0.5),
        "ret_gn_gain": 1.0 + nrm(ks[14], (n_ret, RET_WIDTH), 0.1),
    }


def reference(x, pre_norm_gain, post_norm_gain, rel_bias_table, nsa_w_in, nsa_w_out,
              nsa_cmp_k_pos, nsa_cmp_k_w1, nsa_cmp_k_w2, nsa_cmp_v_pos, nsa_cmp_v_w1, nsa_cmp_v_w2,
              ret_w_in, ret_w_out, ret_gn_gain):
    h = x
    for layer in range(DEPTH):
        slot = layer // N_MIXERS
        u = rms_norm(h, pre_norm_gain[layer])
        if layer % N_MIXERS == 0:
            y = nsa_mixer(u, nsa_w_in[slot], nsa_w_out[slot],
                          nsa_cmp_k_pos[slot], nsa_cmp_k_w1[slot], nsa_cmp_k_w2[slot],
                          nsa_cmp_v_pos[slot], nsa_cmp_v_w1[slot], nsa_cmp_v_w2[slot],
                          rel_bias_table)
        else:
            y = retention_mixer(u, ret_w_in[slot], ret_w_out[slot], ret_gn_gain[slot])
        h = h + rms_norm(y, post_norm_gain[layer])
    return h
```

```python
from contextlib import ExitStack
import math
import numpy as np
import concourse.bass as bass
import concourse.mybir as mybir
from concourse.bass_utils import run_bass_kernel_spmd

F32 = mybir.dt.float32
BF16 = mybir.dt.bfloat16
AF = mybir.ActivationFunctionType
ALU = mybir.AluOpType

D_MODEL = 1024
SEQ = 2048
NT = SEQ // 128
DEPTH = 4
RMS_EPS = 1e-6
GN_EPS = 1e-6
NEG = -30000.0

NSA_PROJ = 5680
CMP_N = 127
RET_PROJ = 6144


class Res:
    __slots__ = ("w", "r", "name")
    base = {}

    def __init__(self, name=""):
        self.w = {}
        self.r = dict(Res.base)
        self.name = name


def _merge(d, src):
    for k, (sem, v) in src.items():
        cur = d.get(k)
        if cur is None or cur[1] < v:
            d[k] = (sem, v)


class Sched:
    def __init__(self, nc, es, n_dma=40):
        self.nc = nc
        self.eng = {"pe": nc.tensor, "act": nc.scalar, "dve": nc.vector,
                    "pool": nc.gpsimd, "sp": nc.sync}
        self.esem = {}
        self.cnt = {}
        self.seen = {}
        for k in self.eng:
            self.esem[k] = es.enter_context(nc.semaphore("e_" + k))
            self.cnt[k] = 0
            self.seen[k] = {}
        self.dsem = [es.enter_context(nc.semaphore("d%d" % i)) for i in range(n_dma)]
        self.dtot = [0] * n_dma
        self.dnext = 0
        self.dnext_sw = 0
        self.sw_hist = []
        self.n_sw = 12
        self.n_wait = 0
        self.n_inst = 0
        self.clear_all()

    def clear_all(self):
        for sem in list(self.esem.values()) + self.dsem:
            self.nc.gpsimd.sem_clear(sem)
        self.nc.all_engine_barrier()

    def _wait(self, E, deps, skip_own=True):
        own = id(self.esem[E]) if skip_own else None
        seen = self.seen[E]
        for k, (sem, v) in deps.items():
            if k == own:
                continue
            if seen.get(k, 0) >= v:
                continue
            self.eng[E].wait_ge(sem, v)
            seen[k] = v
            self.n_wait += 1

    def _deps(self, r, w, pw):
        deps = {}
        for x in r:
            _merge(deps, x.w)
        for x in w:
            _merge(deps, x.w)
            _merge(deps, x.r)
        for x in pw:
            _merge(deps, x.r)
        return deps

    def _commit(self, tok, r, w, pw):
        k = id(tok[0])
        for x in r:
            cur = x.r.get(k)
            if cur is None or cur[1] < tok[1]:
                x.r[k] = tok
        for x in w:
            x.w = {k: tok}
        for x in pw:
            x.w[k] = tok

    def op(self, E, fn, r=(), w=(), pw=()):
        self._wait(E, self._deps(r, w, pw), skip_own=(E == "pe"))
        inst = fn()
        self.cnt[E] += 1
        inst.then_inc(self.esem[E], 1)
        self._commit((self.esem[E], self.cnt[E]), r, w, pw)
        self.n_inst += 1
        return inst

    def dma(self, q, out, in_, r=(), w=(), pw=(), **kw):
        self._wait(q, self._deps(r, w, pw), skip_own=False)
        nsw = self.n_sw
        if q == "pool":
            i = self.dnext_sw
            self.dnext_sw = (self.dnext_sw + 1) % nsw
        else:
            i = nsw + self.dnext
            self.dnext = (self.dnext + 1) % (len(self.dsem) - nsw)
        sem = self.dsem[i]
        if self.dtot[i] > 0:
            self._wait(q, {id(sem): (sem, self.dtot[i])})
        if q == "pool":
            if len(self.sw_hist) >= 3:
                psem, pval = self.sw_hist[-3]
                self._wait(q, {id(psem): (psem, pval)})
        inst = self.eng[q].dma_start(out=out, in_=in_, **kw)
        self.dtot[i] += 16
        inst.then_inc(sem, 16)
        self._commit((sem, self.dtot[i]), r, w, pw)
        if q == "pool":
            self.sw_hist.append((sem, self.dtot[i]))
        self.n_inst += 1
        return inst

    def fence(self):
        base = {}
        for k, sem in self.esem.items():
            if self.cnt[k] > 0:
                base[id(sem)] = (sem, self.cnt[k])
        for sem, tot in zip(self.dsem, self.dtot):
            if tot > 0:
                base[id(sem)] = (sem, tot)
        Res.base = base

    def finish(self, resources):
        deps = {}
        for x in resources:
            _merge(deps, x.w)
        self._wait("sp", deps)


def _ret_consts():
    H, C = 4, 128
    log_g = np.log(1.0 - 2.0 ** (-5.0 - np.arange(H, dtype=np.float64)))
    i = np.arange(C, dtype=np.float64)
    innerT = np.exp(-(i[:, None, None] + 1.0) * log_g[None, :, None]) * \
        (i[None, None, :] >= i[:, None, None])
    zeta = np.exp((C - 1.0 - i)[:, None] * log_g[None, :])
    xi = np.exp((i + 1.0)[:, None] * log_g[None, :])
    epsp = GN_EPS / (xi * xi)
    gC = np.exp(C * log_g)
    inv = (1.0 / (np.float32(10000.0) ** np.linspace(0.0, 1.0, 128, dtype=np.float32))).astype(np.float32)
    ang = (np.arange(SEQ, dtype=np.float32)[:, None] * inv[None, :]).astype(np.float32)
    cosT = np.ascontiguousarray(np.cos(ang).T.astype(np.float32))
    sinT = np.ascontiguousarray(np.sin(ang).T.astype(np.float32))
    return dict(innerT=innerT.astype(np.float32).reshape(128, 4 * 128),
                zeta=zeta.astype(np.float32), epsp=epsp.astype(np.float32),
                gC=[float(x) for x in gC], cosT=cosT, sinT=sinT)


DEBUG = False
STAGE = 99
TOKSTOP = -1
NOEVAC = ()


class StopEmit(Exception):
    pass


def stage(n):
    return STAGE <= n


class Ctx:
    def dump(self, name, ap, res, dt):
        if not DEBUG:
            return
        d = self.nc.dram_tensor("dbg_" + name, list(ap.shape), dt, kind="ExternalOutput").ap()
        self.dumps.append("dbg_" + name)
        r = Res()
        if len(ap.shape) == 3:
            for j in range(ap.shape[1]):
                self.sc.dma("sp", out=d[:, j, :], in_=ap[:, j, :], r=list(res), pw=[r])
        else:
            self.sc.dma("sp", out=d, in_=ap, r=list(res), w=[r])
        self.dump_res.append(r)


def bcast_row(dram_ap_row, n):
    return bass.AP(dram_ap_row.tensor, dram_ap_row.offset, [[0, 128], [1, n]])


def emit_prenorm(cx, es, li, h_in, hres, pre_gain_row, uT, uTres):
    nc, sc = cx.nc, cx.sc
    gain = es.enter_context(nc.sbuf_tensor("pg%d" % li, [128, D_MODEL], F32))
    gres = Res()
    sc.dma("sp", out=gain[:], in_=bcast_row(pre_gain_row, D_MODEL), w=[gres])
    hb = [es.enter_context(nc.sbuf_tensor("hb%d_%d" % (li, k), [128, D_MODEL], F32)) for k in range(2)]
    hbr = [Res(), Res()]
    ub = [es.enter_context(nc.sbuf_tensor("ub%d_%d" % (li, k), [128, D_MODEL], BF16)) for k in range(2)]
    ubr = [Res(), Res()]
    junk = es.enter_context(nc.sbuf_tensor("junk%d" % li, [128, D_MODEL], BF16))
    junkr = Res()
    ss = es.enter_context(nc.sbuf_tensor("ss%d" % li, [128, NT], F32))
    rs = es.enter_context(nc.sbuf_tensor("rs%d" % li, [128, NT], F32))
    ssr = [Res() for _ in range(NT)]
    for i in range(NT):
        k = i % 2
        sc.dma("sp", out=hb[k][:], in_=h_in[i * 128:(i + 1) * 128, :], r=[hres[i]], w=[hbr[k]])
        sc.op("act", lambda: nc.scalar.activation(out=junk[:], in_=hb[k][:], func=AF.Square,
                                                  accum_out=ss[:, i:i + 1]),
              r=[hbr[k]], w=[junkr, ssr[i]])
        sc.op("act", lambda: nc.scalar.activation(out=rs[:, i:i + 1], in_=ss[:, i:i + 1], func=AF.Ln,
                                                  scale=1.0 / D_MODEL, bias=cx.eps_rms[:, 0:1]),
              r=[ssr[i], cx.cres], w=[ssr[i]])
        sc.op("act", lambda: nc.scalar.activation(out=rs[:, i:i + 1], in_=rs[:, i:i + 1], func=AF.Exp,
                                                  scale=-0.5), r=[ssr[i]], w=[ssr[i]])
        sc.op("dve", lambda: nc.vector.scalar_tensor_tensor(out=ub[k][:], in0=hb[k][:], scalar=rs[:, i:i + 1],
                                                            in1=gain[:], op0=ALU.mult, op1=ALU.mult),
              r=[hbr[k], ssr[i], gres], w=[ubr[k]])
        pt, ptr = cx.psT[i % 2], cx.psTr[i % 2]
        for c in range(8):
            sc.op("pe", lambda: nc.tensor.transpose(out=pt[:, c * 128:(c + 1) * 128],
                                                    in_=ub[k][:, c * 128:(c + 1) * 128], identity=cx.identb[:]),
                  r=[ubr[k], cx.cres], **({"w": [ptr]} if c == 0 else {"pw": [ptr]}))
        src = pt[:].rearrange("p (c s) -> p c s", c=8)
        dst = uT[:, :, i * 128:(i + 1) * 128]
        if i % 2 == 0:
            sc.op("act", lambda: nc.scalar.copy(out=dst, in_=src), r=[ptr], w=[uTres[i]])
        else:
            sc.op("dve", lambda: nc.vector.tensor_copy(out=dst, in_=src), r=[ptr], w=[uTres[i]])


def emit_postnorm_residual(cx, es, li, i, ops, opsr, post_gain, pgres, h_in, hres_in, h_out, hres_out, bufs):
    nc, sc = cx.nc, cx.sc
    k = i % 2
    hb, hbr, tb, tbr, junk, junkr, st, str_ = bufs
    sc.dma("sp", out=hb[k][:], in_=h_in[i * 128:(i + 1) * 128, :], r=[hres_in[i]], w=[hbr[k]])
    sc.op("act", lambda: nc.scalar.activation(out=junk[:], in_=ops[:], func=AF.Square,
                                              accum_out=st[:, 2 * i:2 * i + 1]),
          r=list(opsr), w=[junkr, str_[i]])
    sc.op("act", lambda: nc.scalar.activation(out=st[:, 2 * i + 1:2 * i + 2], in_=st[:, 2 * i:2 * i + 1], func=AF.Ln,
                                              scale=1.0 / D_MODEL, bias=cx.eps_rms[:, 0:1]),
          r=[str_[i], cx.cres], w=[str_[i]])
    sc.op("act", lambda: nc.scalar.activation(out=st[:, 2 * i + 1:2 * i + 2], in_=st[:, 2 * i + 1:2 * i + 2],
                                              func=AF.Exp, scale=-0.5), r=[str_[i]], w=[str_[i]])
    sc.op("dve", lambda: nc.vector.scalar_tensor_tensor(out=tb[k][:], in0=ops[:], scalar=st[:, 2 * i + 1:2 * i + 2],
                                                        in1=post_gain[:], op0=ALU.mult, op1=ALU.mult),
          r=list(opsr) + [str_[i], pgres], w=[tbr[k]])
    sc.op("pool", lambda: nc.gpsimd.tensor_tensor(out=tb[k][:], in0=tb[k][:], in1=hb[k][:], op=ALU.add),
          r=[hbr[k]], w=[tbr[k]])
    sc.dma("sp", out=h_out[i * 128:(i + 1) * 128, :], in_=tb[k][:], r=[tbr[k]], w=[hres_out[i]])


def post_bufs(cx, es, li):
    nc = cx.nc
    hb = [es.enter_context(nc.sbuf_tensor("phb%d_%d" % (li, k), [128, D_MODEL], F32)) for k in range(2)]
    tb = [es.enter_context(nc.sbuf_tensor("ptb%d_%d" % (li, k), [128, D_MODEL], F32)) for k in range(2)]
    junk = es.enter_context(nc.sbuf_tensor("pjunk%d" % li, [128, D_MODEL], BF16))
    st = es.enter_context(nc.sbuf_tensor("pst%d" % li, [128, 2 * NT], F32))
    return (hb, [Res(), Res()], tb, [Res(), Res()], junk, Res(), st, [Res() for _ in range(NT)])


def emit_ret_layer(cx, li, slot, h_in, hres_in, h_out, hres_out):
    nc, sc, P = cx.nc, cx.sc, cx.P
    w_in = P["ret_w_in"][slot]
    w_out = P["ret_w_out"][slot]
    gn = P["ret_gn_gain"][slot]
    RC = cx.RC
    with ExitStack() as es:
        def sb(name, shape, dt):
            return es.enter_context(nc.sbuf_tensor("%s_%d" % (name, li), shape, dt))
        uT = sb("uT", [128, 8, SEQ], BF16)
        uTres = [Res() for _ in range(NT)]
        yT = sb("yT", [128, 16, SEQ], BF16)
        yTres = [Res() for _ in range(NT)]
        with ExitStack() as es0:
            emit_prenorm(cx, es0, li, h_in, hres_in, P["pre_norm_gain"][li], uT, uTres)
        sc.fence()
        with ExitStack() as es1:
            def sb1(name, shape, dt):
                return es1.enter_context(nc.sbuf_tensor("%s_%d" % (name, li), shape, dt))
            cosb = [sb1("cosb%d" % k, [128, 512], F32) for k in range(2)]
            sinb = [sb1("sinb%d" % k, [128, 512], F32) for k in range(2)]
            csr = [Res(), Res()]
            wq = sb1("wq", [128, 8, 256], BF16)
            wk = sb1("wk", [128, 8, 256], BF16)
            wv = sb1("wv", [128, 8, 512], BF16)
            wz = wv
            wres = {n: Res() for n in "qkv"}
            wres["z"] = wres["v"]
            qT = sb1("qT", [128, 2, SEQ], BF16)
            kT = sb1("kT", [128, 2, SEQ], BF16)
            qTr = [Res() for _ in range(4)]
            kTr = [Res() for _ in range(4)]
            ktok = sb1("ktok", [128, NT, 256], BF16)
            ktokr = [Res() for _ in range(NT)]
            vt = sb1("vt", [128, NT, 512], BF16)
            vtr = [Res() for _ in range(NT)]
            zs = sb1("zs", [128, NT, 512], BF16)
            zsr = [Res() for _ in range(NT)]
            gnb = sb1("gnb", [128, 512], F32)
            gnr = Res()
            xa = [sb1("xa0", [128, 512], F32)] * 2
            xb = [sb1("xb0", [128, 512], F32)] * 2
            xar = [Res()] * 2
            xbr = [Res()] * 2
            t1 = [sb1("t1_0", [128, 512], F32)] * 2
            t2 = [sb1("t2_0", [128, 512], F32)] * 2
            t3 = [sb1("t3_0", [128, 512], F32)] * 2
            t4 = [sb1("t4_0", [128, 512], F32)] * 2
            t12r = [Res()] * 2
            t34r = [Res()] * 2
            St = sb1("St", [128, 2, 512], F32)
            Sb = sb1("Sb", [128, 2, 512], BF16)
            Str = [Res(), Res()]
            Sbr = [Res(), Res()]
            at = [sb1("at%d" % k, [128, 128], BF16) for k in range(2)]
            atr = [Res(), Res()]
            on = [sb1("on%d" % k, [128, 512], F32) for k in range(2)]
            onr = [Res(), Res()]
            yb = [sb1("yb%d" % k, [128, 512], BF16) for k in range(2)]
            ybr = [Res(), Res()]
            stt = sb1("stt", [128, 2, 8], F32)
            mv = sb1("mv", [128, 2, 4], F32)
            sttr = [Res(), Res()]
            wviews = {
                "q": lambda hd: w_in[:, hd * 256:(hd + 1) * 256],
                "k": lambda hd: w_in[:, 1024 + hd * 256:1024 + (hd + 1) * 256],
                "v": lambda hd: w_in[:, 2048 + hd * 512:2048 + (hd + 1) * 512],
                "z": lambda hd: w_in[:, 4096 + hd * 512:4096 + (hd + 1) * 512],
            }
            wt = {"q": wq, "k": wk, "v": wv, "z": wz}
            bank = cx.bank
            bres = cx.bres
            nb = 0
            for hd in range(4):
                for n in "qkv":
                    sc.dma("pool", out=wt[n][:], in_=wviews[n](hd).rearrange("(k p) c -> p k c", p=128),
                           w=[wres[n]])
                sc.dma("sp", out=gnb[:], in_=bcast_row(gn[hd * 512:(hd + 1) * 512], 512), w=[gnr])
                for s4 in range(4):
                    cb = s4 % 2
                    sc.dma("sp", out=cosb[cb][:], in_=P["c_cosT"][:, s4 * 512:(s4 + 1) * 512], w=[csr[cb]])
                    sc.dma("sp", out=sinb[cb][:], in_=P["c_sinT"][:, s4 * 512:(s4 + 1) * 512], pw=[csr[cb]])
                    cres = csr[cb]
                    for n, dst, dres, scale in (("q", qT, qTr, 1.0), ("k", kT, kTr, 1.0 / 16.0)):
                        pa, pb = nb % 6, (nb + 1) % 6
                        nb += 2
                        for c, pbk in ((0, pa), (1, pb)):
                            for kc in range(8):
                                sc.op("pe", lambda: nc.tensor.matmul(
                                    bank(pbk), lhsT=wt[n][:, kc, c * 128:(c + 1) * 128],
                                    rhs=uT[:, kc, s4 * 512:(s4 + 1) * 512], start=(kc == 0), stop=(kc == 7)),
                                    r=[wres[n]] + uTres[s4 * 4:(s4 + 1) * 4],
                                    **({"w": [bres[pbk]]} if kc == 0 else {"pw": [bres[pbk]]}))
                        k2 = s4 % 2
                        sc.op("act", lambda: nc.scalar.activation(out=xa[k2][:], in_=bank(pa), func=AF.Copy,
                                                                  scale=scale), r=[bres[pa]], w=[xar[k2]])
                        sc.op("act", lambda: nc.scalar.activation(out=xb[k2][:], in_=bank(pb), func=AF.Copy,
                                                                  scale=scale), r=[bres[pb]], w=[xbr[k2]])
                        cs = cosb[cb][:]
                        sn = sinb[cb][:]
                        sc.op("dve", lambda: nc.vector.tensor_tensor(out=t1[k2][:], in0=xa[k2][:], in1=cs, op=ALU.mult),
                              r=[xar[k2], cres], w=[t12r[k2]])
                        sc.op("dve", lambda: nc.vector.tensor_tensor(out=t2[k2][:], in0=xb[k2][:], in1=sn, op=ALU.mult),
                              r=[xbr[k2], cres], pw=[t12r[k2]])
                        sc.op("dve", lambda: nc.vector.tensor_tensor(out=dst[:, 0, s4 * 512:(s4 + 1) * 512],
                                                                     in0=t1[k2][:], in1=t2[k2][:], op=ALU.subtract),
                              r=[t12r[k2]], w=[dres[s4]])
                        sc.op("pool", lambda: nc.gpsimd.tensor_tensor(out=t3[k2][:], in0=xa[k2][:], in1=sn, op=ALU.mult),
                              r=[xar[k2], cres], w=[t34r[k2]])
                        sc.op("pool", lambda: nc.gpsimd.tensor_tensor(out=t4[k2][:], in0=xb[k2][:], in1=cs, op=ALU.mult),
                              r=[xbr[k2], cres], pw=[t34r[k2]])
                        sc.op("pool", lambda: nc.gpsimd.tensor_tensor(out=dst[:, 1, s4 * 512:(s4 + 1) * 512],
                                                                      in0=t3[k2][:], in1=t4[k2][:], op=ALU.add),
                              r=[t34r[k2]], pw=[dres[s4]])
                for n in "vz":
                    if n == "z":
                        sc.dma("pool", out=wt["z"][:], in_=wviews["z"](hd).rearrange("(k p) c -> p k c", p=128),
                               w=[wres["z"]])
                    for i in range(NT):
                        pbk = nb % 6
                        nb += 1
                        for kc in range(8):
                            sc.op("pe", lambda: nc.tensor.matmul(
                                bank(pbk), lhsT=uT[:, kc, i * 128:(i + 1) * 128], rhs=wt[n][:, kc, :],
                                start=(kc == 0), stop=(kc == 7)),
                                r=[wres[n], uTres[i]],
                                **({"w": [bres[pbk]]} if kc == 0 else {"pw": [bres[pbk]]}))
                        if n == "v":
                            sc.op("dve", lambda: nc.vector.tensor_copy(out=vt[:, i, :], in_=bank(pbk)),
                                  r=[bres[pbk]], w=[vtr[i]])
                        else:
                            sc.op("act", lambda: nc.scalar.activation(out=zs[:, i, :], in_=bank(pbk), func=AF.Silu),
                                  r=[bres[pbk]], w=[zsr[i]])
                            sc.op("pool", lambda: nc.gpsimd.tensor_tensor(out=zs[:, i, :], in0=zs[:, i, :],
                                                                          in1=gnb[:], op=ALU.mult),
                                  r=[gnr], w=[zsr[i]])
                for i in range(NT):
                    pt, ptr = cx.psT[i % 2], cx.psTr[i % 2]
                    for c in range(2):
                        sc.op("pe", lambda: nc.tensor.transpose(out=pt[:, c * 128:(c + 1) * 128],
                                                                in_=kT[:, c, i * 128:(i + 1) * 128],
                                                                identity=cx.identb[:]),
                              r=[kTr[i // 4], cx.cres], **({"w": [ptr]} if c == 0 else {"pw": [ptr]}))
                    sc.op("dve", lambda: nc.vector.tensor_scalar(out=ktok[:, i, :], in0=pt[:, 0:256],
                                                                 scalar1=RC["zeta"][:, hd:hd + 1], scalar2=None,
                                                                 op0=ALU.mult),
                          r=[ptr, cx.cres], w=[ktokr[i]])
                for n in range(NT):
                    k2 = n % 2
                    ps_s, ps_o = (0, 1)[k2], (2, 3)[k2]
                    sl = slice(n * 128, (n + 1) * 128)
                    for c in range(2):
                        sc.op("pe", lambda: nc.tensor.matmul(bank(ps_s)[:, 0:128], lhsT=kT[:, c, sl], rhs=qT[:, c, sl],
                                                             start=(c == 0), stop=(c == 1)),
                              r=[kTr[n // 4], qTr[n // 4]],
                              **({"w": [bres[ps_s]]} if c == 0 else {"pw": [bres[ps_s]]}))
                    sc.op("dve", lambda: nc.vector.tensor_tensor(out=at[k2][:], in0=bank(ps_s)[:, 0:128],
                                                                 in1=RC["innerT"][:, hd * 128:(hd + 1) * 128],
                                                                 op=ALU.mult),
                          r=[bres[ps_s], cx.cres], w=[atr[k2]])
                    sc.op("pe", lambda: nc.tensor.matmul(bank(ps_o), lhsT=at[k2][:], rhs=vt[:, n, :],
                                                         start=True, stop=(n == 0)),
                          r=[atr[k2], vtr[n]], w=[bres[ps_o]])
                    if n > 0:
                        for c in range(2):
                            sc.op("pe", lambda: nc.tensor.matmul(bank(ps_o), lhsT=qT[:, c, sl], rhs=Sb[:, c, :],
                                                                 start=False, stop=(c == 1)),
                                  r=[qTr[n // 4], Sbr[c]], pw=[bres[ps_o]])
                    if n < NT - 1:
                        for c in range(2):
                            pd = 4 + c
                            sc.op("pe", lambda: nc.tensor.matmul(bank(pd), lhsT=ktok[:, n, c * 128:(c + 1) * 128],
                                                                 rhs=vt[:, n, :], start=True, stop=True),
                                  r=[ktokr[n], vtr[n]], w=[bres[pd]])
                            if n == 0:
                                sc.op("dve", lambda: nc.vector.tensor_copy(out=St[:, c, :], in_=bank(pd)),
                                      r=[bres[pd]], w=[Str[c]])
                            else:
                                sc.op("dve", lambda: nc.vector.scalar_tensor_tensor(
                                    out=St[:, c, :], in0=St[:, c, :], scalar=RC["gC"][hd], in1=bank(pd),
                                    op0=ALU.mult, op1=ALU.add), r=[bres[pd]], w=[Str[c]])
                            sc.op("act", lambda: nc.scalar.copy(out=Sb[:, c, :], in_=St[:, c, :]),
                                  r=[Str[c]], w=[Sbr[c]])
                    sc.op("dve", lambda: nc.vector.bn_stats(out=stt[:, k2, 0:6], in_=bank(ps_o)),
                          r=[bres[ps_o]], w=[sttr[k2]])
                    sc.op("dve", lambda: nc.vector.bn_aggr(out=mv[:, k2, 0:2], in_=stt[:, k2, 0:6]),
                          r=[], w=[sttr[k2]])
                    sc.op("act", lambda: nc.scalar.activation(out=mv[:, k2, 2:3], in_=mv[:, k2, 1:2], func=AF.Ln,
                                                              bias=RC["epsp"][:, hd:hd + 1]),
                          r=[cx.cres], w=[sttr[k2]])
                    sc.op("act", lambda: nc.scalar.activation(out=mv[:, k2, 2:3], in_=mv[:, k2, 2:3], func=AF.Exp,
                                                              scale=-0.5), w=[sttr[k2]])
                    sc.op("dve", lambda: nc.vector.scalar_tensor_tensor(
                        out=mv[:, k2, 3:4], in0=mv[:, k2, 0:1], scalar=-1.0, in1=mv[:, k2, 2:3],
                        op0=ALU.mult, op1=ALU.mult), w=[sttr[k2]])
                    sc.op("act", lambda: nc.scalar.activation(out=on[k2][:], in_=bank(ps_o), func=AF.Identity,
                                                              scale=mv[:, k2, 2:3], bias=mv[:, k2, 3:4]),
                          r=[bres[ps_o], sttr[k2]], w=[onr[k2]])
                    sc.op("pool", lambda: nc.gpsimd.tensor_tensor(out=yb[k2][:], in0=on[k2][:], in1=zs[:, n, :],
                                                                  op=ALU.mult),
                          r=[onr[k2], zsr[n]], w=[ybr[k2]])
                    pt, ptr = cx.psT[k2], cx.psTr[k2]
                    for c in range(4):
                        sc.op("pe", lambda: nc.tensor.transpose(out=pt[:, c * 128:(c + 1) * 128],
                                                                in_=yb[k2][:, c * 128:(c + 1) * 128],
                                                                identity=cx.identb[:]),
                              r=[ybr[k2], cx.cres], **({"w": [ptr]} if c == 0 else {"pw": [ptr]}))
                    sc.op("act", lambda: nc.scalar.copy(out=yT[:, hd * 4:(hd + 1) * 4, sl],
                                                        in_=pt[:, 0:512].rearrange("p (c s) -> p c s", c=4)),
                          r=[ptr], **({"w": [yTres[n]]} if hd == 0 else {"pw": [yTres[n]]}))
                if hd == 3:
                    cx.dump("qT", qT[:], qTr, BF16)
                    cx.dump("kT", kT[:], kTr, BF16)
                    cx.dump("vt", vt[:], vtr, BF16)
                    cx.dump("zs", zs[:], zsr, BF16)
                    cx.dump("ktok", ktok[:], ktokr, BF16)
                    cx.dump("St", St[:], Str, F32)
            cx.dump("uT", uT[:], uTres, BF16)
            cx.dump("yT", yT[:], yTres, BF16)
        sc.fence()
        with ExitStack() as es2:
            wo = es2.enter_context(nc.sbuf_tensor("wo_%d" % li, [128, 16, D_MODEL], BF16))
            wor = Res()
            wov = w_out.rearrange("(k p) c -> p k c", p=128)
            for q4 in range(4):
                sc.dma("pool", out=wo[:, q4 * 4:(q4 + 1) * 4, :], in_=wov[:, q4 * 4:(q4 + 1) * 4, :],
                       **({"w": [wor]} if q4 == 0 else {"pw": [wor]}))
            pgain = es2.enter_context(nc.sbuf_tensor("pog_%d" % li, [128, D_MODEL], F32))
            pgres = Res()
            sc.dma("sp", out=pgain[:], in_=bcast_row(P["post_norm_gain"][li], D_MODEL), w=[pgres])
            bufs = post_bufs(cx, es2, li)
            for i in range(NT):
                ops, opsr = cx.pbig[i % 3], cx.pbigr[i % 3]
                for half in range(2):
                    for kc in range(16):
                        sc.op("pe", lambda: nc.tensor.matmul(
                            ops[:, half * 512:(half + 1) * 512], lhsT=yT[:, kc, i * 128:(i + 1) * 128],
                            rhs=wo[:, kc, half * 512:(half + 1) * 512], start=(kc == 0), stop=(kc == 15)),
                            r=[yTres[i], wor],
                            **({"w": opsr} if (kc == 0 and half == 0) else {"pw": opsr}))
                emit_postnorm_residual(cx, es2, li, i, ops, opsr, pgain, pgres, h_in, hres_in, h_out, hres_out, bufs)
    sc.fence()


def _t5_bucket(n):
    n = np.maximum(n, 0)
    nf = np.maximum(n, 1).astype(np.float32)
    large = 16 + (np.log(nf / np.float32(16)) / np.float32(math.log(128 / 16)) * np.float32(16)).astype(np.int32)
    large = np.minimum(large, 31)
    return np.where(n < 16, n, large)


def _nsa_consts():
    def onehot(n, valid):
        b = _t5_bucket(n)
        oh = np.zeros((33, n.shape[0]), np.float32)
        idx = np.arange(n.shape[0])
        oh[b[valid], idx[valid]] = 1.0
        oh[31, idx[valid]] -= 1.0
        oh[32, idx[~valid]] = 1.0
        return oh
    nw = np.arange(768) - 127
    ohw = onehot(nw, (nw >= 0) & (nw < 512))
    ncm = np.arange(4096) - 2047
    ohc = onehot(ncm, ncm >= 0)
    E = np.zeros((128, SEQ), np.float32)
    E[np.arange(SEQ) // 64, np.arange(SEQ)] = 1.0
    cs = np.arange(CMP_N) * 16
    j = np.arange(32)
    ov = ((cs[:, None] < (j[None, :] + 1) * 64) & (cs[:, None] + 32 > j[None, :] * 64)).astype(np.float32)
    t = np.arange(SEQ)
    cur = t // 64
    valid = j[None, :] * 64 <= t[:, None]
    forced = (j[None, :] == 0) | (j[None, :] == cur[:, None]) | (j[None, :] == cur[:, None] - 1)
    fm = np.where(valid, np.where(forced, 1000.0, 0.0), -1e30).astype(np.float32)
    fm = fm[1024:].reshape(8, 128, 32).transpose(1, 0, 2).reshape(128, 256)
    return {"c_ohw": ohw, "c_ohc": ohc, "c_E": E, "c_ov": np.ascontiguousarray(ov),
            "c_forced": np.ascontiguousarray(fm)}


def emit_nsa_prologue(cx, es):
    nc, sc, P = cx.nc, cx.sc, cx.P
    fw_d = nc.dram_tensor("fw_d", [16, 768], BF16, kind="Internal")
    fc_d = nc.dram_tensor("fc_d", [16, 4096], BF16, kind="Internal")
    cx.fc_d = fc_d
    cx.BM = es.enter_context(nc.sbuf_tensor("BM", [128, 16, 2, 128], BF16))
    cx.BM4 = es.enter_context(nc.sbuf_tensor("BM4", [128, 128], BF16))
    cx.Epad = es.enter_context(nc.sbuf_tensor("Epad", [128, SEQ], BF16))
    cx.bmres = Res()
    cx.fcres = Res()
    sc.dma("pool", out=cx.Epad[:], in_=P["c_E"][:, :], pw=[cx.bmres])
    with ExitStack() as ep:
        tab = ep.enter_context(nc.sbuf_tensor("tab", [33, 16], F32))
        ohw = ep.enter_context(nc.sbuf_tensor("ohw", [33, 768], F32))
        ohc = ep.enter_context(nc.sbuf_tensor("ohc", [33, 4096], F32))
        fwb = ep.enter_context(nc.sbuf_tensor("fwb", [16, 768], BF16))
        fcb = ep.enter_context(nc.sbuf_tensor("fcb", [16, 4096], BF16))
        tr, fr = Res(), Res()
        sc.op("dve", lambda: nc.vector.memset(tab[32:33, :], NEG), pw=[tr])
        sc.dma("sp", out=tab[0:32, :], in_=P["rel_bias_table"][:, :], pw=[tr])
        sc.dma("sp", out=ohw[:], in_=P["c_ohw"][:, :], pw=[tr])
        for q in range(4):
            sc.dma("sp", out=ohc[:, q * 1024:(q + 1) * 1024], in_=P["c_ohc"][:, q * 1024:(q + 1) * 1024], pw=[tr])
        bank, bres = cx.bank, cx.bres
        for q, (c0, c1) in enumerate(((0, 512), (512, 768))):
            sc.op("pe", lambda: nc.tensor.matmul(bank(q)[0:16, 0:c1 - c0], lhsT=tab[:, :], rhs=ohw[:, c0:c1],
                                                 start=True, stop=True), r=[tr], w=[bres[q]])
            sc.op("dve", lambda: nc.vector.tensor_copy(out=fwb[:, c0:c1], in_=bank(q)[0:16, 0:c1 - c0]),
                  r=[bres[q]], pw=[fr])
        for q in range(8):
            b = 2 + q % 4
            sc.op("pe", lambda: nc.tensor.matmul(bank(b)[0:16, :], lhsT=tab[:, :], rhs=ohc[:, q * 512:(q + 1) * 512],
                                                 start=True, stop=True), r=[tr], w=[bres[b]])
            sc.op("dve", lambda: nc.vector.tensor_copy(out=fcb[:, q * 512:(q + 1) * 512], in_=bank(b)[0:16, :]),
                  r=[bres[b]], pw=[fr])
        dres = Res()
        sc.dma("sp", out=fw_d.ap(), in_=fwb[:], r=[fr], w=[dres])
        sc.dma("sp", out=fc_d.ap(), in_=fcb[:], r=[fr], w=[cx.fcres])
        for h0 in range(0, 16, 4):
            src = bass.AP(fw_d, h0 * 768, [[1, 128], [768, 4], [128, 2], [1, 128]])
            sc.dma("sp", out=cx.BM[:, h0:h0 + 4, :, :], in_=src, r=[dres], pw=[cx.bmres])
        sc.dma("sp", out=cx.BM4[:], in_=bass.AP(fw_d, 512, [[1, 128], [1, 128]]), r=[dres], pw=[cx.bmres])
        sc.finish([cx.bmres, cx.fcres, dres])
    sc.fence()


def emit_nsa_layer(cx, li, slot, h_in, hres_in, h_out, hres_out):
    nc, sc, P = cx.nc, cx.sc, cx.P
    w_in = P["nsa_w_in"][slot]
    w_out = P["nsa_w_out"][slot]
    bank, bres = cx.bank, cx.bres
    identb = cx.identb
    with ExitStack() as es:
        def sbL(name, shape, dt):
            return es.enter_context(nc.sbuf_tensor("%s_%d" % (name, li), shape, dt))
        uT = sbL("uT", [128, 8, SEQ], BF16)
        uTres = [Res() for _ in range(NT)]
        y = sbL("y", [128, NT, D_MODEL], BF16)
        yres = [Res() for _ in range(NT)]
        with ExitStack() as es0:
            emit_prenorm(cx, es0, li, h_in, hres_in, P["pre_norm_gain"][li], uT, uTres)
        sc.fence()
        if stage(0):
            return
        with ExitStack() as es1:
            def sb(name, shape, dt):
                return es1.enter_context(nc.sbuf_tensor("%s_%d" % (name, li), shape, dt))
            wview = w_in.rearrange("(k p) c -> p k c", p=128)
            W1p = {n: sb("W1p" + n, [128, 16, 256], BF16) for n in "kv"}
            w2k = sb("w2k", [128, 2, 128], BF16)
            w2v = sb("w2v", [128, 2, 64], BF16)
            pos2 = {n: sb("pos2" + n, [128, 16], BF16) for n in "kv"}
            pb = {n: sb("pb" + n, [128, 2], F32) for n in "kv"}
            wg = sb("wg", [128, 8, 48], BF16)
            forced = sb("forced", [128, 8, 32], F32)
            lres = Res()
            for n in "kv":
                sc.dma("pool", out=W1p[n][:], in_=P["nsa_cmp_%s_w1" % n][slot].rearrange("(lp p) c -> p lp c", p=128),
                       pw=[lres])
                pos = P["nsa_cmp_%s_pos" % n][slot]
                for par in range(2):
                    src = bass.AP(pos.tensor, pos.offset + par * 64, [[1, 64], [128, 16]])
                    sc.dma("pool", out=pos2[n][par * 64:(par + 1) * 64, :], in_=src, pw=[lres],
                           allow_slow_non_contiguous=True)
            w2kv = P["nsa_cmp_k_w2"][slot].rearrange("(c p) d -> p c d", p=128)
            sc.dma("pool", out=w2k[:, :, 0:64], in_=w2kv, pw=[lres])
            sc.dma("pool", out=w2k[:, :, 64:128], in_=w2kv, pw=[lres])
            sc.dma("pool", out=w2v[:], in_=P["nsa_cmp_v_w2"][slot].rearrange("(c p) d -> p c d", p=128), pw=[lres])
            sc.dma("pool", out=wg[:], in_=wview[:, :, 2560:2608], pw=[lres])
            sc.dma("sp", out=forced[:], in_=P["c_forced"][:, :].rearrange("p (a b) -> p a b", a=8), pw=[lres])
            wq = sb("wq", [128, 8, 256], BF16)
            wks2 = sb("wks2", [128, 8, 128], BF16)
            wkw2 = sb("wkw2", [128, 8, 128], BF16)
            wkc2 = sb("wkc2", [128, 8, 128], BF16)
            wvc2 = sb("wvc2", [128, 8, 128], BF16)
            wtok = sb("wtok", [128, 8, 896], BF16)
            wres = Res()
            qT2 = sb("qT2", [128, 2, SEQ], BF16)
            qres = [Res() for _ in range(4)]
            kspad = sb("kspad", [128, 2, SEQ], BF16)
            kwpad = sb("kwpad", [128, 2, SEQ], BF16)
            ksres = [Res() for _ in range(4)]
            kwres = [Res() for _ in range(4)]
            kc2 = sb("kc2", [128, SEQ], BF16)
            vc2 = sb("vc2", [128, SEQ], BF16)
            kc2res, vc2res = Res(), Res()
            vsw = sb("vsw", [128, NT, 2, 80], BF16)
            vres = [Res() for _ in range(NT)]
            zs = sb("zs", [128, NT, 3, 256], BF16)
            zres = [Res() for _ in range(NT)]
            graw = sb("graw", [128, NT, 48], F32)
            gate = sb("gate", [128, NT, 48], F32)
            gres = Res()
            hT = {n: sb("hT" + n, [128, 2, 128], BF16) for n in "kv"}
            hres = {n: Res() for n in "kv"}
            kcpad = sb("kcpad", [128, 2, 128], BF16)
            kcres = Res()
            vcaug = sb("vcaug", [128, 98], BF16)
            vcres = Res()
            ovf = sb("ovf", [128, 32], F32)
            Pc = [sb("Pc%d" % k, [128, 512], BF16) for k in range(4)]
            Pcres = [Res() for _ in range(4)]
            cbm = [sb("cbm%d" % k, [128, 512], BF16) for k in range(2)]
            cbmres = [Res(), Res()]
            pbuf = [sb("pbuf%d" % k, [128, 512], BF16) for k in range(3)]
            pbres = [Res() for _ in range(3)]
            selbT = sb("selbT", [128, 512], BF16)
            selres = [Res() for _ in range(4)]
            imp = sb("imp", [128, 4, 32], F32)
            impres = [Res() for _ in range(4)]
            tk = sb("tk", [128, 4, 32 + 32 + 8 + 8 + 32], F32)
            selb = sb("selb", [128, 4, 32], BF16)
            tkres = [Res() for _ in range(4)]
            pp = sb("pp", [128, 2, 16], F32)
            ppres = [Res(), Res()]
            tmp3 = sb("tmp3", [128, 4, 32], F32)
            tmp3res = Res()
            yacc = [sb("yacc%d" % k, [128, 256], F32) for k in range(4)]
            yaccres = [Res() for _ in range(4)]
            ytmp = [sb("ytmp%d" % k, [128, 256], F32) for k in range(2)]
            ytmpres = [Res(), Res()]
            ires = Res()
            sc.op("pool", lambda: nc.gpsimd.memset(kspad[64:128, 0, :], 0.0), pw=ksres)
            sc.op("pool", lambda: nc.gpsimd.memset(kspad[0:64, 1, :], 0.0), pw=ksres)
            sc.op("pool", lambda: nc.gpsimd.memset(kwpad[64:128, 0, :], 0.0), pw=kwres)
            sc.op("pool", lambda: nc.gpsimd.memset(kwpad[0:64, 1, :], 0.0), pw=kwres)
            sc.op("pool", lambda: nc.gpsimd.memset(kcpad[64:128, 0, :], 0.0), pw=[kcres])
            sc.op("pool", lambda: nc.gpsimd.memset(kcpad[0:64, 1, :], 0.0), pw=[kcres])
            sc.op("pool", lambda: nc.gpsimd.memset(kc2[64:128, SEQ - 1:SEQ], 0.0), pw=[kc2res])
            sc.op("pool", lambda: nc.gpsimd.memset(vc2[64:128, SEQ - 1:SEQ], 0.0), pw=[vc2res])
            sc.op("pool", lambda: nc.gpsimd.memset(vsw[:, :, :, 64:65], 1.0), pw=vres)
            sc.op("pool", lambda: nc.gpsimd.memset(vcaug[:, 64:66], 1.0), pw=[vcres])
            sc.op("pool", lambda: nc.gpsimd.memset(selbT[:], 0.0), pw=selres)
            sc.dma("sp", out=ovf[0:CMP_N, :], in_=P["c_ov"][:, :], w=[ires])
            sc.op("dve", lambda: nc.vector.tensor_copy(out=vcaug[0:CMP_N, 66:98], in_=ovf[0:CMP_N, :]),
                  r=[ires], pw=[vcres])

            def load_group_weights(g):
                sc.dma("pool", out=wq[:], in_=wview[:, :, 256 * g:256 * (g + 1)], w=[wres])
                for t, dst in ((0, wkc2), (1, wvc2), (2, wks2), (4, wkw2)):
                    c0 = 1024 + 256 * t + 64 * g
                    for half in range(2):
                        sc.dma("pool", out=dst[:, :, half * 64:(half + 1) * 64], in_=wview[:, :, c0:c0 + 64], pw=[wres])
                for t, c in ((3, 0), (5, 64)):
                    c0 = 1024 + 256 * t + 64 * g
                    sc.dma("pool", out=wtok[:, :, c:c + 64], in_=wview[:, :, c0:c0 + 64], pw=[wres])
                for br in range(3):
                    c0 = 2608 + 1024 * br + 256 * g
                    sc.dma("pool", out=wtok[:, :, 128 + 256 * br:128 + 256 * (br + 1)], in_=wview[:, :, c0:c0 + 256],
                           pw=[wres])

            nbk = [0]

            def nextbank():
                b = nbk[0] % 6
                nbk[0] += 1
                return b

            def proj_fm(wt, ncols, s4, b):
                for kc in range(8):
                    sc.op("pe", lambda: nc.tensor.matmul(bank(b), lhsT=wt[:, kc, ncols:ncols + 128],
                                                         rhs=uT[:, kc, s4 * 512:(s4 + 1) * 512],
                                                         start=(kc == 0), stop=(kc == 7)),
                          r=[wres, lres] + uTres[s4 * 4:(s4 + 1) * 4],
                          **({"w": [bres[b]]} if kc == 0 else {"pw": [bres[b]]}))

            njob = [0]
            nacc = [0]
            load_group_weights(0)
            for n in "kv":
                for hc in range(2):
                    b = nextbank()
                    for lp in range(16):
                        sc.op("pe", lambda: nc.tensor.matmul(bank(b)[:, 0:1], lhsT=W1p[n][:, lp, hc * 128:(hc + 1) * 128],
                                                             rhs=pos2[n][:, lp:lp + 1], start=(lp == 0), stop=(lp == 15)),
                              r=[lres], **({"w": [bres[b]]} if lp == 0 else {"pw": [bres[b]]}))
                    sc.op("dve", lambda: nc.vector.tensor_copy(out=pb[n][:, hc:hc + 1], in_=bank(b)[:, 0:1]),
                          r=[bres[b]], pw=[lres])

            if stage(1):
                return
            for g in range(4):
                for s4 in range(4):
                    sl = slice(s4 * 512, (s4 + 1) * 512)
                    for m in range(2):
                        b = nextbank()
                        proj_fm(wq, m * 128, s4, b)
                        sc.op("act", lambda: nc.scalar.activation(out=qT2[:, m, sl], in_=bank(b), func=AF.Copy,
                                                                  scale=0.125),
                              r=[bres[b]], **({"w": [qres[s4]]} if m == 0 else {"pw": [qres[s4]]}))
                    if stage(1.1):
                        return
                    for wt, dst, dres in ((wks2, kspad, ksres), (wkw2, kwpad, kwres)):
                        b = nextbank()
                        proj_fm(wt, 0, s4, b)
                        sc.op("dve", lambda: nc.vector.tensor_copy(out=dst[0:64, 0, sl], in_=bank(b)[0:64, :]),
                              r=[bres[b]], w=[dres[s4]])
                        sc.op("act", lambda: nc.scalar.copy(out=dst[64:128, 1, sl], in_=bank(b)[64:128, :]),
                              r=[bres[b]], pw=[dres[s4]])
                    if stage(1.2):
                        return
                    for wt, dst, dres in ((wkc2, kc2, kc2res), (wvc2, vc2, vc2res)):
                        b = nextbank()
                        proj_fm(wt, 0, s4, b)
                        sc.op("dve", lambda: nc.vector.tensor_copy(out=dst[0:64, sl], in_=bank(b)[0:64, :]),
                              r=[bres[b]], **({"w": [dres]} if s4 == 0 else {"pw": [dres]}))
                        if s4 == 0:
                            sc.op("act", lambda: nc.scalar.copy(out=dst[64:128, 0:511], in_=bank(b)[64:128, 1:512]),
                                  r=[bres[b]], pw=[dres])
                        else:
                            sc.op("act", lambda: nc.scalar.copy(out=dst[64:128, s4 * 512 - 1:(s4 + 1) * 512 - 1],
                                                                in_=bank(b)[64:128, :]),
                                  r=[bres[b]], pw=[dres])
                if stage(1.4):
                    return
                for i in range(NT):
                    if i == 1 and stage(1.5):
                        return
                    if i == TOKSTOP:
                        return
                    ba, bb = nextbank(), nextbank()
                    bg = nextbank() if g == 0 else None
                    for kc in range(8):
                        lhs = uT[:, kc, i * 128:(i + 1) * 128]
                        fl = dict(start=(kc == 0), stop=(kc == 7))
                        wk = "w" if kc == 0 else "pw"
                        sc.op("pe", lambda: nc.tensor.matmul(bank(ba)[:, 0:384], lhsT=lhs, rhs=wtok[:, kc, 0:384], **fl),
                              r=[wres, uTres[i]], **{wk: [bres[ba]]})
                    for kc in range(8):
                        lhs = uT[:, kc, i * 128:(i + 1) * 128]
                        fl = dict(start=(kc == 0), stop=(kc == 7))
                        wk = "w" if kc == 0 else "pw"
                        sc.op("pe", lambda: nc.tensor.matmul(bank(bb), lhsT=lhs, rhs=wtok[:, kc, 384:896], **fl),
                              r=[wres, uTres[i]], **{wk: [bres[bb]]})
                    if g == 0:
                        for kc in range(8):
                            lhs = uT[:, kc, i * 128:(i + 1) * 128]
                            fl = dict(start=(kc == 0), stop=(kc == 7))
                            wk = "w" if kc == 0 else "pw"
                            sc.op("pe", lambda: nc.tensor.matmul(bank(bg)[:, 0:48], lhsT=lhs, rhs=wg[:, kc, :], **fl),
                                  r=[lres, uTres[i]], **{wk: [bres[bg]]})
                    if i == 0 and stage(1.45):
                        return
                    sc.op("dve", lambda: nc.vector.tensor_copy(
                        out=vsw[:, i, :, 0:64], in_=bank(ba)[:, 0:128].rearrange("p (a d) -> p a d", a=2)),
                        r=[bres[ba]], w=[vres[i]])
                    sc.op("act", lambda: nc.scalar.activation(out=zs[:, i, 0, :], in_=bank(ba)[:, 128:384], func=AF.Silu),
                          r=[bres[ba], vres[i]], w=[zres[i]])
                    sc.op("act", lambda: nc.scalar.activation(out=zs[:, i, 1:3, :],
                                                              in_=bank(bb).rearrange("p (a d) -> p a d", a=2),
                                                              func=AF.Silu),
                          r=[bres[bb]], pw=[zres[i]])
                    if g == 0:
                        sc.op("dve", lambda: nc.vector.tensor_copy(out=graw[:, i, :], in_=bank(bg)[:, 0:48]),
                              r=[bres[bg]], pw=[gres])
                if stage(2):
                    return
                for n, src2, sres2 in (("k", kc2, kc2res), ("v", vc2, vc2res)):
                    for hc in range(2):
                        b = nextbank()
                        for lp in range(16):
                            sc.op("pe", lambda: nc.tensor.matmul(
                                bank(b)[:, 0:CMP_N], lhsT=W1p[n][:, lp, hc * 128:(hc + 1) * 128],
                                rhs=src2[:, 2 * lp:2 * lp + 16 * (CMP_N - 1) + 1:16],
                                start=(lp == 0), stop=(lp == 15)),
                                r=[lres, sres2], **({"w": [bres[b]]} if lp == 0 else {"pw": [bres[b]]}))
                        sc.op("act", lambda: nc.scalar.activation(out=hT[n][:, hc, 0:CMP_N], in_=bank(b)[:, 0:CMP_N],
                                                                  func=AF.Silu, bias=pb[n][:, hc:hc + 1]),
                              r=[bres[b], lres], **({"w": [hres[n]]} if hc == 0 else {"pw": [hres[n]]}))
                b = nextbank()
                for hc in range(2):
                    sc.op("pe", lambda: nc.tensor.matmul(bank(b)[:, 0:CMP_N], lhsT=w2k[:, hc, :], rhs=hT["k"][:, hc, 0:CMP_N],
                                                         start=(hc == 0), stop=(hc == 1)),
                          r=[lres, hres["k"]], **({"w": [bres[b]]} if hc == 0 else {"pw": [bres[b]]}))
                sc.op("dve", lambda: nc.vector.tensor_copy(out=kcpad[0:64, 0, 0:CMP_N], in_=bank(b)[0:64, 0:CMP_N]),
                      r=[bres[b]], w=[kcres])
                sc.op("dve", lambda: nc.vector.tensor_copy(out=kcpad[64:128, 1, 0:CMP_N], in_=bank(b)[64:128, 0:CMP_N]),
                      r=[bres[b]], pw=[kcres])
                b = nextbank()
                for hc in range(2):
                    sc.op("pe", lambda: nc.tensor.matmul(bank(b)[0:CMP_N, 0:64], lhsT=hT["v"][:, hc, 0:CMP_N], rhs=w2v[:, hc, :],
                                                         start=(hc == 0), stop=(hc == 1)),
                          r=[lres, hres["v"]], **({"w": [bres[b]]} if hc == 0 else {"pw": [bres[b]]}))
                sc.op("dve", lambda: nc.vector.tensor_copy(out=vcaug[0:CMP_N, 0:64], in_=bank(b)[0:CMP_N, 0:64]),
                      r=[bres[b]], w=[vcres])
                if g < 3:
                    load_group_weights(g + 1)
                if g == 0:
                    sc.op("act", lambda: nc.scalar.activation(out=gate[:], in_=graw[:], func=AF.Exp, scale=-1.0),
                          r=[gres], w=[gres])
                    sc.op("dve", lambda: nc.vector.tensor_scalar(out=gate[:], in0=gate[:], scalar1=1.0, scalar2=None,
                                                                 op0=ALU.add), w=[gres])
                    sc.op("dve", lambda: nc.vector.reciprocal(out=gate[:], in_=gate[:]), w=[gres])

                if stage(3):
                    return
                def attn_tile(i, t, br, kpad, kres_, vidx, kts, bmfn, use_sel):
                    ob = 3 + nacc[0] % 2
                    nacc[0] += 1
                    jobs = []
                    for r in range(4):
                        for a in range(0, len(kts), 4):
                            jobs.append((r, kts[a:a + 4]))
                    qsl = slice(i * 128, (i + 1) * 128)

                    def qk(job, jn):
                        r, ks_ = job
                        sb_ = jn % 3
                        first = True
                        for a, kt in enumerate(ks_):
                            extra = []
                            if use_sel:
                                extra.append((cx.Epad[:, kt * 128:(kt + 1) * 128], selbT[:, t * 128:(t + 1) * 128],
                                              [cx.bmres, selres[t]]))
                            bm = bmfn(4 * g + r, i - kt)
                            if bm is not None:
                                extra.append((cx.antib[:], bm, [cx.bmres, cx.cres]))
                            out = bank(sb_)[:, a * 128:(a + 1) * 128]
                            sc.op("pe", lambda: nc.tensor.matmul(out, lhsT=kpad[:, r % 2, kt * 128:(kt + 1) * 128],
                                                                 rhs=qT2[:, r // 2, qsl], start=True,
                                                                 stop=(len(extra) == 0)),
                                  r=[kres_[kt // 4], qres[i // 4]],
                                  **({"w": [bres[sb_]]} if first else {"pw": [bres[sb_]]}))
                            first = False
                            for e, (l_, r_, rr_) in enumerate(extra):
                                sc.op("pe", lambda: nc.tensor.matmul(out, lhsT=l_, rhs=r_, start=False,
                                                                     stop=(e == len(extra) - 1)),
                                      r=rr_, pw=[bres[sb_]])

                    def expv(job, jn):
                        r, ks_ = job
                        sb_ = jn % 3
                        w_ = len(ks_) * 128
                        sc.op("act", lambda: nc.scalar.activation(out=pbuf[sb_][:, 0:w_], in_=bank(sb_)[:, 0:w_],
                                                                  func=AF.Exp),
                              r=[bres[sb_]], w=[pbres[sb_]])
                        for a, kt in enumerate(ks_):
                            fst = (kt == kts[0])
                            sc.op("pe", lambda: nc.tensor.matmul(bank(ob)[:, r * 65:(r + 1) * 65],
                                                                 lhsT=pbuf[sb_][:, a * 128:(a + 1) * 128],
                                                                 rhs=vsw[:, kt, vidx, 0:65], start=fst, stop=(kt == kts[-1])),
                                  r=[pbres[sb_], vres[kt]],
                                  **({"w": [bres[ob]]} if (fst and r == 0) else {"pw": [bres[ob]]}))

                    j0 = njob[0]
                    qk(jobs[0], j0)
                    for n_, job in enumerate(jobs):
                        if n_ + 1 < len(jobs):
                            qk(jobs[n_ + 1], j0 + n_ + 1)
                        expv(job, j0 + n_)
                    njob[0] += len(jobs)
                    return ob

                def combine(ob, i, br, width, first, last):
                    k2 = nacc[0] % 2
                    o3 = bank(ob)[:, 0:4 * width].rearrange("p (r c) -> p r c", r=4)
                    rden = pp[:, k2, 0:4]
                    fac = pp[:, k2, 4:8]
                    sc.op("dve", lambda: nc.vector.tensor_scalar(out=rden, in0=o3[:, :, 64], scalar1=1e-30, scalar2=None,
                                                                 op0=ALU.max), r=[bres[ob]], w=[ppres[k2]])
                    sc.op("dve", lambda: nc.vector.reciprocal(out=rden, in_=rden), w=[ppres[k2]])
                    sc.op("dve", lambda: nc.vector.tensor_tensor(out=fac, in0=rden,
                                                                 in1=gate[:, i, 16 * br + 4 * g:16 * br + 4 * g + 4],
                                                                 op=ALU.mult), r=[gres], w=[ppres[k2]])
                    ya = yacc[i % 4]
                    yar = yaccres[i % 4]
                    dst = ya if first else ytmp[k2]
                    dres = yar if first else ytmpres[k2]
                    sc.op("dve", lambda: nc.vector.tensor_tensor(
                        out=dst[:].rearrange("p (r d) -> p r d", r=4), in0=o3[:, :, 0:64],
                        in1=fac.unsqueeze(2).to_broadcast([128, 4, 64]), op=ALU.mult),
                        r=[bres[ob], ppres[k2]], w=[dres])
                    sc.op("pool", lambda: nc.gpsimd.tensor_tensor(out=dst[:], in0=dst[:], in1=zs[:, i, br, :], op=ALU.mult),
                          r=[zres[i]], w=[dres])
                    if not first:
                        if last:
                            sc.op("pool", lambda: nc.gpsimd.tensor_tensor(out=y[:, i, 256 * g:256 * (g + 1)], in0=ya[:],
                                                                          in1=dst[:], op=ALU.add),
                                  r=[dres, yar], pw=[yres[i]])
                        else:
                            sc.op("pool", lambda: nc.gpsimd.tensor_tensor(out=ya[:], in0=ya[:], in1=dst[:], op=ALU.add),
                                  r=[dres], w=[yar])
                    return rden

                def bm_selwin(h, d):
                    if d == 0 or d == 1:
                        return cx.BM[:, h, d, :]
                    if d == 4:
                        return cx.BM4[:]
                    return None

                def bm_sel(h, d):
                    return cx.BM[:, h, d, :] if d in (0, 1) else None

                for i4 in range(4):
                    sl = slice(i4 * 512, (i4 + 1) * 512)
                    for r in range(4):
                        h = 4 * g + r
                        cb = (4 * i4 + r) % 2
                        src = bass.AP(cx.fc_d, h * 4096 + i4 * 512, [[16, CMP_N], [1, 512]])
                        sc.dma("sp", out=cbm[cb][0:CMP_N, :], in_=src, r=[cx.fcres], w=[cbmres[cb]])
                        sb_ = njob[0] % 3
                        njob[0] += 1
                        sc.op("pe", lambda: nc.tensor.matmul(bank(sb_)[0:CMP_N, :], lhsT=kcpad[:, r % 2, 0:CMP_N],
                                                             rhs=qT2[:, r // 2, sl], start=True, stop=False),
                              r=[kcres, qres[i4]], w=[bres[sb_]])
                        sc.op("pe", lambda: nc.tensor.matmul(bank(sb_)[0:CMP_N, :], lhsT=cx.antib[0:CMP_N, 1:128],
                                                             rhs=cbm[cb][0:CMP_N, :], start=False, stop=True),
                              r=[cbmres[cb], cx.cres], pw=[bres[sb_]])
                        sc.op("act", lambda: nc.scalar.activation(out=Pc[r][0:CMP_N, :], in_=bank(sb_)[0:CMP_N, :],
                                                                  func=AF.Exp),
                              r=[bres[sb_]], w=[Pcres[r]])
                    for t in range(4):
                        i = 4 * i4 + t
                        ob = 3 + nacc[0] % 2
                        nacc[0] += 1
                        for r in range(4):
                            sc.op("pe", lambda: nc.tensor.matmul(bank(ob)[:, r * 98:(r + 1) * 98],
                                                                 lhsT=Pc[r][0:CMP_N, t * 128:(t + 1) * 128],
                                                                 rhs=vcaug[0:CMP_N, :], start=True, stop=True),
                                  r=[Pcres[r], vcres], **({"w": [bres[ob]]} if r == 0 else {"pw": [bres[ob]]}))
                        rden = combine(ob, i, 0, 98, True, False)
                        if i >= 8:
                            o3 = bank(ob)[:, 0:392].rearrange("p (r c) -> p r c", r=4)
                            k2 = nacc[0] % 2
                            sc.op("dve", lambda: nc.vector.tensor_tensor(
                                out=tmp3[:], in0=o3[:, :, 66:98], in1=rden.unsqueeze(2).to_broadcast([128, 4, 32]),
                                op=ALU.mult), r=[bres[ob], ppres[k2]], w=[tmp3res])
                            s1 = tk[:, t, 0:32]
                            s2 = tk[:, t, 32:64]
                            m1 = tk[:, t, 64:72]
                            m2 = tk[:, t, 72:80]
                            sc.op("dve", lambda: nc.vector.tensor_reduce(
                                out=s1, in_=tmp3[:].rearrange("p r j -> p j r"), axis=mybir.AxisListType.X, op=ALU.add),
                                r=[tmp3res], w=[tkres[t]])
                            sc.op("dve", lambda: nc.vector.tensor_tensor(out=s1, in0=s1, in1=forced[:, i - 8, :],
                                                                         op=ALU.add), r=[lres], w=[tkres[t]])
                            sc.op("dve", lambda: nc.vector.max(out=m1, in_=s1), w=[tkres[t]])
                            sc.op("dve", lambda: nc.vector.match_replace(out=s2, in_to_replace=m1, in_values=s1,
                                                                         imm_value=-3.0e38), w=[tkres[t]])
                            sc.op("dve", lambda: nc.vector.max(out=m2, in_=s2), w=[tkres[t]])
                            sc.op("dve", lambda: nc.vector.tensor_scalar(out=selb[:, t, :], in0=s1, scalar1=m2[:, 7:8],
                                                                         scalar2=NEG, op0=ALU.is_lt, op1=ALU.mult),
                                  w=[tkres[t]])
                            pt, ptr = cx.psT[t % 2], cx.psTr[t % 2]
                            sc.op("pe", lambda: nc.tensor.transpose(out=pt[0:32, 0:128], in_=selb[:, t, :],
                                                                    identity=identb[:]),
                                  r=[tkres[t], cx.cres], w=[ptr])
                            sc.op("dve", lambda: nc.vector.tensor_copy(out=selbT[0:32, t * 128:(t + 1) * 128],
                                                                       in_=pt[0:32, 0:128]),
                                  r=[ptr], w=[selres[t]])
                    if stage(4 if i4 < 2 else 5):
                        return
                    for t in range(4):
                        i = 4 * i4 + t
                        ob = attn_tile(i, t, 1, kspad, ksres, 0, list(range(0, i + 1)), bm_sel, i >= 8)
                        combine(ob, i, 1, 65, False, False)
                        ob = attn_tile(i, t, 2, kwpad, kwres, 1, list(range(max(0, i - 4), i + 1)), bm_selwin, False)
                        combine(ob, i, 2, 65, False, True)
        sc.fence()
        with ExitStack() as es2:
            yT = uT
            yTres = uTres
            for i in range(NT):
                pt, ptr = cx.psT[i % 2], cx.psTr[i % 2]
                for c in range(8):
                    sc.op("pe", lambda: nc.tensor.transpose(out=pt[:, c * 128:(c + 1) * 128],
                                                            in_=y[:, i, c * 128:(c + 1) * 128], identity=identb[:]),
                          r=[yres[i], cx.cres], **({"w": [ptr]} if c == 0 else {"pw": [ptr]}))
                src = pt[:].rearrange("p (c s) -> p c s", c=8)
                dst = yT[:, :, i * 128:(i + 1) * 128]
                if i % 2 == 0:
                    sc.op("act", lambda: nc.scalar.copy(out=dst, in_=src), r=[ptr], w=[yTres[i]])
                else:
                    sc.op("dve", lambda: nc.vector.tensor_copy(out=dst, in_=src), r=[ptr], w=[yTres[i]])
            wo = es2.enter_context(nc.sbuf_tensor("wo_%d" % li, [128, 8, D_MODEL], BF16))
            wor = Res()
            wov = w_out.rearrange("(k p) c -> p k c", p=128)
            for q4 in range(2):
                sc.dma("pool", out=wo[:, q4 * 4:(q4 + 1) * 4, :], in_=wov[:, q4 * 4:(q4 + 1) * 4, :], pw=[wor])
            pgain = es2.enter_context(nc.sbuf_tensor("pog_%d" % li, [128, D_MODEL], F32))
            pgres = Res()
            sc.dma("sp", out=pgain[:], in_=bcast_row(P["post_norm_gain"][li], D_MODEL), w=[pgres])
            bufs = post_bufs(cx, es2, li)
            for i in range(NT):
                ops, opsr = cx.pbig[i % 3], cx.pbigr[i % 3]
                for half in range(2):
                    for kc in range(8):
                        sc.op("pe", lambda: nc.tensor.matmul(
                            ops[:, half * 512:(half + 1) * 512], lhsT=yT[:, kc, i * 128:(i + 1) * 128],
                            rhs=wo[:, kc, half * 512:(half + 1) * 512], start=(kc == 0), stop=(kc == 7)),
                            r=[yTres[i], wor],
                            **({"w": opsr} if (kc == 0 and half == 0) else {"pw": opsr}))
                emit_postnorm_residual(cx, es2, li, i, ops, opsr, pgain, pgres, h_in, hres_in, h_out, hres_out, bufs)
    sc.fence()


def _needed_params(layers):
    need = {"pre_norm_gain": None, "post_norm_gain": None}
    nsa = sorted({li // 2 for li in layers if li % 2 == 0})
    ret = sorted({li // 2 for li in layers if li % 2 == 1})
    if nsa:
        need["rel_bias_table"] = None
        for n in ("nsa_w_in", "nsa_w_out", "nsa_cmp_k_pos", "nsa_cmp_k_w1", "nsa_cmp_k_w2",
                  "nsa_cmp_v_pos", "nsa_cmp_v_w1", "nsa_cmp_v_w2"):
            need[n] = nsa
    if ret:
        for n in ("ret_w_in", "ret_w_out", "ret_gn_gain"):
            need[n] = ret
    return need


PARAM_SHAPES = {
    "pre_norm_gain": [4, 1024], "post_norm_gain": [4, 1024], "rel_bias_table": [32, 16],
    "nsa_w_in": [2, 1024, NSA_PROJ], "nsa_w_out": [2, 1024, 1024],
    "nsa_cmp_k_pos": [2, 32, 64], "nsa_cmp_k_w1": [2, 2048, 256], "nsa_cmp_k_w2": [2, 256, 64],
    "nsa_cmp_v_pos": [2, 32, 64], "nsa_cmp_v_w1": [2, 2048, 256], "nsa_cmp_v_w2": [2, 256, 64],
    "ret_w_in": [2, 1024, RET_PROJ], "ret_w_out": [2, 2048, 1024], "ret_gn_gain": [2, 2048],
}


def host_consts():
    rc = _ret_consts()
    c = {
        "c_cosT": rc["cosT"], "c_sinT": rc["sinT"],
        "c_innerT": rc["innerT"], "c_zeta": rc["zeta"], "c_epsp": rc["epsp"],
        "c_ident": np.eye(128, dtype=np.float32),
        "c_anti": np.ascontiguousarray(np.fliplr(np.eye(128, dtype=np.float32))),
    }
    c.update(_nsa_consts())
    return c, rc


def build_program(layers, first_from_x=True):
    consts, rc = host_consts()
    nc = bass.Bass("TRN2", target_bir_lowering=False, dynamic_dma_scratch_size=8192)
    P = {}
    x = nc.dram_tensor("x", [SEQ, D_MODEL], F32, kind="ExternalInput").ap()
    out = nc.dram_tensor("out", [SEQ, D_MODEL], F32, kind="ExternalOutput").ap()
    need = _needed_params(layers)
    slotmap = {}
    for name, shp in PARAM_SHAPES.items():
        if name not in need:
            continue
        shp = list(shp)
        if need[name] is not None:
            shp[0] = len(need[name])
            slotmap[name] = {s_: k_ for k_, s_ in enumerate(need[name])}
        P[name] = nc.dram_tensor(name, shp, F32, kind="ExternalInput").ap()
    has_nsa = any(li % 2 == 0 for li in layers)
    has_ret = any(li % 2 == 1 for li in layers)
    nsa_c = ("c_ohw", "c_ohc", "c_E", "c_ov", "c_forced")
    ret_c = ("c_cosT", "c_sinT")
    consts = {k: v for k, v in consts.items()
              if not ((k in nsa_c and not has_nsa) or (k in ret_c and not has_ret))}
    for name, arr in consts.items():
        P[name] = nc.dram_tensor(name, list(arr.shape), F32, kind="ExternalInput").ap()
    scratch = [nc.dram_tensor("hs%d" % i, [SEQ, D_MODEL], F32, kind="Internal").ap() for i in range(2)]
    Res.base = {}
    with ExitStack() as es:
        sc = Sched(nc, es)
        cx = Ctx()
        cx.nc, cx.sc, cx.P = nc, sc, P
        cx.dumps, cx.dump_res = [], []
        cx.pbig = [es.enter_context(nc.psum_tensor("pbig%d" % i, [128, 1024], F32)) for i in range(3)]
        cx.bres = [Res() for _ in range(6)]
        cx.pbigr = [[cx.bres[2 * i], cx.bres[2 * i + 1]] for i in range(3)]
        cx.bank = lambda k: cx.pbig[k // 2][:, (k % 2) * 512:(k % 2 + 1) * 512]
        cx.psT = [es.enter_context(nc.psum_tensor("psT%d" % i, [128, 1024], BF16)) for i in range(2)]
        cx.psTr = [Res(), Res()]
        cx.cres = Res()
        identf = es.enter_context(nc.sbuf_tensor("identf", [128, 128], F32))
        cx.identb = es.enter_context(nc.sbuf_tensor("identb", [128, 128], BF16))
        cx.eps_rms = es.enter_context(nc.sbuf_tensor("eps_rms", [128, 1], F32))
        sc.dma("sp", out=identf[:], in_=P["c_ident"][:, :], w=[cx.cres])
        sc.op("dve", lambda: nc.vector.tensor_copy(out=cx.identb[:], in_=identf[:]), r=[cx.cres], w=[cx.cres])
        sc.op("dve", lambda: nc.vector.memset(cx.eps_rms[:], RMS_EPS), pw=[cx.cres])
        antif = es.enter_context(nc.sbuf_tensor("antif", [128, 128], F32))
        cx.antib = es.enter_context(nc.sbuf_tensor("antib", [128, 128], BF16))
        ares = Res()
        sc.dma("sp", out=antif[:], in_=P["c_anti"][:, :], w=[ares])
        sc.op("dve", lambda: nc.vector.tensor_copy(out=cx.antib[:], in_=antif[:]), r=[ares], pw=[cx.cres])
        RC = {"gC": rc["gC"]}
        RC["innerT"] = es.enter_context(nc.sbuf_tensor("innerT", [128, 512], F32))
        RC["zeta"] = es.enter_context(nc.sbuf_tensor("zeta", [128, 4], F32))
        RC["epsp"] = es.enter_context(nc.sbuf_tensor("epsp", [128, 4], F32))
        sc.dma("sp", out=RC["innerT"][:], in_=P["c_innerT"][:, :], pw=[cx.cres])
        sc.dma("sp", out=RC["zeta"][:], in_=P["c_zeta"][:, :], pw=[cx.cres])
        sc.dma("sp", out=RC["epsp"][:], in_=P["c_epsp"][:, :], pw=[cx.cres])
        cx.RC = RC
        if any(li % 2 == 0 for li in layers):
            emit_nsa_prologue(cx, es)
        xres = [Res() for _ in range(NT)]
        ores = [Res() for _ in range(NT)]
        sres = [[Res() for _ in range(NT)] for _ in range(2)]
        cur, curres = x, xres
        for n, li in enumerate(layers):
            last = (n == len(layers) - 1)
            dst, dstres = (out, ores) if last else (scratch[n % 2], sres[n % 2])
            if li % 2 == 0:
                emit_nsa_layer(cx, li, slotmap["nsa_w_in"][li // 2], cur, curres, dst, dstres)
            else:
                emit_ret_layer(cx, li, slotmap["ret_w_in"][li // 2], cur, curres, dst, dstres)
            if STAGE < 99:
                sc.fence()
                sc._wait("sp", Res.base, skip_own=False)
                break
            cur, curres = dst, dstres
        sc.finish(ores + cx.dump_res)
        nc.all_engine_barrier()
        sc.clear_all()
    nc._dumps = cx.dumps
    return nc, consts


_PROG_CACHE = {}


def run_layers(layers, x, params):
    key = tuple(layers)
    if key not in _PROG_CACHE:
        _PROG_CACHE[key] = build_program(list(layers))
    nc, consts = _PROG_CACHE[key]
    B = x.shape[0]
    need = _needed_params(list(layers))
    shared = {}
    for name, sl in need.items():
        a = np.asarray(params[name], dtype=np.float32)
        shared[name] = np.ascontiguousarray(a if sl is None else a[sl])
    in_maps = []
    for b in range(B):
        m = {"x": np.ascontiguousarray(x[b])}
        m.update(shared)
        m.update(consts)
        in_maps.append(m)
    res = run_bass_kernel_spmd(nc, in_maps, core_ids=list(range(B)))
    global LAST_RESULTS
    LAST_RESULTS = res.results
    return np.stack([r["out"] for r in res.results], axis=0)


def kernel(x, **params):
    x = np.asarray(x, dtype=np.float32)
    h = x
    for li in range(DEPTH):
        h = run_layers([li], h, params)
    return h.astype(np.float32)
```

```python
from contextlib import ExitStack
import math
import numpy as np
import concourse.bass as bass
import concourse.mybir as mybir
from concourse.bass_utils import run_bass_kernel_spmd

F32 = mybir.dt.float32
BF16 = mybir.dt.bfloat16
AF = mybir.ActivationFunctionType
ALU = mybir.AluOpType

D_MODEL = 1024
SEQ = 2048
NT = SEQ // 128
DEPTH = 4
RMS_EPS = 1e-6
GN_EPS = 1e-6
NEG = -30000.0

NSA_PROJ = 5680
CMP_N = 127
RET_PROJ = 6144


class Res:
    __slots__ = ("w", "r", "name")
    base = {}

    def __init__(self, name=""):
        self.w = {}
        self.r = dict(Res.base)
        self.name = name


def _merge(d, src):
    for k, (sem, v) in src.items():
        cur = d.get(k)
        if cur is None or cur[1] < v:
            d[k] = (sem, v)


class Sched:
    def __init__(self, nc, es, n_dma=40):
        self.nc = nc
        self.eng = {"pe": nc.tensor, "act": nc.scalar, "dve": nc.vector,
                    "pool": nc.gpsimd, "sp": nc.sync}
        self.esem = {}
        self.cnt = {}
        self.seen = {}
        for k in self.eng:
            self.esem[k] = es.enter_context(nc.semaphore("e_" + k))
            self.cnt[k] = 0
            self.seen[k] = {}
        self.dsem = [es.enter_context(nc.semaphore("d%d" % i)) for i in range(n_dma)]
        self.dtot = [0] * n_dma
        self.dnext = 0
        self.dnext_sw = 0
        self.sw_hist = []
        self.n_sw = 12
        self.n_wait = 0
        self.n_inst = 0
        self.clear_all()

    def clear_all(self):
        for sem in list(self.esem.values()) + self.dsem:
            self.nc.gpsimd.sem_clear(sem)
        self.nc.all_engine_barrier()

    def _wait(self, E, deps, skip_own=True):
        own = id(self.esem[E]) if skip_own else None
        seen = self.seen[E]
        for k, (sem, v) in deps.items():
            if k == own:
                continue
            if seen.get(k, 0) >= v:
                continue
            self.eng[E].wait_ge(sem, v)
            seen[k] = v
            self.n_wait += 1

    def _deps(self, r, w, pw):
        deps = {}
        for x in r:
            _merge(deps, x.w)
        for x in w:
            _merge(deps, x.w)
            _merge(deps, x.r)
        for x in pw:
            _merge(deps, x.r)
        return deps

    def _commit(self, tok, r, w, pw):
        k = id(tok[0])
        for x in r:
            cur = x.r.get(k)
            if cur is None or cur[1] < tok[1]:
                x.r[k] = tok
        for x in w:
            x.w = {k: tok}
        for x in pw:
            x.w[k] = tok

    def op(self, E, fn, r=(), w=(), pw=()):
        self._wait(E, self._deps(r, w, pw), skip_own=(E == "pe"))
        inst = fn()
        self.cnt[E] += 1
        inst.then_inc(self.esem[E], 1)
        self._commit((self.esem[E], self.cnt[E]), r, w, pw)
        self.n_inst += 1
        return inst

    def dma(self, q, out, in_, r=(), w=(), pw=(), **kw):
        self._wait(q, self._deps(r, w, pw), skip_own=False)
        nsw = self.n_sw
        if q == "pool":
            i = self.dnext_sw
            self.dnext_sw = (self.dnext_sw + 1) % nsw
        else:
            i = nsw + self.dnext
            self.dnext = (self.dnext + 1) % (len(self.dsem) - nsw)
        sem = self.dsem[i]
        if self.dtot[i] > 0:
            self._wait(q, {id(sem): (sem, self.dtot[i])})
        if q == "pool":
            if len(self.sw_hist) >= 3:
                psem, pval = self.sw_hist[-3]
                self._wait(q, {id(psem): (psem, pval)})
        inst = self.eng[q].dma_start(out=out, in_=in_, **kw)
        self.dtot[i] += 16
        inst.then_inc(sem, 16)
        self._commit((sem, self.dtot[i]), r, w, pw)
        if q == "pool":
            self.sw_hist.append((sem, self.dtot[i]))
        self.n_inst += 1
        return inst

    def fence(self):
        base = {}
        for k, sem in self.esem.items():
            if self.cnt[k] > 0:
                base[id(sem)] = (sem, self.cnt[k])
        for sem, tot in zip(self.dsem, self.dtot):
            if tot > 0:
                base[id(sem)] = (sem, tot)
        Res.base = base

    def finish(self, resources):
        deps = {}
        for x in resources:
            _merge(deps, x.w)
        self._wait("sp", deps)


def _ret_consts():
    H, C = 4, 128
    log_g = np.log(1.0 - 2.0 ** (-5.0 - np.arange(H, dtype=np.float64)))
    i = np.arange(C, dtype=np.float64)
    innerT = np.exp(-(i[:, None, None] + 1.0) * log_g[None, :, None]) * \
        (i[None, None, :] >= i[:, None, None])
    zeta = np.exp((C - 1.0 - i)[:, None] * log_g[None, :])
    xi = np.exp((i + 1.0)[:, None] * log_g[None, :])
    epsp = GN_EPS / (xi * xi)
    gC = np.exp(C * log_g)
    inv = (1.0 / (np.float32(10000.0) ** np.linspace(0.0, 1.0, 128, dtype=np.float32))).astype(np.float32)
    ang = (np.arange(SEQ, dtype=np.float32)[:, None] * inv[None, :]).astype(np.float32)
    cosT = np.ascontiguousarray(np.cos(ang).T.astype(np.float32))
    sinT = np.ascontiguousarray(np.sin(ang).T.astype(np.float32))
    return dict(innerT=innerT.astype(np.float32).reshape(128, 4 * 128),
                zeta=zeta.astype(np.float32), epsp=epsp.astype(np.float32),
                gC=[float(x) for x in gC], cosT=cosT, sinT=sinT)


DEBUG = False
STAGE = 99
TOKSTOP = -1
NOEVAC = ()


class StopEmit(Exception):
    pass


def stage(n):
    return STAGE <= n


class Ctx:
    def dump(self, name, ap, res, dt):
        if not DEBUG:
            return
        d = self.nc.dram_tensor("dbg_" + name, list(ap.shape), dt, kind="ExternalOutput").ap()
        self.dumps.append("dbg_" + name)
        r = Res()
        if len(ap.shape) == 3:
            for j in range(ap.shape[1]):
                self.sc.dma("sp", out=d[:, j, :], in_=ap[:, j, :], r=list(res), pw=[r])
        else:
            self.sc.dma("sp", out=d, in_=ap, r=list(res), w=[r])
        self.dump_res.append(r)


def bcast_row(dram_ap_row, n):
    return bass.AP(dram_ap_row.tensor, dram_ap_row.offset, [[0, 128], [1, n]])


def emit_prenorm(cx, es, li, h_in, hres, pre_gain_row, uT, uTres):
    nc, sc = cx.nc, cx.sc
    gain = es.enter_context(nc.sbuf_tensor("pg%d" % li, [128, D_MODEL], F32))
    gres = Res()
    sc.dma("sp", out=gain[:], in_=bcast_row(pre_gain_row, D_MODEL), w=[gres])
    hb = [es.enter_context(nc.sbuf_tensor("hb%d_%d" % (li, k), [128, D_MODEL], F32)) for k in range(2)]
    hbr = [Res(), Res()]
    ub = [es.enter_context(nc.sbuf_tensor("ub%d_%d" % (li, k), [128, D_MODEL], BF16)) for k in range(2)]
    ubr = [Res(), Res()]
    junk = es.enter_context(nc.sbuf_tensor("junk%d" % li, [128, D_MODEL], BF16))
    junkr = Res()
    ss = es.enter_context(nc.sbuf_tensor("ss%d" % li, [128, NT], F32))
    rs = es.enter_context(nc.sbuf_tensor("rs%d" % li, [128, NT], F32))
    ssr = [Res() for _ in range(NT)]
    for i in range(NT):
        k = i % 2
        sc.dma("sp", out=hb[k][:], in_=h_in[i * 128:(i + 1) * 128, :], r=[hres[i]], w=[hbr[k]])
        sc.op("act", lambda: nc.scalar.activation(out=junk[:], in_=hb[k][:], func=AF.Square,
                                                  accum_out=ss[:, i:i + 1]),
              r=[hbr[k]], w=[junkr, ssr[i]])
        sc.op("act", lambda: nc.scalar.activation(out=rs[:, i:i + 1], in_=ss[:, i:i + 1], func=AF.Ln,
                                                  scale=1.0 / D_MODEL, bias=cx.eps_rms[:, 0:1]),
              r=[ssr[i], cx.cres], w=[ssr[i]])
        sc.op("act", lambda: nc.scalar.activation(out=rs[:, i:i + 1], in_=rs[:, i:i + 1], func=AF.Exp,
                                                  scale=-0.5), r=[ssr[i]], w=[ssr[i]])
        sc.op("dve", lambda: nc.vector.scalar_tensor_tensor(out=ub[k][:], in0=hb[k][:], scalar=rs[:, i:i + 1],
                                                            in1=gain[:], op0=ALU.mult, op1=ALU.mult),
              r=[hbr[k], ssr[i], gres], w=[ubr[k]])
        pt, ptr = cx.psT[i % 2], cx.psTr[i % 2]
        for c in range(8):
            sc.op("pe", lambda: nc.tensor.transpose(out=pt[:, c * 128:(c + 1) * 128],
                                                    in_=ub[k][:, c * 128:(c + 1) * 128], identity=cx.identb[:]),
                  r=[ubr[k], cx.cres], **({"w": [ptr]} if c == 0 else {"pw": [ptr]}))
        src = pt[:].rearrange("p (c s) -> p c s", c=8)
        dst = uT[:, :, i * 128:(i + 1) * 128]
        if i % 2 == 0:
            sc.op("act", lambda: nc.scalar.copy(out=dst, in_=src), r=[ptr], w=[uTres[i]])
        else:
            sc.op("dve", lambda: nc.vector.tensor_copy(out=dst, in_=src), r=[ptr], w=[uTres[i]])


def emit_postnorm_residual(cx, es, li, i, ops, opsr, post_gain, pgres, h_in, hres_in, h_out, hres_out, bufs):
    nc, sc = cx.nc, cx.sc
    k = i % 2
    hb, hbr, tb, tbr, junk, junkr, st, str_ = bufs
    sc.dma("sp", out=hb[k][:], in_=h_in[i * 128:(i + 1) * 128, :], r=[hres_in[i]], w=[hbr[k]])
    sc.op("act", lambda: nc.scalar.activation(out=junk[:], in_=ops[:], func=AF.Square,
                                              accum_out=st[:, 2 * i:2 * i + 1]),
          r=list(opsr), w=[junkr, str_[i]])
    sc.op("act", lambda: nc.scalar.activation(out=st[:, 2 * i + 1:2 * i + 2], in_=st[:, 2 * i:2 * i + 1], func=AF.Ln,
                                              scale=1.0 / D_MODEL, bias=cx.eps_rms[:, 0:1]),
          r=[str_[i], cx.cres], w=[str_[i]])
    sc.op("act", lambda: nc.scalar.activation(out=st[:, 2 * i + 1:2 * i + 2], in_=st[:, 2 * i + 1:2 * i + 2],
                                              func=AF.Exp, scale=-0.5), r=[str_[i]], w=[str_[i]])
    sc.op("dve", lambda: nc.vector.scalar_tensor_tensor(out=tb[k][:], in0=ops[:], scalar=st[:, 2 * i + 1:2 * i + 2],
                                                        in1=post_gain[:], op0=ALU.mult, op1=ALU.mult),
          r=list(opsr) + [str_[i], pgres], w=[tbr[k]])
    sc.op("pool", lambda: nc.gpsimd.tensor_tensor(out=tb[k][:], in0=tb[k][:], in1=hb[k][:], op=ALU.add),
          r=[hbr[k]], w=[tbr[k]])
    sc.dma("sp", out=h_out[i * 128:(i + 1) * 128, :], in_=tb[k][:], r=[tbr[k]], w=[hres_out[i]])


def post_bufs(cx, es, li):
    nc = cx.nc
    hb = [es.enter_context(nc.sbuf_tensor("phb%d_%d" % (li, k), [128, D_MODEL], F32)) for k in range(2)]
    tb = [es.enter_context(nc.sbuf_tensor("ptb%d_%d" % (li, k), [128, D_MODEL], F32)) for k in range(2)]
    junk = es.enter_context(nc.sbuf_tensor("pjunk%d" % li, [128, D_MODEL], BF16))
    st = es.enter_context(nc.sbuf_tensor("pst%d" % li, [128, 2 * NT], F32))
    return (hb, [Res(), Res()], tb, [Res(), Res()], junk, Res(), st, [Res() for _ in range(NT)])


def emit_ret_layer(cx, li, slot, h_in, hres_in, h_out, hres_out):
    nc, sc, P = cx.nc, cx.sc, cx.P
    w_in = P["ret_w_in"][slot]
    w_out = P["ret_w_out"][slot]
    gn = P["ret_gn_gain"][slot]
    RC = cx.RC
    with ExitStack() as es:
        def sb(name, shape, dt):
            return es.enter_context(nc.sbuf_tensor("%s_%d" % (name, li), shape, dt))
        uT = sb("uT", [128, 8, SEQ], BF16)
        uTres = [Res() for _ in range(NT)]
        yT = sb("yT", [128, 16, SEQ], BF16)
        yTres = [Res() for _ in range(NT)]
        with ExitStack() as es0:
            emit_prenorm(cx, es0, li, h_in, hres_in, P["pre_norm_gain"][li], uT, uTres)
        sc.fence()
        with ExitStack() as es1:
            def sb1(name, shape, dt):
                return es1.enter_context(nc.sbuf_tensor("%s_%d" % (name, li), shape, dt))
            cosb = [sb1("cosb0", [128, 512], F32)] * 2
            sinb = [sb1("sinb0", [128, 512], F32)] * 2
            csr = [Res()] * 2
            wq = sb1("wq", [128, 8, 256], BF16)
            wk = sb1("wk", [128, 8, 256], BF16)
            wv = sb1("wv", [128, 8, 512], BF16)
            wz = wv
            wres = {n: Res() for n in "qkv"}
            wres["z"] = wres["v"]
            qT = sb1("qT", [128, 2, SEQ], BF16)
            kT = sb1("kT", [128, 2, SEQ], BF16)
            qTr = [Res() for _ in range(4)]
            kTr = [Res() for _ in range(4)]
            ktok = sb1("ktok", [128, NT, 256], BF16)
            ktokr = [Res() for _ in range(NT)]
            vt = sb1("vt", [128, NT, 512], BF16)
            vtr = [Res() for _ in range(NT)]
            zs = sb1("zs", [128, NT, 512], BF16)
            zsr = [Res() for _ in range(NT)]
            gnb = sb1("gnb", [128, 512], F32)
            gnr = Res()
            xa = [sb1("xa0", [128, 512], F32)] * 2
            xb = [sb1("xb0", [128, 512], F32)] * 2
            xar = [Res()] * 2
            xbr = [Res()] * 2
            t1 = [sb1("t1_0", [128, 512], F32)] * 2
            t2 = [sb1("t2_0", [128, 512], F32)] * 2
            t3 = [sb1("t3_0", [128, 512], F32)] * 2
            t4 = [sb1("t4_0", [128, 512], F32)] * 2
            t12r = [Res()] * 2
            t34r = [Res()] * 2
            St = sb1("St", [128, 2, 512], F32)
            Sb = sb1("Sb", [128, 2, 512], BF16)
            Str = [Res(), Res()]
            Sbr = [Res(), Res()]
            at = [sb1("at%d" % k, [128, 128], BF16) for k in range(2)]
            atr = [Res(), Res()]
            on = [sb1("on0", [128, 512], F32)] * 2
            onr = [Res()] * 2
            yb = [sb1("yb0", [128, 512], BF16)] * 2
            ybr = [Res()] * 2
            stt = sb1("stt", [128, 2, 8], F32)
            mv = sb1("mv", [128, 2, 4], F32)
            sttr = [Res(), Res()]
            wviews = {
                "q": lambda hd: w_in[:, hd * 256:(hd + 1) * 256],
                "k": lambda hd: w_in[:, 1024 + hd * 256:1024 + (hd + 1) * 256],
                "v": lambda hd: w_in[:, 2048 + hd * 512:2048 + (hd + 1) * 512],
                "z": lambda hd: w_in[:, 4096 + hd * 512:4096 + (hd + 1) * 512],
            }
            wt = {"q": wq, "k": wk, "v": wv, "z": wz}
            bank = cx.bank
            bres = cx.bres
            nb = 0
            for hd in range(4):
                for n in "qkv":
                    sc.dma("pool", out=wt[n][:], in_=wviews[n](hd).rearrange("(k p) c -> p k c", p=128),
                           w=[wres[n]])
                sc.dma("sp", out=gnb[:], in_=bcast_row(gn[hd * 512:(hd + 1) * 512], 512), w=[gnr])
                for s4 in range(4):
                    cb = s4 % 2
                    sc.dma("sp", out=cosb[cb][:], in_=P["c_cosT"][:, s4 * 512:(s4 + 1) * 512], w=[csr[cb]])
                    sc.dma("sp", out=sinb[cb][:], in_=P["c_sinT"][:, s4 * 512:(s4 + 1) * 512], pw=[csr[cb]])
                    cres = csr[cb]
                    for n, dst, dres, scale in (("q", qT, qTr, 1.0), ("k", kT, kTr, 1.0 / 16.0)):
                        pa, pb = nb % 6, (nb + 1) % 6
                        nb += 2
                        for c, pbk in ((0, pa), (1, pb)):
                            for kc in range(8):
                                sc.op("pe", lambda: nc.tensor.matmul(
                                    bank(pbk), lhsT=wt[n][:, kc, c * 128:(c + 1) * 128],
                                    rhs=uT[:, kc, s4 * 512:(s4 + 1) * 512], start=(kc == 0), stop=(kc == 7)),
                                    r=[wres[n]] + uTres[s4 * 4:(s4 + 1) * 4],
                                    **({"w": [bres[pbk]]} if kc == 0 else {"pw": [bres[pbk]]}))
                        k2 = s4 % 2
                        sc.op("act", lambda: nc.scalar.activation(out=xa[k2][:], in_=bank(pa), func=AF.Copy,
                                                                  scale=scale), r=[bres[pa]], w=[xar[k2]])
                        sc.op("act", lambda: nc.scalar.activation(out=xb[k2][:], in_=bank(pb), func=AF.Copy,
                                                                  scale=scale), r=[bres[pb]], w=[xbr[k2]])
                        cs = cosb[cb][:]
                        sn = sinb[cb][:]
                        sc.op("dve", lambda: nc.vector.tensor_tensor(out=t1[k2][:], in0=xa[k2][:], in1=cs, op=ALU.mult),
                              r=[xar[k2], cres], w=[t12r[k2]])
                        sc.op("dve", lambda: nc.vector.tensor_tensor(out=t2[k2][:], in0=xb[k2][:], in1=sn, op=ALU.mult),
                              r=[xbr[k2], cres], pw=[t12r[k2]])
                        sc.op("dve", lambda: nc.vector.tensor_tensor(out=dst[:, 0, s4 * 512:(s4 + 1) * 512],
                                                                     in0=t1[k2][:], in1=t2[k2][:], op=ALU.subtract),
                              r=[t12r[k2]], w=[dres[s4]])
                        sc.op("pool", lambda: nc.gpsimd.tensor_tensor(out=t3[k2][:], in0=xa[k2][:], in1=sn, op=ALU.mult),
                              r=[xar[k2], cres], w=[t34r[k2]])
                        sc.op("pool", lambda: nc.gpsimd.tensor_tensor(out=t4[k2][:], in0=xb[k2][:], in1=cs, op=ALU.mult),
                              r=[xbr[k2], cres], pw=[t34r[k2]])
                        sc.op("pool", lambda: nc.gpsimd.tensor_tensor(out=dst[:, 1, s4 * 512:(s4 + 1) * 512],
                                                                      in0=t3[k2][:], in1=t4[k2][:], op=ALU.add),
                              r=[t34r[k2]], pw=[dres[s4]])
                for n in "vz":
                    if n == "z":
                        sc.dma("pool", out=wt["z"][:], in_=wviews["z"](hd).rearrange("(k p) c -> p k c", p=128),
                               w=[wres["z"]])
                    for i in range(NT):
                        pbk = nb % 6
                        nb += 1
                        for kc in range(8):
                            sc.op("pe", lambda: nc.tensor.matmul(
                                bank(pbk), lhsT=uT[:, kc, i * 128:(i + 1) * 128], rhs=wt[n][:, kc, :],
                                start=(kc == 0), stop=(kc == 7)),
                                r=[wres[n], uTres[i]],
                                **({"w": [bres[pbk]]} if kc == 0 else {"pw": [bres[pbk]]}))
                        if n == "v":
                            sc.op("dve", lambda: nc.vector.tensor_copy(out=vt[:, i, :], in_=bank(pbk)),
                                  r=[bres[pbk]], w=[vtr[i]])
                        else:
                            sc.op("act", lambda: nc.scalar.activation(out=zs[:, i, :], in_=bank(pbk), func=AF.Silu),
                                  r=[bres[pbk]], w=[zsr[i]])
                            sc.op("pool", lambda: nc.gpsimd.tensor_tensor(out=zs[:, i, :], in0=zs[:, i, :],
                                                                          in1=gnb[:], op=ALU.mult),
                                  r=[gnr], w=[zsr[i]])
                for i in range(NT):
                    pt, ptr = cx.psT[i % 2], cx.psTr[i % 2]
                    for c in range(2):
                        sc.op("pe", lambda: nc.tensor.transpose(out=pt[:, c * 128:(c + 1) * 128],
                                                                in_=kT[:, c, i * 128:(i + 1) * 128],
                                                                identity=cx.identb[:]),
                              r=[kTr[i // 4], cx.cres], **({"w": [ptr]} if c == 0 else {"pw": [ptr]}))
                    sc.op("dve", lambda: nc.vector.tensor_scalar(out=ktok[:, i, :], in0=pt[:, 0:256],
                                                                 scalar1=RC["zeta"][:, hd:hd + 1], scalar2=None,
                                                                 op0=ALU.mult),
                          r=[ptr, cx.cres], w=[ktokr[i]])
                for n in range(NT):
                    k2 = n % 2
                    ps_s, ps_o = (0, 1)[k2], (2, 3)[k2]
                    sl = slice(n * 128, (n + 1) * 128)
                    for c in range(2):
                        sc.op("pe", lambda: nc.tensor.matmul(bank(ps_s)[:, 0:128], lhsT=kT[:, c, sl], rhs=qT[:, c, sl],
                                                             start=(c == 0), stop=(c == 1)),
                              r=[kTr[n // 4], qTr[n // 4]],
                              **({"w": [bres[ps_s]]} if c == 0 else {"pw": [bres[ps_s]]}))
                    sc.op("dve", lambda: nc.vector.tensor_tensor(out=at[k2][:], in0=bank(ps_s)[:, 0:128],
                                                                 in1=RC["innerT"][:, hd * 128:(hd + 1) * 128],
                                                                 op=ALU.mult),
                          r=[bres[ps_s], cx.cres], w=[atr[k2]])
                    sc.op("pe", lambda: nc.tensor.matmul(bank(ps_o), lhsT=at[k2][:], rhs=vt[:, n, :],
                                                         start=True, stop=(n == 0)),
                          r=[atr[k2], vtr[n]], w=[bres[ps_o]])
                    if n > 0:
                        for c in range(2):
                            sc.op("pe", lambda: nc.tensor.matmul(bank(ps_o), lhsT=qT[:, c, sl], rhs=Sb[:, c, :],
                                                                 start=False, stop=(c == 1)),
                                  r=[qTr[n // 4], Sbr[c]], pw=[bres[ps_o]])
                    if n < NT - 1:
                        for c in range(2):
                            pd = 4 + c
                            sc.op("pe", lambda: nc.tensor.matmul(bank(pd), lhsT=ktok[:, n, c * 128:(c + 1) * 128],
                                                                 rhs=vt[:, n, :], start=True, stop=True),
                                  r=[ktokr[n], vtr[n]], w=[bres[pd]])
                            if n == 0:
                                sc.op("dve", lambda: nc.vector.tensor_copy(out=St[:, c, :], in_=bank(pd)),
                                      r=[bres[pd]], w=[Str[c]])
                            else:
                                sc.op("dve", lambda: nc.vector.scalar_tensor_tensor(
                                    out=St[:, c, :], in0=St[:, c, :], scalar=RC["gC"][hd], in1=bank(pd),
                                    op0=ALU.mult, op1=ALU.add), r=[bres[pd]], w=[Str[c]])
                            sc.op("act", lambda: nc.scalar.copy(out=Sb[:, c, :], in_=St[:, c, :]),
                                  r=[Str[c]], w=[Sbr[c]])
                    sc.op("dve", lambda: nc.vector.bn_stats(out=stt[:, k2, 0:6], in_=bank(ps_o)),
                          r=[bres[ps_o]], w=[sttr[k2]])
                    sc.op("dve", lambda: nc.vector.bn_aggr(out=mv[:, k2, 0:2], in_=stt[:, k2, 0:6]),
                          r=[], w=[sttr[k2]])
                    sc.op("act", lambda: nc.scalar.activation(out=mv[:, k2, 2:3], in_=mv[:, k2, 1:2], func=AF.Ln,
                                                              bias=RC["epsp"][:, hd:hd + 1]),
                          r=[cx.cres], w=[sttr[k2]])
                    sc.op("act", lambda: nc.scalar.activation(out=mv[:, k2, 2:3], in_=mv[:, k2, 2:3], func=AF.Exp,
                                                              scale=-0.5), w=[sttr[k2]])
                    sc.op("dve", lambda: nc.vector.scalar_tensor_tensor(
                        out=mv[:, k2, 3:4], in0=mv[:, k2, 0:1], scalar=-1.0, in1=mv[:, k2, 2:3],
                        op0=ALU.mult, op1=ALU.mult), w=[sttr[k2]])
                    sc.op("act", lambda: nc.scalar.activation(out=on[k2][:], in_=bank(ps_o), func=AF.Identity,
                                                              scale=mv[:, k2, 2:3], bias=mv[:, k2, 3:4]),
                          r=[bres[ps_o], sttr[k2]], w=[onr[k2]])
                    sc.op("pool", lambda: nc.gpsimd.tensor_tensor(out=yb[k2][:], in0=on[k2][:], in1=zs[:, n, :],
                                                                  op=ALU.mult),
                          r=[onr[k2], zsr[n]], w=[ybr[k2]])
                    pt, ptr = cx.psT[k2], cx.psTr[k2]
                    for c in range(4):
                        sc.op("pe", lambda: nc.tensor.transpose(out=pt[:, c * 128:(c + 1) * 128],
                                                                in_=yb[k2][:, c * 128:(c + 1) * 128],
                                                                identity=cx.identb[:]),
                              r=[ybr[k2], cx.cres], **({"w": [ptr]} if c == 0 else {"pw": [ptr]}))
                    sc.op("act", lambda: nc.scalar.copy(out=yT[:, hd * 4:(hd + 1) * 4, sl],
                                                        in_=pt[:, 0:512].rearrange("p (c s) -> p c s", c=4)),
                          r=[ptr], **({"w": [yTres[n]]} if hd == 0 else {"pw": [yTres[n]]}))
                if hd == 3:
                    cx.dump("qT", qT[:], qTr, BF16)
                    cx.dump("kT", kT[:], kTr, BF16)
                    cx.dump("vt", vt[:], vtr, BF16)
                    cx.dump("zs", zs[:], zsr, BF16)
                    cx.dump("ktok", ktok[:], ktokr, BF16)
                    cx.dump("St", St[:], Str, F32)
            cx.dump("uT", uT[:], uTres, BF16)
            cx.dump("yT", yT[:], yTres, BF16)
        sc.fence()
        with ExitStack() as es2:
            wo = es2.enter_context(nc.sbuf_tensor("wo_%d" % li, [128, 16, D_MODEL], BF16))
            wor = Res()
            wov = w_out.rearrange("(k p) c -> p k c", p=128)
            for q4 in range(4):
                sc.dma("pool", out=wo[:, q4 * 4:(q4 + 1) * 4, :], in_=wov[:, q4 * 4:(q4 + 1) * 4, :],
                       **({"w": [wor]} if q4 == 0 else {"pw": [wor]}))
            pgain = es2.enter_context(nc.sbuf_tensor("pog_%d" % li, [128, D_MODEL], F32))
            pgres = Res()
            sc.dma("sp", out=pgain[:], in_=bcast_row(P["post_norm_gain"][li], D_MODEL), w=[pgres])
            bufs = post_bufs(cx, es2, li)
            for i in range(NT):
                ops, opsr = cx.pbig[i % 3], cx.pbigr[i % 3]
                for half in range(2):
                    for kc in range(16):
                        sc.op("pe", lambda: nc.tensor.matmul(
                            ops[:, half * 512:(half + 1) * 512], lhsT=yT[:, kc, i * 128:(i + 1) * 128],
                            rhs=wo[:, kc, half * 512:(half + 1) * 512], start=(kc == 0), stop=(kc == 15)),
                            r=[yTres[i], wor],
                            **({"w": opsr} if (kc == 0 and half == 0) else {"pw": opsr}))
                emit_postnorm_residual(cx, es2, li, i, ops, opsr, pgain, pgres, h_in, hres_in, h_out, hres_out, bufs)
    sc.fence()


def _t5_bucket(n):
    n = np.maximum(n, 0)
    nf = np.maximum(n, 1).astype(np.float32)
    large = 16 + (np.log(nf / np.float32(16)) / np.float32(math.log(128 / 16)) * np.float32(16)).astype(np.int32)
    large = np.minimum(large, 31)
    return np.where(n < 16, n, large)


def _nsa_consts():
    def onehot(n, valid):
        b = _t5_bucket(n)
        oh = np.zeros((33, n.shape[0]), np.float32)
        idx = np.arange(n.shape[0])
        oh[b[valid], idx[valid]] = 1.0
        oh[31, idx[valid]] -= 1.0
        oh[32, idx[~valid]] = 1.0
        return oh
    nw = np.arange(768) - 127
    ohw = onehot(nw, (nw >= 0) & (nw < 512))
    ncm = np.arange(4096) - 2047
    ohc = onehot(ncm, ncm >= 0)
    E = np.zeros((128, SEQ), np.float32)
    E[np.arange(SEQ) // 64, np.arange(SEQ)] = 1.0
    cs = np.arange(CMP_N) * 16
    j = np.arange(32)
    ov = ((cs[:, None] < (j[None, :] + 1) * 64) & (cs[:, None] + 32 > j[None, :] * 64)).astype(np.float32)
    t = np.arange(SEQ)
    cur = t // 64
    valid = j[None, :] * 64 <= t[:, None]
    forced = (j[None, :] == 0) | (j[None, :] == cur[:, None]) | (j[None, :] == cur[:, None] - 1)
    fm = np.where(valid, np.where(forced, 1000.0, 0.0), -1e30).astype(np.float32)
    fm = fm[1024:].reshape(8, 128, 32).transpose(1, 0, 2).reshape(128, 256)
    return {"c_ohw": ohw, "c_ohc": ohc, "c_E": E, "c_ov": np.ascontiguousarray(ov),
            "c_forced": np.ascontiguousarray(fm)}


def emit_nsa_prologue(cx, es):
    nc, sc, P = cx.nc, cx.sc, cx.P
    fw_d = nc.dram_tensor("fw_d", [16, 768], BF16, kind="Internal")
    fc_d = nc.dram_tensor("fc_d", [16, 4096], BF16, kind="Internal")
    cx.fc_d = fc_d
    cx.BM = es.enter_context(nc.sbuf_tensor("BM", [128, 16, 2, 128], BF16))
    cx.BM4 = es.enter_context(nc.sbuf_tensor("BM4", [128, 128], BF16))
    cx.Epad = es.enter_context(nc.sbuf_tensor("Epad", [128, SEQ], BF16))
    cx.bmres = Res()
    cx.fcres = Res()
    sc.dma("pool", out=cx.Epad[:], in_=P["c_E"][:, :], pw=[cx.bmres])
    with ExitStack() as ep:
        tab = ep.enter_context(nc.sbuf_tensor("tab", [33, 16], F32))
        ohw = ep.enter_context(nc.sbuf_tensor("ohw", [33, 768], F32))
        ohc = ep.enter_context(nc.sbuf_tensor("ohc", [33, 4096], F32))
        fwb = ep.enter_context(nc.sbuf_tensor("fwb", [16, 768], BF16))
        fcb = ep.enter_context(nc.sbuf_tensor("fcb", [16, 4096], BF16))
        tr, fr = Res(), Res()
        sc.op("dve", lambda: nc.vector.memset(tab[32:33, :], NEG), pw=[tr])
        sc.dma("sp", out=tab[0:32, :], in_=P["rel_bias_table"][:, :], pw=[tr])
        sc.dma("sp", out=ohw[:], in_=P["c_ohw"][:, :], pw=[tr])
        for q in range(4):
            sc.dma("sp", out=ohc[:, q * 1024:(q + 1) * 1024], in_=P["c_ohc"][:, q * 1024:(q + 1) * 1024], pw=[tr])
        bank, bres = cx.bank, cx.bres
        for q, (c0, c1) in enumerate(((0, 512), (512, 768))):
            sc.op("pe", lambda: nc.tensor.matmul(bank(q)[0:16, 0:c1 - c0], lhsT=tab[:, :], rhs=ohw[:, c0:c1],
                                                 start=True, stop=True), r=[tr], w=[bres[q]])
            sc.op("dve", lambda: nc.vector.tensor_copy(out=fwb[:, c0:c1], in_=bank(q)[0:16, 0:c1 - c0]),
                  r=[bres[q]], pw=[fr])
        for q in range(8):
            b = 2 + q % 4
            sc.op("pe", lambda: nc.tensor.matmul(bank(b)[0:16, :], lhsT=tab[:, :], rhs=ohc[:, q * 512:(q + 1) * 512],
                                                 start=True, stop=True), r=[tr], w=[bres[b]])
            sc.op("dve", lambda: nc.vector.tensor_copy(out=fcb[:, q * 512:(q + 1) * 512], in_=bank(b)[0:16, :]),
                  r=[bres[b]], pw=[fr])
        dres = Res()
        sc.dma("sp", out=fw_d.ap(), in_=fwb[:], r=[fr], w=[dres])
        sc.dma("sp", out=fc_d.ap(), in_=fcb[:], r=[fr], w=[cx.fcres])
        for h0 in range(0, 16, 4):
            src = bass.AP(fw_d, h0 * 768, [[1, 128], [768, 4], [128, 2], [1, 128]])
            sc.dma("sp", out=cx.BM[:, h0:h0 + 4, :, :], in_=src, r=[dres], pw=[cx.bmres])
        sc.dma("sp", out=cx.BM4[:], in_=bass.AP(fw_d, 512, [[1, 128], [1, 128]]), r=[dres], pw=[cx.bmres])
        sc.finish([cx.bmres, cx.fcres, dres])
    sc.fence()


def emit_nsa_layer(cx, li, slot, h_in, hres_in, h_out, hres_out):
    nc, sc, P = cx.nc, cx.sc, cx.P
    w_in = P["nsa_w_in"][slot]
    w_out = P["nsa_w_out"][slot]
    bank, bres = cx.bank, cx.bres
    identb = cx.identb
    with ExitStack() as es:
        def sbL(name, shape, dt):
            return es.enter_context(nc.sbuf_tensor("%s_%d" % (name, li), shape, dt))
        uT = sbL("uT", [128, 8, SEQ], BF16)
        uTres = [Res() for _ in range(NT)]
        y = sbL("y", [128, NT, D_MODEL], BF16)
        yres = [Res() for _ in range(NT)]
        with ExitStack() as es0:
            emit_prenorm(cx, es0, li, h_in, hres_in, P["pre_norm_gain"][li], uT, uTres)
        sc.fence()
        if stage(0):
            return
        with ExitStack() as es1:
            def sb(name, shape, dt):
                return es1.enter_context(nc.sbuf_tensor("%s_%d" % (name, li), shape, dt))
            wview = w_in.rearrange("(k p) c -> p k c", p=128)
            W1p = {n: sb("W1p" + n, [128, 16, 256], BF16) for n in "kv"}
            w2k = sb("w2k", [128, 2, 128], BF16)
            w2v = sb("w2v", [128, 2, 64], BF16)
            pos2 = {n: sb("pos2" + n, [128, 16], BF16) for n in "kv"}
            pb = {n: sb("pb" + n, [128, 2], F32) for n in "kv"}
            wg = sb("wg", [128, 8, 48], BF16)
            forced = sb("forced", [128, 8, 32], F32)
            lres = Res()
            for n in "kv":
                sc.dma("pool", out=W1p[n][:], in_=P["nsa_cmp_%s_w1" % n][slot].rearrange("(lp p) c -> p lp c", p=128),
                       pw=[lres])
                pos = P["nsa_cmp_%s_pos" % n][slot]
                for par in range(2):
                    src = bass.AP(pos.tensor, pos.offset + par * 64, [[1, 64], [128, 16]])
                    sc.dma("pool", out=pos2[n][par * 64:(par + 1) * 64, :], in_=src, pw=[lres],
                           allow_slow_non_contiguous=True)
            w2kv = P["nsa_cmp_k_w2"][slot].rearrange("(c p) d -> p c d", p=128)
            sc.dma("pool", out=w2k[:, :, 0:64], in_=w2kv, pw=[lres])
            sc.dma("pool", out=w2k[:, :, 64:128], in_=w2kv, pw=[lres])
            sc.dma("pool", out=w2v[:], in_=P["nsa_cmp_v_w2"][slot].rearrange("(c p) d -> p c d", p=128), pw=[lres])
            sc.dma("pool", out=wg[:], in_=wview[:, :, 2560:2608], pw=[lres])
            sc.dma("sp", out=forced[:], in_=P["c_forced"][:, :].rearrange("p (a b) -> p a b", a=8), pw=[lres])
            wq = sb("wq", [128, 8, 256], BF16)
            wks2 = sb("wks2", [128, 8, 128], BF16)
            wkw2 = sb("wkw2", [128, 8, 128], BF16)
            wkc2 = sb("wkc2", [128, 8, 128], BF16)
            wvc2 = sb("wvc2", [128, 8, 128], BF16)
            wtok = sb("wtok", [128, 8, 896], BF16)
            wres = Res()
            qT2 = sb("qT2", [128, 2, SEQ], BF16)
            qres = [Res() for _ in range(4)]
            kspad = sb("kspad", [128, 2, SEQ], BF16)
            kwpad = sb("kwpad", [128, 2, SEQ], BF16)
            ksres = [Res() for _ in range(4)]
            kwres = [Res() for _ in range(4)]
            kc2 = sb("kc2", [128, SEQ], BF16)
            vc2 = sb("vc2", [128, SEQ], BF16)
            kc2res, vc2res = Res(), Res()
            vsw = sb("vsw", [128, NT, 2, 80], BF16)
            vres = [Res() for _ in range(NT)]
            zs = sb("zs", [128, NT, 3, 256], BF16)
            zres = [Res() for _ in range(NT)]
            graw = sb("graw", [128, NT, 48], F32)
            gate = sb("gate", [128, NT, 48], F32)
            gres = Res()
            hT = {n: sb("hT" + n, [128, 2, 128], BF16) for n in "kv"}
            hres = {n: Res() for n in "kv"}
            kcpad = sb("kcpad", [128, 2, 128], BF16)
            kcres = Res()
            vcaug = sb("vcaug", [128, 98], BF16)
            vcres = Res()
            ovf = sb("ovf", [128, 32], F32)
            Pc = [sb("Pc%d" % k, [128, 512], BF16) for k in range(4)]
            Pcres = [Res() for _ in range(4)]
            cbm = [sb("cbm%d" % k, [128, 512], BF16) for k in range(2)]
            cbmres = [Res(), Res()]
            pbuf = [sb("pbuf%d" % k, [128, 512], BF16) for k in range(3)]
            pbres = [Res() for _ in range(3)]
            selbT = sb("selbT", [128, 512], BF16)
            selres = [Res() for _ in range(4)]
            imp = sb("imp", [128, 4, 32], F32)
            impres = [Res() for _ in range(4)]
            tk = sb("tk", [128, 4, 32 + 32 + 8 + 8 + 32], F32)
            selb = sb("selb", [128, 4, 32], BF16)
            tkres = [Res() for _ in range(4)]
            pp = sb("pp", [128, 2, 16], F32)
            ppres = [Res(), Res()]
            tmp3 = sb("tmp3", [128, 4, 32], F32)
            tmp3res = Res()
            yacc = [sb("yacc%d" % k, [128, 256], F32) for k in range(4)]
            yaccres = [Res() for _ in range(4)]
            ytmp = [sb("ytmp%d" % k, [128, 256], F32) for k in range(2)]
            ytmpres = [Res(), Res()]
            ires = Res()
            sc.op("pool", lambda: nc.gpsimd.memset(kspad[64:128, 0, :], 0.0), pw=ksres)
            sc.op("pool", lambda: nc.gpsimd.memset(kspad[0:64, 1, :], 0.0), pw=ksres)
            sc.op("pool", lambda: nc.gpsimd.memset(kwpad[64:128, 0, :], 0.0), pw=kwres)
            sc.op("pool", lambda: nc.gpsimd.memset(kwpad[0:64, 1, :], 0.0), pw=kwres)
            sc.op("pool", lambda: nc.gpsimd.memset(kcpad[64:128, 0, :], 0.0), pw=[kcres])
            sc.op("pool", lambda: nc.gpsimd.memset(kcpad[0:64, 1, :], 0.0), pw=[kcres])
            sc.op("pool", lambda: nc.gpsimd.memset(kc2[64:128, SEQ - 1:SEQ], 0.0), pw=[kc2res])
            sc.op("pool", lambda: nc.gpsimd.memset(vc2[64:128, SEQ - 1:SEQ], 0.0), pw=[vc2res])
            sc.op("pool", lambda: nc.gpsimd.memset(vsw[:, :, :, 64:65], 1.0), pw=vres)
            sc.op("pool", lambda: nc.gpsimd.memset(vcaug[:, 64:66], 1.0), pw=[vcres])
            sc.op("pool", lambda: nc.gpsimd.memset(selbT[:], 0.0), pw=selres)
            sc.dma("sp", out=ovf[0:CMP_N, :], in_=P["c_ov"][:, :], w=[ires])
            sc.op("dve", lambda: nc.vector.tensor_copy(out=vcaug[0:CMP_N, 66:98], in_=ovf[0:CMP_N, :]),
                  r=[ires], pw=[vcres])

            def load_group_weights(g):
                sc.dma("pool", out=wq[:], in_=wview[:, :, 256 * g:256 * (g + 1)], w=[wres])
                for t, dst in ((0, wkc2), (1, wvc2), (2, wks2), (4, wkw2)):
                    c0 = 1024 + 256 * t + 64 * g
                    for half in range(2):
                        sc.dma("pool", out=dst[:, :, half * 64:(half + 1) * 64], in_=wview[:, :, c0:c0 + 64], pw=[wres])
                for t, c in ((3, 0), (5, 64)):
                    c0 = 1024 + 256 * t + 64 * g
                    sc.dma("pool", out=wtok[:, :, c:c + 64], in_=wview[:, :, c0:c0 + 64], pw=[wres])
                for br in range(3):
                    c0 = 2608 + 1024 * br + 256 * g
                    sc.dma("pool", out=wtok[:, :, 128 + 256 * br:128 + 256 * (br + 1)], in_=wview[:, :, c0:c0 + 256],
                           pw=[wres])

            nbk = [0]

            def nextbank():
                b = nbk[0] % 6
                nbk[0] += 1
                return b

            def proj_fm(wt, ncols, s4, b):
                for kc in range(8):
                    sc.op("pe", lambda: nc.tensor.matmul(bank(b), lhsT=wt[:, kc, ncols:ncols + 128],
                                                         rhs=uT[:, kc, s4 * 512:(s4 + 1) * 512],
                                                         start=(kc == 0), stop=(kc == 7)),
                          r=[wres, lres] + uTres[s4 * 4:(s4 + 1) * 4],
                          **({"w": [bres[b]]} if kc == 0 else {"pw": [bres[b]]}))

            njob = [0]
            nacc = [0]
            load_group_weights(0)
            for n in "kv":
                for hc in range(2):
                    b = nextbank()
                    for lp in range(16):
                        sc.op("pe", lambda: nc.tensor.matmul(bank(b)[:, 0:1], lhsT=W1p[n][:, lp, hc * 128:(hc + 1) * 128],
                                                             rhs=pos2[n][:, lp:lp + 1], start=(lp == 0), stop=(lp == 15)),
                              r=[lres], **({"w": [bres[b]]} if lp == 0 else {"pw": [bres[b]]}))
                    sc.op("dve", lambda: nc.vector.tensor_copy(out=pb[n][:, hc:hc + 1], in_=bank(b)[:, 0:1]),
                          r=[bres[b]], pw=[lres])

            if stage(1):
                return
            for g in range(4):
                for s4 in range(4):
                    sl = slice(s4 * 512, (s4 + 1) * 512)
                    for m in range(2):
                        b = nextbank()
                        proj_fm(wq, m * 128, s4, b)
                        sc.op("act", lambda: nc.scalar.activation(out=qT2[:, m, sl], in_=bank(b), func=AF.Copy,
                                                                  scale=0.125),
                              r=[bres[b]], **({"w": [qres[s4]]} if m == 0 else {"pw": [qres[s4]]}))
                    if stage(1.1):
                        return
                    for wt, dst, dres in ((wks2, kspad, ksres), (wkw2, kwpad, kwres)):
                        b = nextbank()
                        proj_fm(wt, 0, s4, b)
                        sc.op("dve", lambda: nc.vector.tensor_copy(out=dst[0:64, 0, sl], in_=bank(b)[0:64, :]),
                              r=[bres[b]], w=[dres[s4]])
                        sc.op("act", lambda: nc.scalar.copy(out=dst[64:128, 1, sl], in_=bank(b)[64:128, :]),
                              r=[bres[b]], pw=[dres[s4]])
                    if stage(1.2):
                        return
                    for wt, dst, dres in ((wkc2, kc2, kc2res), (wvc2, vc2, vc2res)):
                        b = nextbank()
                        proj_fm(wt, 0, s4, b)
                        sc.op("dve", lambda: nc.vector.tensor_copy(out=dst[0:64, sl], in_=bank(b)[0:64, :]),
                              r=[bres[b]], **({"w": [dres]} if s4 == 0 else {"pw": [dres]}))
                        if s4 == 0:
                            sc.op("act", lambda: nc.scalar.copy(out=dst[64:128, 0:511], in_=bank(b)[64:128, 1:512]),
                                  r=[bres[b]], pw=[dres])
                        else:
                            sc.op("act", lambda: nc.scalar.copy(out=dst[64:128, s4 * 512 - 1:(s4 + 1) * 512 - 1],
                                                                in_=bank(b)[64:128, :]),
                                  r=[bres[b]], pw=[dres])
                if stage(1.4):
                    return
                for i in range(NT):
                    if i == 1 and stage(1.5):
                        return
                    if i == TOKSTOP:
                        return
                    ba, bb = nextbank(), nextbank()
                    bg = nextbank() if g == 0 else None
                    for kc in range(8):
                        lhs = uT[:, kc, i * 128:(i + 1) * 128]
                        fl = dict(start=(kc == 0), stop=(kc == 7))
                        wk = "w" if kc == 0 else "pw"
                        sc.op("pe", lambda: nc.tensor.matmul(bank(ba)[:, 0:384], lhsT=lhs, rhs=wtok[:, kc, 0:384], **fl),
                              r=[wres, uTres[i]], **{wk: [bres[ba]]})
                    for kc in range(8):
                        lhs = uT[:, kc, i * 128:(i + 1) * 128]
                        fl = dict(start=(kc == 0), stop=(kc == 7))
                        wk = "w" if kc == 0 else "pw"
                        sc.op("pe", lambda: nc.tensor.matmul(bank(bb), lhsT=lhs, rhs=wtok[:, kc, 384:896], **fl),
                              r=[wres, uTres[i]], **{wk: [bres[bb]]})
                    if g == 0:
                        for kc in range(8):
                            lhs = uT[:, kc, i * 128:(i + 1) * 128]
                            fl = dict(start=(kc == 0), stop=(kc == 7))
                            wk = "w" if kc == 0 else "pw"
                            sc.op("pe", lambda: nc.tensor.matmul(bank(bg)[:, 0:48], lhsT=lhs, rhs=wg[:, kc, :], **fl),
                                  r=[lres, uTres[i]], **{wk: [bres[bg]]})
                    if i == 0 and stage(1.45):
                        return
                    sc.op("dve", lambda: nc.vector.tensor_copy(
                        out=vsw[:, i, :, 0:64], in_=bank(ba)[:, 0:128].rearrange("p (a d) -> p a d", a=2)),
                        r=[bres[ba]], w=[vres[i]])
                    sc.op("act", lambda: nc.scalar.activation(out=zs[:, i, 0, :], in_=bank(ba)[:, 128:384], func=AF.Silu),
                          r=[bres[ba], vres[i]], w=[zres[i]])
                    sc.op("act", lambda: nc.scalar.activation(out=zs[:, i, 1:3, :],
                                                              in_=bank(bb).rearrange("p (a d) -> p a d", a=2),
                                                              func=AF.Silu),
                          r=[bres[bb]], pw=[zres[i]])
                    if g == 0:
                        sc.op("dve", lambda: nc.vector.tensor_copy(out=graw[:, i, :], in_=bank(bg)[:, 0:48]),
                              r=[bres[bg]], pw=[gres])
                if stage(2):
                    return
                for n, src2, sres2 in (("k", kc2, kc2res), ("v", vc2, vc2res)):
                    for hc in range(2):
                        b = nextbank()
                        for lp in range(16):
                            sc.op("pe", lambda: nc.tensor.matmul(
                                bank(b)[:, 0:CMP_N], lhsT=W1p[n][:, lp, hc * 128:(hc + 1) * 128],
                                rhs=src2[:, 2 * lp:2 * lp + 16 * (CMP_N - 1) + 1:16],
                                start=(lp == 0), stop=(lp == 15)),
                                r=[lres, sres2], **({"w": [bres[b]]} if lp == 0 else {"pw": [bres[b]]}))
                        sc.op("act", lambda: nc.scalar.activation(out=hT[n][:, hc, 0:CMP_N], in_=bank(b)[:, 0:CMP_N],
                                                                  func=AF.Silu, bias=pb[n][:, hc:hc + 1]),
                              r=[bres[b], lres], **({"w": [hres[n]]} if hc == 0 else {"pw": [hres[n]]}))
                b = nextbank()
                for hc in range(2):
                    sc.op("pe", lambda: nc.tensor.matmul(bank(b)[:, 0:CMP_N], lhsT=w2k[:, hc, :], rhs=hT["k"][:, hc, 0:CMP_N],
                                                         start=(hc == 0), stop=(hc == 1)),
                          r=[lres, hres["k"]], **({"w": [bres[b]]} if hc == 0 else {"pw": [bres[b]]}))
                sc.op("dve", lambda: nc.vector.tensor_copy(out=kcpad[0:64, 0, 0:CMP_N], in_=bank(b)[0:64, 0:CMP_N]),
                      r=[bres[b]], w=[kcres])
                sc.op("dve", lambda: nc.vector.tensor_copy(out=kcpad[64:128, 1, 0:CMP_N], in_=bank(b)[64:128, 0:CMP_N]),
                      r=[bres[b]], pw=[kcres])
                b = nextbank()
                for hc in range(2):
                    sc.op("pe", lambda: nc.tensor.matmul(bank(b)[0:CMP_N, 0:64], lhsT=hT["v"][:, hc, 0:CMP_N], rhs=w2v[:, hc, :],
                                                         start=(hc == 0), stop=(hc == 1)),
                          r=[lres, hres["v"]], **({"w": [bres[b]]} if hc == 0 else {"pw": [bres[b]]}))
                sc.op("dve", lambda: nc.vector.tensor_copy(out=vcaug[0:CMP_N, 0:64], in_=bank(b)[0:CMP_N, 0:64]),
                      r=[bres[b]], w=[vcres])
                if g < 3:
                    load_group_weights(g + 1)
                if g == 0:
                    sc.op("act", lambda: nc.scalar.activation(out=gate[:], in_=graw[:], func=AF.Exp, scale=-1.0),
                          r=[gres], w=[gres])
                    sc.op("dve", lambda: nc.vector.tensor_scalar(out=gate[:], in0=gate[:], scalar1=1.0, scalar2=None,
                                                                 op0=ALU.add), w=[gres])
                    sc.op("dve", lambda: nc.vector.reciprocal(out=gate[:], in_=gate[:]), w=[gres])

                if stage(3):
                    return
                def attn_tile(i, t, br, kpad, kres_, vidx, kts, bmfn, use_sel):
                    ob = 3 + nacc[0] % 2
                    nacc[0] += 1
                    jobs = []
                    for r in range(4):
                        for a in range(0, len(kts), 4):
                            jobs.append((r, kts[a:a + 4]))
                    qsl = slice(i * 128, (i + 1) * 128)

                    def qk(job, jn):
                        r, ks_ = job
                        sb_ = jn % 3
                        first = True
                        for a, kt in enumerate(ks_):
                            extra = []
                            if use_sel:
                                extra.append((cx.Epad[:, kt * 128:(kt + 1) * 128], selbT[:, t * 128:(t + 1) * 128],
                                              [cx.bmres, selres[t]]))
                            bm = bmfn(4 * g + r, i - kt)
                            if bm is not None:
                                extra.append((cx.antib[:], bm, [cx.bmres, cx.cres]))
                            out = bank(sb_)[:, a * 128:(a + 1) * 128]
                            sc.op("pe", lambda: nc.tensor.matmul(out, lhsT=kpad[:, r % 2, kt * 128:(kt + 1) * 128],
                                                                 rhs=qT2[:, r // 2, qsl], start=True,
                                                                 stop=(len(extra) == 0)),
                                  r=[kres_[kt // 4], qres[i // 4]],
                                  **({"w": [bres[sb_]]} if first else {"pw": [bres[sb_]]}))
                            first = False
                            for e, (l_, r_, rr_) in enumerate(extra):
                                sc.op("pe", lambda: nc.tensor.matmul(out, lhsT=l_, rhs=r_, start=False,
                                                                     stop=(e == len(extra) - 1)),
                                      r=rr_, pw=[bres[sb_]])

                    def expv(job, jn):
                        r, ks_ = job
                        sb_ = jn % 3
                        w_ = len(ks_) * 128
                        sc.op("act", lambda: nc.scalar.activation(out=pbuf[sb_][:, 0:w_], in_=bank(sb_)[:, 0:w_],
                                                                  func=AF.Exp),
                              r=[bres[sb_]], w=[pbres[sb_]])
                        for a, kt in enumerate(ks_):
                            fst = (kt == kts[0])
                            sc.op("pe", lambda: nc.tensor.matmul(bank(ob)[:, r * 65:(r + 1) * 65],
                                                                 lhsT=pbuf[sb_][:, a * 128:(a + 1) * 128],
                                                                 rhs=vsw[:, kt, vidx, 0:65], start=fst, stop=(kt == kts[-1])),
                                  r=[pbres[sb_], vres[kt]],
                                  **({"w": [bres[ob]]} if (fst and r == 0) else {"pw": [bres[ob]]}))

                    j0 = njob[0]
                    qk(jobs[0], j0)
                    for n_, job in enumerate(jobs):
                        if n_ + 1 < len(jobs):
                            qk(jobs[n_ + 1], j0 + n_ + 1)
                        expv(job, j0 + n_)
                    njob[0] += len(jobs)
                    return ob

                def combine(ob, i, br, width, first, last):
                    k2 = nacc[0] % 2
                    o3 = bank(ob)[:, 0:4 * width].rearrange("p (r c) -> p r c", r=4)
                    rden = pp[:, k2, 0:4]
                    fac = pp[:, k2, 4:8]
                    sc.op("dve", lambda: nc.vector.tensor_scalar(out=rden, in0=o3[:, :, 64], scalar1=1e-30, scalar2=None,
                                                                 op0=ALU.max), r=[bres[ob]], w=[ppres[k2]])
                    sc.op("dve", lambda: nc.vector.reciprocal(out=rden, in_=rden), w=[ppres[k2]])
                    sc.op("dve", lambda: nc.vector.tensor_tensor(out=fac, in0=rden,
                                                                 in1=gate[:, i, 16 * br + 4 * g:16 * br + 4 * g + 4],
                                                                 op=ALU.mult), r=[gres], w=[ppres[k2]])
                    ya = yacc[i % 4]
                    yar = yaccres[i % 4]
                    dst = ya if first else ytmp[k2]
                    dres = yar if first else ytmpres[k2]
                    sc.op("dve", lambda: nc.vector.tensor_tensor(
                        out=dst[:].rearrange("p (r d) -> p r d", r=4), in0=o3[:, :, 0:64],
                        in1=fac.unsqueeze(2).to_broadcast([128, 4, 64]), op=ALU.mult),
                        r=[bres[ob], ppres[k2]], w=[dres])
                    sc.op("pool", lambda: nc.gpsimd.tensor_tensor(out=dst[:], in0=dst[:], in1=zs[:, i, br, :], op=ALU.mult),
                          r=[zres[i]], w=[dres])
                    if not first:
                        if last:
                            sc.op("pool", lambda: nc.gpsimd.tensor_tensor(out=y[:, i, 256 * g:256 * (g + 1)], in0=ya[:],
                                                                          in1=dst[:], op=ALU.add),
                                  r=[dres, yar], pw=[yres[i]])
                        else:
                            sc.op("pool", lambda: nc.gpsimd.tensor_tensor(out=ya[:], in0=ya[:], in1=dst[:], op=ALU.add),
                                  r=[dres], w=[yar])
                    return rden

                def bm_selwin(h, d):
                    if d == 0 or d == 1:
                        return cx.BM[:, h, d, :]
                    if d == 4:
                        return cx.BM4[:]
                    return None

                def bm_sel(h, d):
                    return cx.BM[:, h, d, :] if d in (0, 1) else None

                for i4 in range(4):
                    sl = slice(i4 * 512, (i4 + 1) * 512)
                    for r in range(4):
                        h = 4 * g + r
                        cb = (4 * i4 + r) % 2
                        src = bass.AP(cx.fc_d, h * 4096 + i4 * 512, [[16, CMP_N], [1, 512]])
                        sc.dma("sp", out=cbm[cb][0:CMP_N, :], in_=src, r=[cx.fcres], w=[cbmres[cb]])
                        sb_ = njob[0] % 3
                        njob[0] += 1
                        sc.op("pe", lambda: nc.tensor.matmul(bank(sb_)[0:CMP_N, :], lhsT=kcpad[:, r % 2, 0:CMP_N],
                                                             rhs=qT2[:, r // 2, sl], start=True, stop=False),
                              r=[kcres, qres[i4]], w=[bres[sb_]])
                        sc.op("pe", lambda: nc.tensor.matmul(bank(sb_)[0:CMP_N, :], lhsT=cx.antib[0:CMP_N, 1:128],
                                                             rhs=cbm[cb][0:CMP_N, :], start=False, stop=True),
                              r=[cbmres[cb], cx.cres], pw=[bres[sb_]])
                        sc.op("act", lambda: nc.scalar.activation(out=Pc[r][0:CMP_N, :], in_=bank(sb_)[0:CMP_N, :],
                                                                  func=AF.Exp),
                              r=[bres[sb_]], w=[Pcres[r]])
                    for t in range(4):
                        i = 4 * i4 + t
                        ob = 3 + nacc[0] % 2
                        nacc[0] += 1
                        for r in range(4):
                            sc.op("pe", lambda: nc.tensor.matmul(bank(ob)[:, r * 98:(r + 1) * 98],
                                                                 lhsT=Pc[r][0:CMP_N, t * 128:(t + 1) * 128],
                                                                 rhs=vcaug[0:CMP_N, :], start=True, stop=True),
                                  r=[Pcres[r], vcres], **({"w": [bres[ob]]} if r == 0 else {"pw": [bres[ob]]}))
                        rden = combine(ob, i, 0, 98, True, False)
                        if i >= 8:
                            o3 = bank(ob)[:, 0:392].rearrange("p (r c) -> p r c", r=4)
                            k2 = nacc[0] % 2
                            sc.op("dve", lambda: nc.vector.tensor_tensor(
                                out=tmp3[:], in0=o3[:, :, 66:98], in1=rden.unsqueeze(2).to_broadcast([128, 4, 32]),
                                op=ALU.mult), r=[bres[ob], ppres[k2]], w=[tmp3res])
                            s1 = tk[:, t, 0:32]
                            s2 = tk[:, t, 32:64]
                            m1 = tk[:, t, 64:72]
                            m2 = tk[:, t, 72:80]
                            sc.op("dve", lambda: nc.vector.tensor_reduce(
                                out=s1, in_=tmp3[:].rearrange("p r j -> p j r"), axis=mybir.AxisListType.X, op=ALU.add),
                                r=[tmp3res], w=[tkres[t]])
                            sc.op("dve", lambda: nc.vector.tensor_tensor(out=s1, in0=s1, in1=forced[:, i - 8, :],
                                                                         op=ALU.add), r=[lres], w=[tkres[t]])
                            sc.op("dve", lambda: nc.vector.max(out=m1, in_=s1), w=[tkres[t]])
                            sc.op("dve", lambda: nc.vector.match_replace(out=s2, in_to_replace=m1, in_values=s1,
                                                                         imm_value=-3.0e38), w=[tkres[t]])
                            sc.op("dve", lambda: nc.vector.max(out=m2, in_=s2), w=[tkres[t]])
                            sc.op("dve", lambda: nc.vector.tensor_scalar(out=selb[:, t, :], in0=s1, scalar1=m2[:, 7:8],
                                                                         scalar2=NEG, op0=ALU.is_lt, op1=ALU.mult),
                                  w=[tkres[t]])
                            pt, ptr = cx.psT[t % 2], cx.psTr[t % 2]
                            sc.op("pe", lambda: nc.tensor.transpose(out=pt[0:32, 0:128], in_=selb[:, t, :],
                                                                    identity=identb[:]),
                                  r=[tkres[t], cx.cres], w=[ptr])
                            sc.op("dve", lambda: nc.vector.tensor_copy(out=selbT[0:32, t * 128:(t + 1) * 128],
                                                                       in_=pt[0:32, 0:128]),
                                  r=[ptr], w=[selres[t]])
                    if stage(4 if i4 < 2 else 5):
                        return
                    for t in range(4):
                        i = 4 * i4 + t
                        ob = attn_tile(i, t, 1, kspad, ksres, 0, list(range(0, i + 1)), bm_sel, i >= 8)
                        combine(ob, i, 1, 65, False, False)
                        ob = attn_tile(i, t, 2, kwpad, kwres, 1, list(range(max(0, i - 4), i + 1)), bm_selwin, False)
                        combine(ob, i, 2, 65, False, True)
        sc.fence()
        with ExitStack() as es2:
            yT = uT
            yTres = uTres
            for i in range(NT):
                pt, ptr = cx.psT[i % 2], cx.psTr[i % 2]
                for c in range(8):
                    sc.op("pe", lambda: nc.tensor.transpose(out=pt[:, c * 128:(c + 1) * 128],
                                                            in_=y[:, i, c * 128:(c + 1) * 128], identity=identb[:]),
                          r=[yres[i], cx.cres], **({"w": [ptr]} if c == 0 else {"pw": [ptr]}))
                src = pt[:].rearrange("p (c s) -> p c s", c=8)
                dst = yT[:, :, i * 128:(i + 1) * 128]
                if i % 2 == 0:
                    sc.op("act", lambda: nc.scalar.copy(out=dst, in_=src), r=[ptr], w=[yTres[i]])
                else:
                    sc.op("dve", lambda: nc.vector.tensor_copy(out=dst, in_=src), r=[ptr], w=[yTres[i]])
            wo = es2.enter_context(nc.sbuf_tensor("wo_%d" % li, [128, 8, D_MODEL], BF16))
            wor = Res()
            wov = w_out.rearrange("(k p) c -> p k c", p=128)
            for q4 in range(2):
                sc.dma("pool", out=wo[:, q4 * 4:(q4 + 1) * 4, :], in_=wov[:, q4 * 4:(q4 + 1) * 4, :], pw=[wor])
            pgain = es2.enter_context(nc.sbuf_tensor("pog_%d" % li, [128, D_MODEL], F32))
            pgres = Res()
            sc.dma("sp", out=pgain[:], in_=bcast_row(P["post_norm_gain"][li], D_MODEL), w=[pgres])
            bufs = post_bufs(cx, es2, li)
            for i in range(NT):
                ops, opsr = cx.pbig[i % 3], cx.pbigr[i % 3]
                for half in range(2):
                    for kc in range(8):
                        sc.op("pe", lambda: nc.tensor.matmul(
                            ops[:, half * 512:(half + 1) * 512], lhsT=yT[:, kc, i * 128:(i + 1) * 128],
                            rhs=wo[:, kc, half * 512:(half + 1) * 512], start=(kc == 0), stop=(kc == 7)),
                            r=[yTres[i], wor],
                            **({"w": opsr} if (kc == 0 and half == 0) else {"pw": opsr}))
                emit_postnorm_residual(cx, es2, li, i, ops, opsr, pgain, pgres, h_in, hres_in, h_out, hres_out, bufs)
    sc.fence()


def _needed_params(layers):
    need = {"pre_norm_gain": None, "post_norm_gain": None}
    nsa = sorted({li // 2 for li in layers if li % 2 == 0})
    ret = sorted({li // 2 for li in layers if li % 2 == 1})
    if nsa:
        need["rel_bias_table"] = None
        for n in ("nsa_w_in", "nsa_w_out", "nsa_cmp_k_pos", "nsa_cmp_k_w1", "nsa_cmp_k_w2",
                  "nsa_cmp_v_pos", "nsa_cmp_v_w1", "nsa_cmp_v_w2"):
            need[n] = nsa
    if ret:
        for n in ("ret_w_in", "ret_w_out", "ret_gn_gain"):
            need[n] = ret
    return need


PARAM_SHAPES = {
    "pre_norm_gain": [4, 1024], "post_norm_gain": [4, 1024], "rel_bias_table": [32, 16],
    "nsa_w_in": [2, 1024, NSA_PROJ], "nsa_w_out": [2, 1024, 1024],
    "nsa_cmp_k_pos": [2, 32, 64], "nsa_cmp_k_w1": [2, 2048, 256], "nsa_cmp_k_w2": [2, 256, 64],
    "nsa_cmp_v_pos": [2, 32, 64], "nsa_cmp_v_w1": [2, 2048, 256], "nsa_cmp_v_w2": [2, 256, 64],
    "ret_w_in": [2, 1024, RET_PROJ], "ret_w_out": [2, 2048, 1024], "ret_gn_gain": [2, 2048],
}


def host_consts():
    rc = _ret_consts()
    c = {
        "c_cosT": rc["cosT"], "c_sinT": rc["sinT"],
        "c_innerT": rc["innerT"], "c_zeta": rc["zeta"], "c_epsp": rc["epsp"],
        "c_ident": np.eye(128, dtype=np.float32),
        "c_anti": np.ascontiguousarray(np.fliplr(np.eye(128, dtype=np.float32))),
    }
    c.update(_nsa_consts())
    return c, rc


def build_program(layers, first_from_x=True):
    consts, rc = host_consts()
    nc = bass.Bass("TRN2", target_bir_lowering=False, dynamic_dma_scratch_size=8192)
    P = {}
    x = nc.dram_tensor("x", [SEQ, D_MODEL], F32, kind="ExternalInput").ap()
    out = nc.dram_tensor("out", [SEQ, D_MODEL], F32, kind="ExternalOutput").ap()
    need = _needed_params(layers)
    slotmap = {}
    for name, shp in PARAM_SHAPES.items():
        if name not in need:
            continue
        shp = list(shp)
        if need[name] is not None:
            shp[0] = len(need[name])
            slotmap[name] = {s_: k_ for k_, s_ in enumerate(need[name])}
        P[name] = nc.dram_tensor(name, shp, F32, kind="ExternalInput").ap()
    has_nsa = any(li % 2 == 0 for li in layers)
    has_ret = any(li % 2 == 1 for li in layers)
    nsa_c = ("c_ohw", "c_ohc", "c_E", "c_ov", "c_forced")
    ret_c = ("c_cosT", "c_sinT")
    consts = {k: v for k, v in consts.items()
              if not ((k in nsa_c and not has_nsa) or (k in ret_c and not has_ret))}
    for name, arr in consts.items():
        P[name] = nc.dram_tensor(name, list(arr.shape), F32, kind="ExternalInput").ap()
    scratch = [nc.dram_tensor("hs%d" % i, [SEQ, D_MODEL], F32, kind="Internal").ap() for i in range(2)]
    Res.base = {}
    with ExitStack() as es:
        sc = Sched(nc, es)
        cx = Ctx()
        cx.nc, cx.sc, cx.P = nc, sc, P
        cx.dumps, cx.dump_res = [], []
        cx.pbig = [es.enter_context(nc.psum_tensor("pbig%d" % i, [128, 1024], F32)) for i in range(3)]
        cx.bres = [Res() for _ in range(6)]
        cx.pbigr = [[cx.bres[2 * i], cx.bres[2 * i + 1]] for i in range(3)]
        cx.bank = lambda k: cx.pbig[k // 2][:, (k % 2) * 512:(k % 2 + 1) * 512]
        cx.psT = [es.enter_context(nc.psum_tensor("psT%d" % i, [128, 1024], BF16)) for i in range(2)]
        cx.psTr = [Res(), Res()]
        cx.cres = Res()
        identf = es.enter_context(nc.sbuf_tensor("identf", [128, 128], F32))
        cx.identb = es.enter_context(nc.sbuf_tensor("identb", [128, 128], BF16))
        cx.eps_rms = es.enter_context(nc.sbuf_tensor("eps_rms", [128, 1], F32))
        sc.dma("sp", out=identf[:], in_=P["c_ident"][:, :], w=[cx.cres])
        sc.op("dve", lambda: nc.vector.tensor_copy(out=cx.identb[:], in_=identf[:]), r=[cx.cres], w=[cx.cres])
        sc.op("dve", lambda: nc.vector.memset(cx.eps_rms[:], RMS_EPS), pw=[cx.cres])
        antif = es.enter_context(nc.sbuf_tensor("antif", [128, 128], F32))
        cx.antib = es.enter_context(nc.sbuf_tensor("antib", [128, 128], BF16))
        ares = Res()
        sc.dma("sp", out=antif[:], in_=P["c_anti"][:, :], w=[ares])
        sc.op("dve", lambda: nc.vector.tensor_copy(out=cx.antib[:], in_=antif[:]), r=[ares], pw=[cx.cres])
        RC = {"gC": rc["gC"]}
        RC["innerT"] = es.enter_context(nc.sbuf_tensor("innerT", [128, 512], F32))
        RC["zeta"] = es.enter_context(nc.sbuf_tensor("zeta", [128, 4], F32))
        RC["epsp"] = es.enter_context(nc.sbuf_tensor("epsp", [128, 4], F32))
        sc.dma("sp", out=RC["innerT"][:], in_=P["c_innerT"][:, :], pw=[cx.cres])
        sc.dma("sp", out=RC["zeta"][:], in_=P["c_zeta"][:, :], pw=[cx.cres])
        sc.dma("sp", out=RC["epsp"][:], in_=P["c_epsp"][:, :], pw=[cx.cres])
        cx.RC = RC
        if any(li % 2 == 0 for li in layers):
            emit_nsa_prologue(cx, es)
        xres = [Res() for _ in range(NT)]
        ores = [Res() for _ in range(NT)]
        sres = [[Res() for _ in range(NT)] for _ in range(2)]
        cur, curres = x, xres
        for n, li in enumerate(layers):
            last = (n == len(layers) - 1)
            dst, dstres = (out, ores) if last else (scratch[n % 2], sres[n % 2])
            if li % 2 == 0:
                emit_nsa_layer(cx, li, slotmap["nsa_w_in"][li // 2], cur, curres, dst, dstres)
            else:
                emit_ret_layer(cx, li, slotmap["ret_w_in"][li // 2], cur, curres, dst, dstres)
            if STAGE < 99:
                sc.fence()
                sc._wait("sp", Res.base, skip_own=False)
                break
            cur, curres = dst, dstres
        sc.finish(ores + cx.dump_res)
        nc.all_engine_barrier()
        sc.clear_all()
    nc._dumps = cx.dumps
    return nc, consts


_PROG_CACHE = {}


def run_layers(layers, x, params):
    key = tuple(layers)
    if key not in _PROG_CACHE:
        _PROG_CACHE[key] = build_program(list(layers))
    nc, consts = _PROG_CACHE[key]
    B = x.shape[0]
    need = _needed_params(list(layers))
    shared = {}
    for name, sl in need.items():
        a = np.asarray(params[name], dtype=np.float32)
        shared[name] = np.ascontiguousarray(a if sl is None else a[sl])
    in_maps = []
    for b in range(B):
        m = {"x": np.ascontiguousarray(x[b])}
        m.update(shared)
        m.update(consts)
        in_maps.append(m)
    res = run_bass_kernel_spmd(nc, in_maps, core_ids=list(range(B)))
    global LAST_RESULTS
    LAST_RESULTS = res.results
    return np.stack([r["out"] for r in res.results], axis=0)


def kernel(x, **params):
    x = np.asarray(x, dtype=np.float32)
    h = run_layers(list(range(DEPTH)), x, params)
    return h.astype(np.float32)
```

```python
from contextlib import ExitStack
import math
import numpy as np
import concourse.bass as bass
import concourse.mybir as mybir
from concourse.bass_utils import run_bass_kernel_spmd

F32 = mybir.dt.float32
BF16 = mybir.dt.bfloat16
AF = mybir.ActivationFunctionType
ALU = mybir.AluOpType

D_MODEL = 1024
SEQ = 2048
NT = SEQ // 128
DEPTH = 4
RMS_EPS = 1e-6
GN_EPS = 1e-6
NEG = -30000.0

NSA_PROJ = 5680
CMP_N = 127
RET_PROJ = 6144


class Res:
    __slots__ = ("w", "r", "name")
    base = {}

    def __init__(self, name=""):
        self.w = {}
        self.r = dict(Res.base)
        self.name = name


def _merge(d, src):
    for k, (sem, v) in src.items():
        cur = d.get(k)
        if cur is None or cur[1] < v:
            d[k] = (sem, v)


class Sched:
    def __init__(self, nc, es, n_dma=40):
        self.nc = nc
        self.eng = {"pe": nc.tensor, "act": nc.scalar, "dve": nc.vector,
                    "pool": nc.gpsimd, "sp": nc.sync}
        self.esem = {}
        self.cnt = {}
        self.seen = {}
        for k in self.eng:
            self.esem[k] = es.enter_context(nc.semaphore("e_" + k))
            self.cnt[k] = 0
            self.seen[k] = {}
        self.dsem = [es.enter_context(nc.semaphore("d%d" % i)) for i in range(n_dma)]
        self.dtot = [0] * n_dma
        self.dnext = 0
        self.dnext_sw = 0
        self.sw_hist = []
        self.n_sw = 12
        self.n_wait = 0
        self.n_inst = 0
        self.clear_all()

    def clear_all(self):
        for sem in list(self.esem.values()) + self.dsem:
            self.nc.gpsimd.sem_clear(sem)
        self.nc.all_engine_barrier()

    def _wait(self, E, deps, skip_own=True):
        own = id(self.esem[E]) if skip_own else None
        seen = self.seen[E]
        for k, (sem, v) in deps.items():
            if k == own:
                continue
            if seen.get(k, 0) >= v:
                continue
            self.eng[E].wait_ge(sem, v)
            seen[k] = v
            self.n_wait += 1

    def _deps(self, r, w, pw):
        deps = {}
        for x in r:
            _merge(deps, x.w)
        for x in w:
            _merge(deps, x.w)
            _merge(deps, x.r)
        for x in pw:
            _merge(deps, x.r)
        return deps

    def _commit(self, tok, r, w, pw):
        k = id(tok[0])
        for x in r:
            cur = x.r.get(k)
            if cur is None or cur[1] < tok[1]:
                x.r[k] = tok
        for x in w:
            x.w = {k: tok}
        for x in pw:
            x.w[k] = tok

    def op(self, E, fn, r=(), w=(), pw=()):
        self._wait(E, self._deps(r, w, pw), skip_own=(E == "pe"))
        inst = fn()
        self.cnt[E] += 1
        inst.then_inc(self.esem[E], 1)
        self._commit((self.esem[E], self.cnt[E]), r, w, pw)
        self.n_inst += 1
        return inst

    def dma(self, q, out, in_, r=(), w=(), pw=(), **kw):
        self._wait(q, self._deps(r, w, pw), skip_own=False)
        nsw = self.n_sw
        if q == "pool":
            i = self.dnext_sw
            self.dnext_sw = (self.dnext_sw + 1) % nsw
        else:
            i = nsw + self.dnext
            self.dnext = (self.dnext + 1) % (len(self.dsem) - nsw)
        sem = self.dsem[i]
        if self.dtot[i] > 0:
            self._wait(q, {id(sem): (sem, self.dtot[i])})
        if q == "pool":
            if len(self.sw_hist) >= 3:
                psem, pval = self.sw_hist[-3]
                self._wait(q, {id(psem): (psem, pval)})
        inst = self.eng[q].dma_start(out=out, in_=in_, **kw)
        self.dtot[i] += 16
        inst.then_inc(sem, 16)
        self._commit((sem, self.dtot[i]), r, w, pw)
        if q == "pool":
            self.sw_hist.append((sem, self.dtot[i]))
        self.n_inst += 1
        return inst

    def fence(self):
        base = {}
        for k, sem in self.esem.items():
            if self.cnt[k] > 0:
                base[id(sem)] = (sem, self.cnt[k])
        for sem, tot in zip(self.dsem, self.dtot):
            if tot > 0:
                base[id(sem)] = (sem, tot)
        Res.base = base

    def finish(self, resources):
        deps = {}
        for x in resources:
            _merge(deps, x.w)
        self._wait("sp", deps)


def _ret_consts():
    H, C = 4, 128
    log_g = np.log(1.0 - 2.0 ** (-5.0 - np.arange(H, dtype=np.float64)))
    i = np.arange(C, dtype=np.float64)
    innerT = np.exp(-(i[:, None, None] + 1.0) * log_g[None, :, None]) * \
        (i[None, None, :] >= i[:, None, None])
    zeta = np.exp((C - 1.0 - i)[:, None] * log_g[None, :])
    xi = np.exp((i + 1.0)[:, None] * log_g[None, :])
    epsp = GN_EPS / (xi * xi)
    gC = np.exp(C * log_g)
    inv = (1.0 / (np.float32(10000.0) ** np.linspace(0.0, 1.0, 128, dtype=np.float32))).astype(np.float32)
    ang = (np.arange(SEQ, dtype=np.float32)[:, None] * inv[None, :]).astype(np.float32)
    cosT = np.ascontiguousarray(np.cos(ang).T.astype(np.float32))
    sinT = np.ascontiguousarray(np.sin(ang).T.astype(np.float32))
    return dict(innerT=innerT.astype(np.float32).reshape(128, 4 * 128),
                zeta=zeta.astype(np.float32), epsp=epsp.astype(np.float32),
                gC=[float(x) for x in gC], cosT=cosT, sinT=sinT)


DEBUG = False
STAGE = 99
TOKSTOP = -1
NOEVAC = ()


class StopEmit(Exception):
    pass


def stage(n):
    return STAGE <= n


class Ctx:
    def dump(self, name, ap, res, dt):
        if not DEBUG:
            return
        d = self.nc.dram_tensor("dbg_" + name, list(ap.shape), dt, kind="ExternalOutput").ap()
        self.dumps.append("dbg_" + name)
        r = Res()
        if len(ap.shape) == 3:
            for j in range(ap.shape[1]):
                self.sc.dma("sp", out=d[:, j, :], in_=ap[:, j, :], r=list(res), pw=[r])
        else:
            self.sc.dma("sp", out=d, in_=ap, r=list(res), w=[r])
        self.dump_res.append(r)


def bcast_row(dram_ap_row, n):
    return bass.AP(dram_ap_row.tensor, dram_ap_row.offset, [[0, 128], [1, n]])


def emit_prenorm(cx, es, li, h_in, hres, pre_gain_row, uT, uTres):
    nc, sc = cx.nc, cx.sc
    gain = es.enter_context(nc.sbuf_tensor("pg%d" % li, [128, D_MODEL], F32))
    gres = Res()
    sc.dma("sp", out=gain[:], in_=bcast_row(pre_gain_row, D_MODEL), w=[gres])
    hb = [es.enter_context(nc.sbuf_tensor("hb%d_%d" % (li, k), [128, D_MODEL], F32)) for k in range(2)]
    hbr = [Res(), Res()]
    ub = [es.enter_context(nc.sbuf_tensor("ub%d_%d" % (li, k), [128, D_MODEL], BF16)) for k in range(2)]
    ubr = [Res(), Res()]
    junk = es.enter_context(nc.sbuf_tensor("junk%d" % li, [128, D_MODEL], BF16))
    junkr = Res()
    ss = es.enter_context(nc.sbuf_tensor("ss%d" % li, [128, NT], F32))
    rs = es.enter_context(nc.sbuf_tensor("rs%d" % li, [128, NT], F32))
    ssr = [Res() for _ in range(NT)]
    for i in range(NT):
        k = i % 2
        sc.dma("sp", out=hb[k][:], in_=h_in[i * 128:(i + 1) * 128, :], r=[hres[i]], w=[hbr[k]])
        sc.op("act", lambda: nc.scalar.activation(out=junk[:], in_=hb[k][:], func=AF.Square,
                                                  accum_out=ss[:, i:i + 1]),
              r=[hbr[k]], w=[junkr, ssr[i]])
        sc.op("act", lambda: nc.scalar.activation(out=rs[:, i:i + 1], in_=ss[:, i:i + 1], func=AF.Ln,
                                                  scale=1.0 / D_MODEL, bias=cx.eps_rms[:, 0:1]),
              r=[ssr[i], cx.cres], w=[ssr[i]])
        sc.op("act", lambda: nc.scalar.activation(out=rs[:, i:i + 1], in_=rs[:, i:i + 1], func=AF.Exp,
                                                  scale=-0.5), r=[ssr[i]], w=[ssr[i]])
        sc.op("dve", lambda: nc.vector.scalar_tensor_tensor(out=ub[k][:], in0=hb[k][:], scalar=rs[:, i:i + 1],
                                                            in1=gain[:], op0=ALU.mult, op1=ALU.mult),
              r=[hbr[k], ssr[i], gres], w=[ubr[k]])
        pt, ptr = cx.psT[i % 2], cx.psTr[i % 2]
        for c in range(8):
            sc.op("pe", lambda: nc.tensor.transpose(out=pt[:, c * 128:(c + 1) * 128],
                                                    in_=ub[k][:, c * 128:(c + 1) * 128], identity=cx.identb[:]),
                  r=[ubr[k], cx.cres], **({"w": [ptr]} if c == 0 else {"pw": [ptr]}))
        src = pt[:].rearrange("p (c s) -> p c s", c=8)
        dst = uT[:, :, i * 128:(i + 1) * 128]
        if i % 2 == 0:
            sc.op("act", lambda: nc.scalar.copy(out=dst, in_=src), r=[ptr], w=[uTres[i]])
        else:
            sc.op("dve", lambda: nc.vector.tensor_copy(out=dst, in_=src), r=[ptr], w=[uTres[i]])


def emit_postnorm_residual(cx, es, li, i, ops, opsr, post_gain, pgres, h_in, hres_in, h_out, hres_out, bufs):
    nc, sc = cx.nc, cx.sc
    k = i % 2
    hb, hbr, tb, tbr, junk, junkr, st, str_ = bufs
    sc.dma("sp", out=hb[k][:], in_=h_in[i * 128:(i + 1) * 128, :], r=[hres_in[i]], w=[hbr[k]])
    sc.op("act", lambda: nc.scalar.activation(out=junk[:], in_=ops[:], func=AF.Square,
                                              accum_out=st[:, 2 * i:2 * i + 1]),
          r=list(opsr), w=[junkr, str_[i]])
    sc.op("act", lambda: nc.scalar.activation(out=st[:, 2 * i + 1:2 * i + 2], in_=st[:, 2 * i:2 * i + 1], func=AF.Ln,
                                              scale=1.0 / D_MODEL, bias=cx.eps_rms[:, 0:1]),
          r=[str_[i], cx.cres], w=[str_[i]])
    sc.op("act", lambda: nc.scalar.activation(out=st[:, 2 * i + 1:2 * i + 2], in_=st[:, 2 * i + 1:2 * i + 2],
                                              func=AF.Exp, scale=-0.5), r=[str_[i]], w=[str_[i]])
    sc.op("dve", lambda: nc.vector.scalar_tensor_tensor(out=tb[k][:], in0=ops[:], scalar=st[:, 2 * i + 1:2 * i + 2],
                                                        in1=post_gain[:], op0=ALU.mult, op1=ALU.mult),
          r=list(opsr) + [str_[i], pgres], w=[tbr[k]])
    sc.op("pool", lambda: nc.gpsimd.tensor_tensor(out=tb[k][:], in0=tb[k][:], in1=hb[k][:], op=ALU.add),
          r=[hbr[k]], w=[tbr[k]])
    sc.dma("sp", out=h_out[i * 128:(i + 1) * 128, :], in_=tb[k][:], r=[tbr[k]], w=[hres_out[i]])


def post_bufs(cx, es, li):
    nc = cx.nc
    hb = [es.enter_context(nc.sbuf_tensor("phb%d_%d" % (li, k), [128, D_MODEL], F32)) for k in range(2)]
    tb = [es.enter_context(nc.sbuf_tensor("ptb%d_%d" % (li, k), [128, D_MODEL], F32)) for k in range(2)]
    junk = es.enter_context(nc.sbuf_tensor("pjunk%d" % li, [128, D_MODEL], BF16))
    st = es.enter_context(nc.sbuf_tensor("pst%d" % li, [128, 2 * NT], F32))
    return (hb, [Res(), Res()], tb, [Res(), Res()], junk, Res(), st, [Res() for _ in range(NT)])


def emit_ret_layer(cx, li, slot, h_in, hres_in, h_out, hres_out):
    nc, sc, P = cx.nc, cx.sc, cx.P
    w_in = P["ret_w_in"][slot]
    w_out = P["ret_w_out"][slot]
    gn = P["ret_gn_gain"][slot]
    RC = cx.RC
    with ExitStack() as es:
        def sb(name, shape, dt):
            return es.enter_context(nc.sbuf_tensor("%s_%d" % (name, li), shape, dt))
        uT = sb("uT", [128, 8, SEQ], BF16)
        uTres = [Res() for _ in range(NT)]
        yT = sb("yT", [128, 16, SEQ], BF16)
        yTres = [Res() for _ in range(NT)]
        with ExitStack() as es0:
            emit_prenorm(cx, es0, li, h_in, hres_in, P["pre_norm_gain"][li], uT, uTres)
        sc.fence()
        with ExitStack() as es1:
            def sb1(name, shape, dt):
                return es1.enter_context(nc.sbuf_tensor("%s_%d" % (name, li), shape, dt))
            cosb = [sb1("cosb0", [128, 512], F32)] * 2
            sinb = [sb1("sinb0", [128, 512], F32)] * 2
            csr = [Res()] * 2
            wq = sb1("wq", [128, 8, 256], BF16)
            wk = sb1("wk", [128, 8, 256], BF16)
            wv = sb1("wv", [128, 8, 512], BF16)
            wz = wv
            wres = {n: Res() for n in "qkv"}
            wres["z"] = wres["v"]
            qT = sb1("qT", [128, 2, SEQ], BF16)
            kT = sb1("kT", [128, 2, SEQ], BF16)
            qTr = [Res() for _ in range(4)]
            kTr = [Res() for _ in range(4)]
            ktok = sb1("ktok", [128, NT, 256], BF16)
            ktokr = [Res() for _ in range(NT)]
            vt = sb1("vt", [128, NT, 512], BF16)
            vtr = [Res() for _ in range(NT)]
            zs = sb1("zs", [128, NT, 512], BF16)
            zsr = [Res() for _ in range(NT)]
            gnb = sb1("gnb", [128, 512], F32)
            gnr = Res()
            xa = [sb1("xa0", [128, 512], F32)] * 2
            xb = [sb1("xb0", [128, 512], F32)] * 2
            xar = [Res()] * 2
            xbr = [Res()] * 2
            t1 = [sb1("t1_0", [128, 512], F32)] * 2
            t2 = [sb1("t2_0", [128, 512], F32)] * 2
            t3 = [sb1("t3_0", [128, 512], F32)] * 2
            t4 = [sb1("t4_0", [128, 512], F32)] * 2
            t12r = [Res()] * 2
            t34r = [Res()] * 2
            St = sb1("St", [128, 2, 512], F32)
            Sb = sb1("Sb", [128, 2, 512], BF16)
            Str = [Res(), Res()]
            Sbr = [Res(), Res()]
            at = [sb1("at%d" % k, [128, 128], BF16) for k in range(2)]
            atr = [Res(), Res()]
            on = [sb1("on0", [128, 512], F32)] * 2
            onr = [Res()] * 2
            yb = [sb1("yb0", [128, 512], BF16)] * 2
            ybr = [Res()] * 2
            stt = sb1("stt", [128, 2, 8], F32)
            mv = sb1("mv", [128, 2, 4], F32)
            sttr = [Res(), Res()]
            wviews = {
                "q": lambda hd: w_in[:, hd * 256:(hd + 1) * 256],
                "k": lambda hd: w_in[:, 1024 + hd * 256:1024 + (hd + 1) * 256],
                "v": lambda hd: w_in[:, 2048 + hd * 512:2048 + (hd + 1) * 512],
                "z": lambda hd: w_in[:, 4096 + hd * 512:4096 + (hd + 1) * 512],
            }
            wt = {"q": wq, "k": wk, "v": wv, "z": wz}
            bank = cx.bank
            bres = cx.bres
            nb = 0
            for hd in range(4):
                for n in "qkv":
                    sc.dma("pool", out=wt[n][:], in_=wviews[n](hd).rearrange("(k p) c -> p k c", p=128),
                           w=[wres[n]])
                sc.dma("sp", out=gnb[:], in_=bcast_row(gn[hd * 512:(hd + 1) * 512], 512), w=[gnr])
                for s4 in range(4):
                    cb = s4 % 2
                    sc.dma("sp", out=cosb[cb][:], in_=P["c_cosT"][:, s4 * 512:(s4 + 1) * 512], w=[csr[cb]])
                    sc.dma("sp", out=sinb[cb][:], in_=P["c_sinT"][:, s4 * 512:(s4 + 1) * 512], pw=[csr[cb]])
                    cres = csr[cb]
                    for n, dst, dres, scale in (("q", qT, qTr, 1.0), ("k", kT, kTr, 1.0 / 16.0)):
                        pa, pb = nb % 6, (nb + 1) % 6
                        nb += 2
                        for c, pbk in ((0, pa), (1, pb)):
                            for kc in range(8):
                                sc.op("pe", lambda: nc.tensor.matmul(
                                    bank(pbk), lhsT=wt[n][:, kc, c * 128:(c + 1) * 128],
                                    rhs=uT[:, kc, s4 * 512:(s4 + 1) * 512], start=(kc == 0), stop=(kc == 7)),
                                    r=[wres[n]] + uTres[s4 * 4:(s4 + 1) * 4],
                                    **({"w": [bres[pbk]]} if kc == 0 else {"pw": [bres[pbk]]}))
                        k2 = s4 % 2
                        sc.op("act", lambda: nc.scalar.activation(out=xa[k2][:], in_=bank(pa), func=AF.Copy,
                                                                  scale=scale), r=[bres[pa]], w=[xar[k2]])
                        sc.op("act", lambda: nc.scalar.activation(out=xb[k2][:], in_=bank(pb), func=AF.Copy,
                                                                  scale=scale), r=[bres[pb]], w=[xbr[k2]])
                        cs = cosb[cb][:]
                        sn = sinb[cb][:]
                        sc.op("dve", lambda: nc.vector.tensor_tensor(out=t1[k2][:], in0=xa[k2][:], in1=cs, op=ALU.mult),
                              r=[xar[k2], cres], w=[t12r[k2]])
                        sc.op("dve", lambda: nc.vector.tensor_tensor(out=t2[k2][:], in0=xb[k2][:], in1=sn, op=ALU.mult),
                              r=[xbr[k2], cres], pw=[t12r[k2]])
                        sc.op("dve", lambda: nc.vector.tensor_tensor(out=dst[:, 0, s4 * 512:(s4 + 1) * 512],
                                                                     in0=t1[k2][:], in1=t2[k2][:], op=ALU.subtract),
                              r=[t12r[k2]], w=[dres[s4]])
                        sc.op("pool", lambda: nc.gpsimd.tensor_tensor(out=t3[k2][:], in0=xa[k2][:], in1=sn, op=ALU.mult),
                              r=[xar[k2], cres], w=[t34r[k2]])
                        sc.op("pool", lambda: nc.gpsimd.tensor_tensor(out=t4[k2][:], in0=xb[k2][:], in1=cs, op=ALU.mult),
                              r=[xbr[k2], cres], pw=[t34r[k2]])
                        sc.op("pool", lambda: nc.gpsimd.tensor_tensor(out=dst[:, 1, s4 * 512:(s4 + 1) * 512],
                                                                      in0=t3[k2][:], in1=t4[k2][:], op=ALU.add),
                              r=[t34r[k2]], pw=[dres[s4]])
                for n in "vz":
                    if n == "z":
                        sc.dma("pool", out=wt["z"][:], in_=wviews["z"](hd).rearrange("(k p) c -> p k c", p=128),
                               w=[wres["z"]])
                    for i in range(NT):
                        pbk = nb % 6
                        nb += 1
                        for kc in range(8):
                            sc.op("pe", lambda: nc.tensor.matmul(
                                bank(pbk), lhsT=uT[:, kc, i * 128:(i + 1) * 128], rhs=wt[n][:, kc, :],
                                start=(kc == 0), stop=(kc == 7)),
                                r=[wres[n], uTres[i]],
                                **({"w": [bres[pbk]]} if kc == 0 else {"pw": [bres[pbk]]}))
                        if n == "v":
                            sc.op("dve", lambda: nc.vector.tensor_copy(out=vt[:, i, :], in_=bank(pbk)),
                                  r=[bres[pbk]], w=[vtr[i]])
                        else:
                            sc.op("act", lambda: nc.scalar.activation(out=zs[:, i, :], in_=bank(pbk), func=AF.Silu),
                                  r=[bres[pbk]], w=[zsr[i]])
                            sc.op("pool", lambda: nc.gpsimd.tensor_tensor(out=zs[:, i, :], in0=zs[:, i, :],
                                                                          in1=gnb[:], op=ALU.mult),
                                  r=[gnr], w=[zsr[i]])
                for i in range(NT):
                    pt, ptr = cx.psT[i % 2], cx.psTr[i % 2]
                    for c in range(2):
                        sc.op("pe", lambda: nc.tensor.transpose(out=pt[:, c * 128:(c + 1) * 128],
                                                                in_=kT[:, c, i * 128:(i + 1) * 128],
                                                                identity=cx.identb[:]),
                              r=[kTr[i // 4], cx.cres], **({"w": [ptr]} if c == 0 else {"pw": [ptr]}))
                    sc.op("dve", lambda: nc.vector.tensor_scalar(out=ktok[:, i, :], in0=pt[:, 0:256],
                                                                 scalar1=RC["zeta"][:, hd:hd + 1], scalar2=None,
                                                                 op0=ALU.mult),
                          r=[ptr, cx.cres], w=[ktokr[i]])
                def phaseA(n):
                    k2 = n % 2
                    ps_s, ps_o = (0, 1)[k2], (2, 3)[k2]
                    sl = slice(n * 128, (n + 1) * 128)
                    for c in range(2):
                        sc.op("pe", lambda: nc.tensor.matmul(bank(ps_s)[:, 0:128], lhsT=kT[:, c, sl], rhs=qT[:, c, sl],
                                                             start=(c == 0), stop=(c == 1)),
                              r=[kTr[n // 4], qTr[n // 4]],
                              **({"w": [bres[ps_s]]} if c == 0 else {"pw": [bres[ps_s]]}))
                    sc.op("dve", lambda: nc.vector.tensor_tensor(out=at[k2][:], in0=bank(ps_s)[:, 0:128],
                                                                 in1=RC["innerT"][:, hd * 128:(hd + 1) * 128],
                                                                 op=ALU.mult),
                          r=[bres[ps_s], cx.cres], w=[atr[k2]])
                    sc.op("pe", lambda: nc.tensor.matmul(bank(ps_o), lhsT=at[k2][:], rhs=vt[:, n, :],
                                                         start=True, stop=(n == 0)),
                          r=[atr[k2], vtr[n]], w=[bres[ps_o]])
                    if n > 0:
                        for c in range(2):
                            sc.op("pe", lambda: nc.tensor.matmul(bank(ps_o), lhsT=qT[:, c, sl], rhs=Sb[:, c, :],
                                                                 start=False, stop=(c == 1)),
                                  r=[qTr[n // 4], Sbr[c]], pw=[bres[ps_o]])
                    if n < NT - 1:
                        for c in range(2):
                            pd = 4 + c
                            sc.op("pe", lambda: nc.tensor.matmul(bank(pd), lhsT=ktok[:, n, c * 128:(c + 1) * 128],
                                                                 rhs=vt[:, n, :], start=True, stop=True),
                                  r=[ktokr[n], vtr[n]], w=[bres[pd]])
                            if n == 0:
                                sc.op("dve", lambda: nc.vector.tensor_copy(out=St[:, c, :], in_=bank(pd)),
                                      r=[bres[pd]], w=[Str[c]])
                            else:
                                sc.op("dve", lambda: nc.vector.scalar_tensor_tensor(
                                    out=St[:, c, :], in0=St[:, c, :], scalar=RC["gC"][hd], in1=bank(pd),
                                    op0=ALU.mult, op1=ALU.add), r=[bres[pd]], w=[Str[c]])
                            sc.op("act", lambda: nc.scalar.copy(out=Sb[:, c, :], in_=St[:, c, :]),
                                  r=[Str[c]], w=[Sbr[c]])

                def phaseB(n):
                    k2 = n % 2
                    ps_o = (2, 3)[k2]
                    sl = slice(n * 128, (n + 1) * 128)
                    sc.op("dve", lambda: nc.vector.bn_stats(out=stt[:, k2, 0:6], in_=bank(ps_o)),
                          r=[bres[ps_o]], w=[sttr[k2]])
                    sc.op("dve", lambda: nc.vector.bn_aggr(out=mv[:, k2, 0:2], in_=stt[:, k2, 0:6]),
                          r=[], w=[sttr[k2]])
                    sc.op("act", lambda: nc.scalar.activation(out=mv[:, k2, 2:3], in_=mv[:, k2, 1:2], func=AF.Ln,
                                                              bias=RC["epsp"][:, hd:hd + 1]),
                          r=[cx.cres], w=[sttr[k2]])
                    sc.op("act", lambda: nc.scalar.activation(out=mv[:, k2, 2:3], in_=mv[:, k2, 2:3], func=AF.Exp,
                                                              scale=-0.5), w=[sttr[k2]])
                    sc.op("dve", lambda: nc.vector.scalar_tensor_tensor(
                        out=mv[:, k2, 3:4], in0=mv[:, k2, 0:1], scalar=-1.0, in1=mv[:, k2, 2:3],
                        op0=ALU.mult, op1=ALU.mult), w=[sttr[k2]])
                    sc.op("act", lambda: nc.scalar.activation(out=on[k2][:], in_=bank(ps_o), func=AF.Identity,
                                                              scale=mv[:, k2, 2:3], bias=mv[:, k2, 3:4]),
                          r=[bres[ps_o], sttr[k2]], w=[onr[k2]])
                    sc.op("pool", lambda: nc.gpsimd.tensor_tensor(out=yb[k2][:], in0=on[k2][:], in1=zs[:, n, :],
                                                                  op=ALU.mult),
                          r=[onr[k2], zsr[n]], w=[ybr[k2]])
                    pt, ptr = cx.psT[k2], cx.psTr[k2]
                    for c in range(4):
                        sc.op("pe", lambda: nc.tensor.transpose(out=pt[:, c * 128:(c + 1) * 128],
                                                                in_=yb[k2][:, c * 128:(c + 1) * 128],
                                                                identity=cx.identb[:]),
                              r=[ybr[k2], cx.cres], **({"w": [ptr]} if c == 0 else {"pw": [ptr]}))
                    sc.op("act", lambda: nc.scalar.copy(out=yT[:, hd * 4:(hd + 1) * 4, sl],
                                                        in_=pt[:, 0:512].rearrange("p (c s) -> p c s", c=4)),
                          r=[ptr], **({"w": [yTres[n]]} if hd == 0 else {"pw": [yTres[n]]}))
                phaseA(0)
                for n in range(NT):
                    if n + 1 < NT:
                        phaseA(n + 1)
                    phaseB(n)
                if hd == 3:
                    cx.dump("qT", qT[:], qTr, BF16)
                    cx.dump("kT", kT[:], kTr, BF16)
                    cx.dump("vt", vt[:], vtr, BF16)
                    cx.dump("zs", zs[:], zsr, BF16)
                    cx.dump("ktok", ktok[:], ktokr, BF16)
                    cx.dump("St", St[:], Str, F32)
            cx.dump("uT", uT[:], uTres, BF16)
            cx.dump("yT", yT[:], yTres, BF16)
        sc.fence()
        with ExitStack() as es2:
            wo = es2.enter_context(nc.sbuf_tensor("wo_%d" % li, [128, 16, D_MODEL], BF16))
            wor = Res()
            wov = w_out.rearrange("(k p) c -> p k c", p=128)
            for q4 in range(4):
                sc.dma("pool", out=wo[:, q4 * 4:(q4 + 1) * 4, :], in_=wov[:, q4 * 4:(q4 + 1) * 4, :],
                       **({"w": [wor]} if q4 == 0 else {"pw": [wor]}))
            pgain = es2.enter_context(nc.sbuf_tensor("pog_%d" % li, [128, D_MODEL], F32))
            pgres = Res()
            sc.dma("sp", out=pgain[:], in_=bcast_row(P["post_norm_gain"][li], D_MODEL), w=[pgres])
            bufs = post_bufs(cx, es2, li)
            for i in range(NT):
                ops, opsr = cx.pbig[i % 3], cx.pbigr[i % 3]
                for half in range(2):
                    for kc in range(16):
                        sc.op("pe", lambda: nc.tensor.matmul(
                            ops[:, half * 512:(half + 1) * 512], lhsT=yT[:, kc, i * 128:(i + 1) * 128],
                            rhs=wo[:, kc, half * 512:(half + 1) * 512], start=(kc == 0), stop=(kc == 15)),
                            r=[yTres[i], wor],
                            **({"w": opsr} if (kc == 0 and half == 0) else {"pw": opsr}))
                emit_postnorm_residual(cx, es2, li, i, ops, opsr, pgain, pgres, h_in, hres_in, h_out, hres_out, bufs)
    sc.fence()


def _t5_bucket(n):
    n = np.maximum(n, 0)
    nf = np.maximum(n, 1).astype(np.float32)
    large = 16 + (np.log(nf / np.float32(16)) / np.float32(math.log(128 / 16)) * np.float32(16)).astype(np.int32)
    large = np.minimum(large, 31)
    return np.where(n < 16, n, large)


def _nsa_consts():
    def onehot(n, valid):
        b = _t5_bucket(n)
        oh = np.zeros((33, n.shape[0]), np.float32)
        idx = np.arange(n.shape[0])
        oh[b[valid], idx[valid]] = 1.0
        oh[31, idx[valid]] -= 1.0
        oh[32, idx[~valid]] = 1.0
        return oh
    nw = np.arange(768) - 127
    ohw = onehot(nw, (nw >= 0) & (nw < 512))
    ncm = np.arange(4096) - 2047
    ohc = onehot(ncm, ncm >= 0)
    E = np.zeros((128, SEQ), np.float32)
    E[np.arange(SEQ) // 64, np.arange(SEQ)] = 1.0
    cs = np.arange(CMP_N) * 16
    j = np.arange(32)
    ov = ((cs[:, None] < (j[None, :] + 1) * 64) & (cs[:, None] + 32 > j[None, :] * 64)).astype(np.float32)
    t = np.arange(SEQ)
    cur = t // 64
    valid = j[None, :] * 64 <= t[:, None]
    forced = (j[None, :] == 0) | (j[None, :] == cur[:, None]) | (j[None, :] == cur[:, None] - 1)
    fm = np.where(valid, np.where(forced, 1000.0, 0.0), -1e30).astype(np.float32)
    fm = fm[1024:].reshape(8, 128, 32).transpose(1, 0, 2).reshape(128, 256)
    return {"c_ohw": ohw, "c_ohc": ohc, "c_E": E, "c_ov": np.ascontiguousarray(ov),
            "c_forced": np.ascontiguousarray(fm)}


def emit_nsa_prologue(cx, es):
    nc, sc, P = cx.nc, cx.sc, cx.P
    fw_d = nc.dram_tensor("fw_d", [16, 768], BF16, kind="Internal")
    fc_d = nc.dram_tensor("fc_d", [16, 4096], BF16, kind="Internal")
    cx.fc_d = fc_d
    cx.BM = es.enter_context(nc.sbuf_tensor("BM", [128, 16, 2, 128], BF16))
    cx.BM4 = es.enter_context(nc.sbuf_tensor("BM4", [128, 128], BF16))
    cx.Epad = es.enter_context(nc.sbuf_tensor("Epad", [128, SEQ], BF16))
    cx.bmres = Res()
    cx.fcres = Res()
    sc.dma("pool", out=cx.Epad[:], in_=P["c_E"][:, :], pw=[cx.bmres])
    with ExitStack() as ep:
        tab = ep.enter_context(nc.sbuf_tensor("tab", [33, 16], F32))
        ohw = ep.enter_context(nc.sbuf_tensor("ohw", [33, 768], F32))
        ohc = ep.enter_context(nc.sbuf_tensor("ohc", [33, 4096], F32))
        fwb = ep.enter_context(nc.sbuf_tensor("fwb", [16, 768], BF16))
        fcb = ep.enter_context(nc.sbuf_tensor("fcb", [16, 4096], BF16))
        tr, fr = Res(), Res()
        sc.op("dve", lambda: nc.vector.memset(tab[32:33, :], NEG), pw=[tr])
        sc.dma("sp", out=tab[0:32, :], in_=P["rel_bias_table"][:, :], pw=[tr])
        sc.dma("sp", out=ohw[:], in_=P["c_ohw"][:, :], pw=[tr])
        for q in range(4):
            sc.dma("sp", out=ohc[:, q * 1024:(q + 1) * 1024], in_=P["c_ohc"][:, q * 1024:(q + 1) * 1024], pw=[tr])
        bank, bres = cx.bank, cx.bres
        for q, (c0, c1) in enumerate(((0, 512), (512, 768))):
            sc.op("pe", lambda: nc.tensor.matmul(bank(q)[0:16, 0:c1 - c0], lhsT=tab[:, :], rhs=ohw[:, c0:c1],
                                                 start=True, stop=True), r=[tr], w=[bres[q]])
            sc.op("dve", lambda: nc.vector.tensor_copy(out=fwb[:, c0:c1], in_=bank(q)[0:16, 0:c1 - c0]),
                  r=[bres[q]], pw=[fr])
        for q in range(8):
            b = 2 + q % 4
            sc.op("pe", lambda: nc.tensor.matmul(bank(b)[0:16, :], lhsT=tab[:, :], rhs=ohc[:, q * 512:(q + 1) * 512],
                                                 start=True, stop=True), r=[tr], w=[bres[b]])
            sc.op("dve", lambda: nc.vector.tensor_copy(out=fcb[:, q * 512:(q + 1) * 512], in_=bank(b)[0:16, :]),
                  r=[bres[b]], pw=[fr])
        dres = Res()
        sc.dma("sp", out=fw_d.ap(), in_=fwb[:], r=[fr], w=[dres])
        sc.dma("sp", out=fc_d.ap(), in_=fcb[:], r=[fr], w=[cx.fcres])
        for h0 in range(0, 16, 4):
            src = bass.AP(fw_d, h0 * 768, [[1, 128], [768, 4], [128, 2], [1, 128]])
            sc.dma("sp", out=cx.BM[:, h0:h0 + 4, :, :], in_=src, r=[dres], pw=[cx.bmres])
        sc.dma("sp", out=cx.BM4[:], in_=bass.AP(fw_d, 512, [[1, 128], [1, 128]]), r=[dres], pw=[cx.bmres])
        sc.finish([cx.bmres, cx.fcres, dres])
    sc.fence()


def emit_nsa_layer(cx, li, slot, h_in, hres_in, h_out, hres_out):
    nc, sc, P = cx.nc, cx.sc, cx.P
    w_in = P["nsa_w_in"][slot]
    w_out = P["nsa_w_out"][slot]
    bank, bres = cx.bank, cx.bres
    identb = cx.identb
    with ExitStack() as es:
        def sbL(name, shape, dt):
            return es.enter_context(nc.sbuf_tensor("%s_%d" % (name, li), shape, dt))
        uT = sbL("uT", [128, 8, SEQ], BF16)
        uTres = [Res() for _ in range(NT)]
        y = sbL("y", [128, NT, D_MODEL], BF16)
        yres = [Res() for _ in range(NT)]
        with ExitStack() as es0:
            emit_prenorm(cx, es0, li, h_in, hres_in, P["pre_norm_gain"][li], uT, uTres)
        sc.fence()
        if stage(0):
            return
        with ExitStack() as es1:
            def sb(name, shape, dt):
                return es1.enter_context(nc.sbuf_tensor("%s_%d" % (name, li), shape, dt))
            wview = w_in.rearrange("(k p) c -> p k c", p=128)
            W1p = {n: sb("W1p" + n, [128, 16, 256], BF16) for n in "kv"}
            w2k = sb("w2k", [128, 2, 128], BF16)
            w2v = sb("w2v", [128, 2, 64], BF16)
            pos2 = {n: sb("pos2" + n, [128, 16], BF16) for n in "kv"}
            pb = {n: sb("pb" + n, [128, 2], F32) for n in "kv"}
            wg = sb("wg", [128, 8, 48], BF16)
            forced = sb("forced", [128, 8, 32], F32)
            lres = Res()
            for n in "kv":
                sc.dma("pool", out=W1p[n][:], in_=P["nsa_cmp_%s_w1" % n][slot].rearrange("(lp p) c -> p lp c", p=128),
                       pw=[lres])
                pos = P["nsa_cmp_%s_pos" % n][slot]
                for par in range(2):
                    src = bass.AP(pos.tensor, pos.offset + par * 64, [[1, 64], [128, 16]])
                    sc.dma("pool", out=pos2[n][par * 64:(par + 1) * 64, :], in_=src, pw=[lres],
                           allow_slow_non_contiguous=True)
            w2kv = P["nsa_cmp_k_w2"][slot].rearrange("(c p) d -> p c d", p=128)
            sc.dma("pool", out=w2k[:, :, 0:64], in_=w2kv, pw=[lres])
            sc.dma("pool", out=w2k[:, :, 64:128], in_=w2kv, pw=[lres])
            sc.dma("pool", out=w2v[:], in_=P["nsa_cmp_v_w2"][slot].rearrange("(c p) d -> p c d", p=128), pw=[lres])
            sc.dma("pool", out=wg[:], in_=wview[:, :, 2560:2608], pw=[lres])
            sc.dma("sp", out=forced[:], in_=P["c_forced"][:, :].rearrange("p (a b) -> p a b", a=8), pw=[lres])
            wq = sb("wq", [128, 8, 256], BF16)
            wks2 = sb("wks2", [128, 8, 128], BF16)
            wkw2 = sb("wkw2", [128, 8, 128], BF16)
            wkc2 = sb("wkc2", [128, 8, 128], BF16)
            wvc2 = sb("wvc2", [128, 8, 128], BF16)
            wtok = sb("wtok", [128, 8, 896], BF16)
            wres = Res()
            qT2 = sb("qT2", [128, 2, SEQ], BF16)
            qres = [Res() for _ in range(4)]
            kspad = sb("kspad", [128, 2, SEQ], BF16)
            kwpad = sb("kwpad", [128, 2, SEQ], BF16)
            ksres = [Res() for _ in range(4)]
            kwres = [Res() for _ in range(4)]
            kc2 = sb("kc2", [128, SEQ], BF16)
            vc2 = sb("vc2", [128, SEQ], BF16)
            kc2res, vc2res = Res(), Res()
            vsw = sb("vsw", [128, NT, 2, 80], BF16)
            vres = [Res() for _ in range(NT)]
            zs = sb("zs", [128, NT, 3, 256], BF16)
            zres = [Res() for _ in range(NT)]
            graw = sb("graw", [128, NT, 48], F32)
            gate = sb("gate", [128, NT, 48], F32)
            gres = Res()
            hT = {n: sb("hT" + n, [128, 2, 128], BF16) for n in "kv"}
            hres = {n: Res() for n in "kv"}
            kcpad = sb("kcpad", [128, 2, 128], BF16)
            kcres = Res()
            vcaug = sb("vcaug", [128, 98], BF16)
            vcres = Res()
            ovf = sb("ovf", [128, 32], F32)
            Pc = [sb("Pc%d" % k, [128, 512], BF16) for k in range(4)]
            Pcres = [Res() for _ in range(4)]
            cbm = [sb("cbm%d" % k, [128, 512], BF16) for k in range(2)]
            cbmres = [Res(), Res()]
            pbuf = [sb("pbuf%d" % k, [128, 512], BF16) for k in range(3)]
            pbres = [Res() for _ in range(3)]
            selbT = sb("selbT", [128, 512], BF16)
            selres = [Res() for _ in range(4)]
            imp = sb("imp", [128, 4, 32], F32)
            impres = [Res() for _ in range(4)]
            tk = sb("tk", [128, 4, 32 + 32 + 8 + 8 + 32], F32)
            selb = sb("selb", [128, 4, 32], BF16)
            tkres = [Res() for _ in range(4)]
            pp = sb("pp", [128, 2, 16], F32)
            ppres = [Res(), Res()]
            tmp3 = sb("tmp3", [128, 4, 32], F32)
            tmp3res = Res()
            yacc = [sb("yacc%d" % k, [128, 256], F32) for k in range(4)]
            yaccres = [Res() for _ in range(4)]
            ytmp = [sb("ytmp%d" % k, [128, 256], F32) for k in range(2)]
            ytmpres = [Res(), Res()]
            ires = Res()
            sc.op("pool", lambda: nc.gpsimd.memset(kspad[64:128, 0, :], 0.0), pw=ksres)
            sc.op("pool", lambda: nc.gpsimd.memset(kspad[0:64, 1, :], 0.0), pw=ksres)
            sc.op("pool", lambda: nc.gpsimd.memset(kwpad[64:128, 0, :], 0.0), pw=kwres)
            sc.op("pool", lambda: nc.gpsimd.memset(kwpad[0:64, 1, :], 0.0), pw=kwres)
            sc.op("pool", lambda: nc.gpsimd.memset(kcpad[64:128, 0, :], 0.0), pw=[kcres])
            sc.op("pool", lambda: nc.gpsimd.memset(kcpad[0:64, 1, :], 0.0), pw=[kcres])
            sc.op("pool", lambda: nc.gpsimd.memset(kc2[64:128, SEQ - 1:SEQ], 0.0), pw=[kc2res])
            sc.op("pool", lambda: nc.gpsimd.memset(vc2[64:128, SEQ - 1:SEQ], 0.0), pw=[vc2res])
            sc.op("pool", lambda: nc.gpsimd.memset(vsw[:, :, :, 64:65], 1.0), pw=vres)
            sc.op("pool", lambda: nc.gpsimd.memset(vcaug[:, 64:66], 1.0), pw=[vcres])
            sc.op("pool", lambda: nc.gpsimd.memset(selbT[:], 0.0), pw=selres)
            sc.dma("sp", out=ovf[0:CMP_N, :], in_=P["c_ov"][:, :], w=[ires])
            sc.op("dve", lambda: nc.vector.tensor_copy(out=vcaug[0:CMP_N, 66:98], in_=ovf[0:CMP_N, :]),
                  r=[ires], pw=[vcres])

            def load_group_weights(g):
                sc.dma("pool", out=wq[:], in_=wview[:, :, 256 * g:256 * (g + 1)], w=[wres])
                for t, dst in ((0, wkc2), (1, wvc2), (2, wks2), (4, wkw2)):
                    c0 = 1024 + 256 * t + 64 * g
                    for half in range(2):
                        sc.dma("pool", out=dst[:, :, half * 64:(half + 1) * 64], in_=wview[:, :, c0:c0 + 64], pw=[wres])
                for t, c in ((3, 0), (5, 64)):
                    c0 = 1024 + 256 * t + 64 * g
                    sc.dma("pool", out=wtok[:, :, c:c + 64], in_=wview[:, :, c0:c0 + 64], pw=[wres])
                for br in range(3):
                    c0 = 2608 + 1024 * br + 256 * g
                    sc.dma("pool", out=wtok[:, :, 128 + 256 * br:128 + 256 * (br + 1)], in_=wview[:, :, c0:c0 + 256],
                           pw=[wres])

            nbk = [0]

            def nextbank():
                b = nbk[0] % 6
                nbk[0] += 1
                return b

            def proj_fm(wt, ncols, s4, b):
                for kc in range(8):
                    sc.op("pe", lambda: nc.tensor.matmul(bank(b), lhsT=wt[:, kc, ncols:ncols + 128],
                                                         rhs=uT[:, kc, s4 * 512:(s4 + 1) * 512],
                                                         start=(kc == 0), stop=(kc == 7)),
                          r=[wres, lres] + uTres[s4 * 4:(s4 + 1) * 4],
                          **({"w": [bres[b]]} if kc == 0 else {"pw": [bres[b]]}))

            njob = [0]
            nacc = [0]
            load_group_weights(0)
            for n in "kv":
                for hc in range(2):
                    b = nextbank()
                    for lp in range(16):
                        sc.op("pe", lambda: nc.tensor.matmul(bank(b)[:, 0:1], lhsT=W1p[n][:, lp, hc * 128:(hc + 1) * 128],
                                                             rhs=pos2[n][:, lp:lp + 1], start=(lp == 0), stop=(lp == 15)),
                              r=[lres], **({"w": [bres[b]]} if lp == 0 else {"pw": [bres[b]]}))
                    sc.op("dve", lambda: nc.vector.tensor_copy(out=pb[n][:, hc:hc + 1], in_=bank(b)[:, 0:1]),
                          r=[bres[b]], pw=[lres])

            if stage(1):
                return
            for g in range(4):
                for s4 in range(4):
                    sl = slice(s4 * 512, (s4 + 1) * 512)
                    for m in range(2):
                        b = nextbank()
                        proj_fm(wq, m * 128, s4, b)
                        sc.op("act", lambda: nc.scalar.activation(out=qT2[:, m, sl], in_=bank(b), func=AF.Copy,
                                                                  scale=0.125),
                              r=[bres[b]], **({"w": [qres[s4]]} if m == 0 else {"pw": [qres[s4]]}))
                    if stage(1.1):
                        return
                    for wt, dst, dres in ((wks2, kspad, ksres), (wkw2, kwpad, kwres)):
                        b = nextbank()
                        proj_fm(wt, 0, s4, b)
                        sc.op("dve", lambda: nc.vector.tensor_copy(out=dst[0:64, 0, sl], in_=bank(b)[0:64, :]),
                              r=[bres[b]], w=[dres[s4]])
                        sc.op("act", lambda: nc.scalar.copy(out=dst[64:128, 1, sl], in_=bank(b)[64:128, :]),
                              r=[bres[b]], pw=[dres[s4]])
                    if stage(1.2):
                        return
                    for wt, dst, dres in ((wkc2, kc2, kc2res), (wvc2, vc2, vc2res)):
                        b = nextbank()
                        proj_fm(wt, 0, s4, b)
                        sc.op("dve", lambda: nc.vector.tensor_copy(out=dst[0:64, sl], in_=bank(b)[0:64, :]),
                              r=[bres[b]], **({"w": [dres]} if s4 == 0 else {"pw": [dres]}))
                        if s4 == 0:
                            sc.op("act", lambda: nc.scalar.copy(out=dst[64:128, 0:511], in_=bank(b)[64:128, 1:512]),
                                  r=[bres[b]], pw=[dres])
                        else:
                            sc.op("act", lambda: nc.scalar.copy(out=dst[64:128, s4 * 512 - 1:(s4 + 1) * 512 - 1],
                                                                in_=bank(b)[64:128, :]),
                                  r=[bres[b]], pw=[dres])
                if stage(1.4):
                    return
                for i in range(NT):
                    if i == 1 and stage(1.5):
                        return
                    if i == TOKSTOP:
                        return
                    ba, bb = nextbank(), nextbank()
                    bg = nextbank() if g == 0 else None
                    for kc in range(8):
                        lhs = uT[:, kc, i * 128:(i + 1) * 128]
                        fl = dict(start=(kc == 0), stop=(kc == 7))
                        wk = "w" if kc == 0 else "pw"
                        sc.op("pe", lambda: nc.tensor.matmul(bank(ba)[:, 0:384], lhsT=lhs, rhs=wtok[:, kc, 0:384], **fl),
                              r=[wres, uTres[i]], **{wk: [bres[ba]]})
                    for kc in range(8):
                        lhs = uT[:, kc, i * 128:(i + 1) * 128]
                        fl = dict(start=(kc == 0), stop=(kc == 7))
                        wk = "w" if kc == 0 else "pw"
                        sc.op("pe", lambda: nc.tensor.matmul(bank(bb), lhsT=lhs, rhs=wtok[:, kc, 384:896], **fl),
                              r=[wres, uTres[i]], **{wk: [bres[bb]]})
                    if g == 0:
                        for kc in range(8):
                            lhs = uT[:, kc, i * 128:(i + 1) * 128]
                            fl = dict(start=(kc == 0), stop=(kc == 7))
                            wk = "w" if kc == 0 else "pw"
                            sc.op("pe", lambda: nc.tensor.matmul(bank(bg)[:, 0:48], lhsT=lhs, rhs=wg[:, kc, :], **fl),
                                  r=[lres, uTres[i]], **{wk: [bres[bg]]})
                    if i == 0 and stage(1.45):
                        return
                    sc.op("dve", lambda: nc.vector.tensor_copy(
                        out=vsw[:, i, :, 0:64], in_=bank(ba)[:, 0:128].rearrange("p (a d) -> p a d", a=2)),
                        r=[bres[ba]], w=[vres[i]])
                    sc.op("act", lambda: nc.scalar.activation(out=zs[:, i, 0, :], in_=bank(ba)[:, 128:384], func=AF.Silu),
                          r=[bres[ba], vres[i]], w=[zres[i]])
                    sc.op("act", lambda: nc.scalar.activation(out=zs[:, i, 1:3, :],
                                                              in_=bank(bb).rearrange("p (a d) -> p a d", a=2),
                                                              func=AF.Silu),
                          r=[bres[bb]], pw=[zres[i]])
                    if g == 0:
                        sc.op("dve", lambda: nc.vector.tensor_copy(out=graw[:, i, :], in_=bank(bg)[:, 0:48]),
                              r=[bres[bg]], pw=[gres])
                if stage(2):
                    return
                for n, src2, sres2 in (("k", kc2, kc2res), ("v", vc2, vc2res)):
                    for hc in range(2):
                        b = nextbank()
                        for lp in range(16):
                            sc.op("pe", lambda: nc.tensor.matmul(
                                bank(b)[:, 0:CMP_N], lhsT=W1p[n][:, lp, hc * 128:(hc + 1) * 128],
                                rhs=src2[:, 2 * lp:2 * lp + 16 * (CMP_N - 1) + 1:16],
                                start=(lp == 0), stop=(lp == 15)),
                                r=[lres, sres2], **({"w": [bres[b]]} if lp == 0 else {"pw": [bres[b]]}))
                        sc.op("act", lambda: nc.scalar.activation(out=hT[n][:, hc, 0:CMP_N], in_=bank(b)[:, 0:CMP_N],
                                                                  func=AF.Silu, bias=pb[n][:, hc:hc + 1]),
                              r=[bres[b], lres], **({"w": [hres[n]]} if hc == 0 else {"pw": [hres[n]]}))
                b = nextbank()
                for hc in range(2):
                    sc.op("pe", lambda: nc.tensor.matmul(bank(b)[:, 0:CMP_N], lhsT=w2k[:, hc, :], rhs=hT["k"][:, hc, 0:CMP_N],
                                                         start=(hc == 0), stop=(hc == 1)),
                          r=[lres, hres["k"]], **({"w": [bres[b]]} if hc == 0 else {"pw": [bres[b]]}))
                sc.op("dve", lambda: nc.vector.tensor_copy(out=kcpad[0:64, 0, 0:CMP_N], in_=bank(b)[0:64, 0:CMP_N]),
                      r=[bres[b]], w=[kcres])
                sc.op("dve", lambda: nc.vector.tensor_copy(out=kcpad[64:128, 1, 0:CMP_N], in_=bank(b)[64:128, 0:CMP_N]),
                      r=[bres[b]], pw=[kcres])
                b = nextbank()
                for hc in range(2):
                    sc.op("pe", lambda: nc.tensor.matmul(bank(b)[0:CMP_N, 0:64], lhsT=hT["v"][:, hc, 0:CMP_N], rhs=w2v[:, hc, :],
                                                         start=(hc == 0), stop=(hc == 1)),
                          r=[lres, hres["v"]], **({"w": [bres[b]]} if hc == 0 else {"pw": [bres[b]]}))
                sc.op("dve", lambda: nc.vector.tensor_copy(out=vcaug[0:CMP_N, 0:64], in_=bank(b)[0:CMP_N, 0:64]),
                      r=[bres[b]], w=[vcres])
                if g < 3:
                    load_group_weights(g + 1)
                if g == 0:
                    sc.op("act", lambda: nc.scalar.activation(out=gate[:], in_=graw[:], func=AF.Exp, scale=-1.0),
                          r=[gres], w=[gres])
                    sc.op("dve", lambda: nc.vector.tensor_scalar(out=gate[:], in0=gate[:], scalar1=1.0, scalar2=None,
                                                                 op0=ALU.add), w=[gres])
                    sc.op("dve", lambda: nc.vector.reciprocal(out=gate[:], in_=gate[:]), w=[gres])

                if stage(3):
                    return
                def attn_tile(i, t, br, kpad, kres_, vidx, kts, bmfn, use_sel):
                    ob = 3 + nacc[0] % 2
                    nacc[0] += 1
                    jobs = []
                    for r in range(4):
                        for a in range(0, len(kts), 4):
                            jobs.append((r, kts[a:a + 4]))
                    qsl = slice(i * 128, (i + 1) * 128)

                    def qk(job, jn):
                        r, ks_ = job
                        sb_ = jn % 3
                        first = True
                        for a, kt in enumerate(ks_):
                            extra = []
                            if use_sel:
                                extra.append((cx.Epad[:, kt * 128:(kt + 1) * 128], selbT[:, t * 128:(t + 1) * 128],
                                              [cx.bmres, selres[t]]))
                            bm = bmfn(4 * g + r, i - kt)
                            if bm is not None:
                                extra.append((cx.antib[:], bm, [cx.bmres, cx.cres]))
                            out = bank(sb_)[:, a * 128:(a + 1) * 128]
                            sc.op("pe", lambda: nc.tensor.matmul(out, lhsT=kpad[:, r % 2, kt * 128:(kt + 1) * 128],
                                                                 rhs=qT2[:, r // 2, qsl], start=True,
                                                                 stop=(len(extra) == 0)),
                                  r=[kres_[kt // 4], qres[i // 4]],
                                  **({"w": [bres[sb_]]} if first else {"pw": [bres[sb_]]}))
                            first = False
                            for e, (l_, r_, rr_) in enumerate(extra):
                                sc.op("pe", lambda: nc.tensor.matmul(out, lhsT=l_, rhs=r_, start=False,
                                                                     stop=(e == len(extra) - 1)),
                                      r=rr_, pw=[bres[sb_]])

                    def expv(job, jn):
                        r, ks_ = job
                        sb_ = jn % 3
                        w_ = len(ks_) * 128
                        sc.op("act", lambda: nc.scalar.activation(out=pbuf[sb_][:, 0:w_], in_=bank(sb_)[:, 0:w_],
                                                                  func=AF.Exp),
                              r=[bres[sb_]], w=[pbres[sb_]])
                        for a, kt in enumerate(ks_):
                            fst = (kt == kts[0])
                            sc.op("pe", lambda: nc.tensor.matmul(bank(ob)[:, r * 65:(r + 1) * 65],
                                                                 lhsT=pbuf[sb_][:, a * 128:(a + 1) * 128],
                                                                 rhs=vsw[:, kt, vidx, 0:65], start=fst, stop=(kt == kts[-1])),
                                  r=[pbres[sb_], vres[kt]],
                                  **({"w": [bres[ob]]} if (fst and r == 0) else {"pw": [bres[ob]]}))

                    j0 = njob[0]
                    qk(jobs[0], j0)
                    for n_, job in enumerate(jobs):
                        if n_ + 1 < len(jobs):
                            qk(jobs[n_ + 1], j0 + n_ + 1)
                        expv(job, j0 + n_)
                    njob[0] += len(jobs)
                    return ob

                def combine(ob, i, br, width, first, last):
                    k2 = nacc[0] % 2
                    o3 = bank(ob)[:, 0:4 * width].rearrange("p (r c) -> p r c", r=4)
                    rden = pp[:, k2, 0:4]
                    fac = pp[:, k2, 4:8]
                    sc.op("dve", lambda: nc.vector.tensor_scalar(out=rden, in0=o3[:, :, 64], scalar1=1e-30, scalar2=None,
                                                                 op0=ALU.max), r=[bres[ob]], w=[ppres[k2]])
                    sc.op("dve", lambda: nc.vector.reciprocal(out=rden, in_=rden), w=[ppres[k2]])
                    sc.op("dve", lambda: nc.vector.tensor_tensor(out=fac, in0=rden,
                                                                 in1=gate[:, i, 16 * br + 4 * g:16 * br + 4 * g + 4],
                                                                 op=ALU.mult), r=[gres], w=[ppres[k2]])
                    ya = yacc[i % 4]
                    yar = yaccres[i % 4]
                    dst = ya if first else ytmp[k2]
                    dres = yar if first else ytmpres[k2]
                    sc.op("dve", lambda: nc.vector.tensor_tensor(
                        out=dst[:].rearrange("p (r d) -> p r d", r=4), in0=o3[:, :, 0:64],
                        in1=fac.unsqueeze(2).to_broadcast([128, 4, 64]), op=ALU.mult),
                        r=[bres[ob], ppres[k2]], w=[dres])
                    sc.op("pool", lambda: nc.gpsimd.tensor_tensor(out=dst[:], in0=dst[:], in1=zs[:, i, br, :], op=ALU.mult),
                          r=[zres[i]], w=[dres])
                    if not first:
                        if last:
                            sc.op("pool", lambda: nc.gpsimd.tensor_tensor(out=y[:, i, 256 * g:256 * (g + 1)], in0=ya[:],
                                                                          in1=dst[:], op=ALU.add),
                                  r=[dres, yar], pw=[yres[i]])
                        else:
                            sc.op("pool", lambda: nc.gpsimd.tensor_tensor(out=ya[:], in0=ya[:], in1=dst[:], op=ALU.add),
                                  r=[dres], w=[yar])
                    return rden

                def bm_selwin(h, d):
                    if d == 0 or d == 1:
                        return cx.BM[:, h, d, :]
                    if d == 4:
                        return cx.BM4[:]
                    return None

                def bm_sel(h, d):
                    return cx.BM[:, h, d, :] if d in (0, 1) else None

                for i4 in range(4):
                    sl = slice(i4 * 512, (i4 + 1) * 512)
                    for r in range(4):
                        h = 4 * g + r
                        cb = (4 * i4 + r) % 2
                        src = bass.AP(cx.fc_d, h * 4096 + i4 * 512, [[16, CMP_N], [1, 512]])
                        sc.dma("sp", out=cbm[cb][0:CMP_N, :], in_=src, r=[cx.fcres], w=[cbmres[cb]])
                        sb_ = njob[0] % 3
                        njob[0] += 1
                        sc.op("pe", lambda: nc.tensor.matmul(bank(sb_)[0:CMP_N, :], lhsT=kcpad[:, r % 2, 0:CMP_N],
                                                             rhs=qT2[:, r // 2, sl], start=True, stop=False),
                              r=[kcres, qres[i4]], w=[bres[sb_]])
                        sc.op("pe", lambda: nc.tensor.matmul(bank(sb_)[0:CMP_N, :], lhsT=cx.antib[0:CMP_N, 1:128],
                                                             rhs=cbm[cb][0:CMP_N, :], start=False, stop=True),
                              r=[cbmres[cb], cx.cres], pw=[bres[sb_]])
                        sc.op("act", lambda: nc.scalar.activation(out=Pc[r][0:CMP_N, :], in_=bank(sb_)[0:CMP_N, :],
                                                                  func=AF.Exp),
                              r=[bres[sb_]], w=[Pcres[r]])
                    for t in range(4):
                        i = 4 * i4 + t
                        ob = 3 + nacc[0] % 2
                        nacc[0] += 1
                        for r in range(4):
                            sc.op("pe", lambda: nc.tensor.matmul(bank(ob)[:, r * 98:(r + 1) * 98],
                                                                 lhsT=Pc[r][0:CMP_N, t * 128:(t + 1) * 128],
                                                                 rhs=vcaug[0:CMP_N, :], start=True, stop=True),
                                  r=[Pcres[r], vcres], **({"w": [bres[ob]]} if r == 0 else {"pw": [bres[ob]]}))
                        rden = combine(ob, i, 0, 98, True, False)
                        if i >= 8:
                            o3 = bank(ob)[:, 0:392].rearrange("p (r c) -> p r c", r=4)
                            k2 = nacc[0] % 2
                            sc.op("dve", lambda: nc.vector.tensor_tensor(
                                out=tmp3[:], in0=o3[:, :, 66:98], in1=rden.unsqueeze(2).to_broadcast([128, 4, 32]),
                                op=ALU.mult), r=[bres[ob], ppres[k2]], w=[tmp3res])
                            s1 = tk[:, t, 0:32]
                            s2 = tk[:, t, 32:64]
                            m1 = tk[:, t, 64:72]
                            m2 = tk[:, t, 72:80]
                            sc.op("dve", lambda: nc.vector.tensor_reduce(
                                out=s1, in_=tmp3[:].rearrange("p r j -> p j r"), axis=mybir.AxisListType.X, op=ALU.add),
                                r=[tmp3res], w=[tkres[t]])
                            sc.op("dve", lambda: nc.vector.tensor_tensor(out=s1, in0=s1, in1=forced[:, i - 8, :],
                                                                         op=ALU.add), r=[lres], w=[tkres[t]])
                            sc.op("dve", lambda: nc.vector.max(out=m1, in_=s1), w=[tkres[t]])
                            sc.op("dve", lambda: nc.vector.match_replace(out=s2, in_to_replace=m1, in_values=s1,
                                                                         imm_value=-3.0e38), w=[tkres[t]])
                            sc.op("dve", lambda: nc.vector.max(out=m2, in_=s2), w=[tkres[t]])
                            sc.op("dve", lambda: nc.vector.tensor_scalar(out=selb[:, t, :], in0=s1, scalar1=m2[:, 7:8],
                                                                         scalar2=NEG, op0=ALU.is_lt, op1=ALU.mult),
                                  w=[tkres[t]])
                            pt, ptr = cx.psT[t % 2], cx.psTr[t % 2]
                            sc.op("pe", lambda: nc.tensor.transpose(out=pt[0:32, 0:128], in_=selb[:, t, :],
                                                                    identity=identb[:]),
                                  r=[tkres[t], cx.cres], w=[ptr])
                            sc.op("dve", lambda: nc.vector.tensor_copy(out=selbT[0:32, t * 128:(t + 1) * 128],
                                                                       in_=pt[0:32, 0:128]),
                                  r=[ptr], w=[selres[t]])
                    if stage(4 if i4 < 2 else 5):
                        return
                    for t in range(4):
                        i = 4 * i4 + t
                        ob = attn_tile(i, t, 1, kspad, ksres, 0, list(range(0, i + 1)), bm_sel, i >= 8)
                        combine(ob, i, 1, 65, False, False)
                        ob = attn_tile(i, t, 2, kwpad, kwres, 1, list(range(max(0, i - 4), i + 1)), bm_selwin, False)
                        combine(ob, i, 2, 65, False, True)
        sc.fence()
        with ExitStack() as es2:
            yT = uT
            yTres = uTres
            for i in range(NT):
                pt, ptr = cx.psT[i % 2], cx.psTr[i % 2]
                for c in range(8):
                    sc.op("pe", lambda: nc.tensor.transpose(out=pt[:, c * 128:(c + 1) * 128],
                                                            in_=y[:, i, c * 128:(c + 1) * 128], identity=identb[:]),
                          r=[yres[i], cx.cres], **({"w": [ptr]} if c == 0 else {"pw": [ptr]}))
                src = pt[:].rearrange("p (c s) -> p c s", c=8)
                dst = yT[:, :, i * 128:(i + 1) * 128]
                if i % 2 == 0:
                    sc.op("act", lambda: nc.scalar.copy(out=dst, in_=src), r=[ptr], w=[yTres[i]])
                else:
                    sc.op("dve", lambda: nc.vector.tensor_copy(out=dst, in_=src), r=[ptr], w=[yTres[i]])
            wo = es2.enter_context(nc.sbuf_tensor("wo_%d" % li, [128, 8, D_MODEL], BF16))
            wor = Res()
            wov = w_out.rearrange("(k p) c -> p k c", p=128)
            for q4 in range(2):
                sc.dma("pool", out=wo[:, q4 * 4:(q4 + 1) * 4, :], in_=wov[:, q4 * 4:(q4 + 1) * 4, :], pw=[wor])
            pgain = es2.enter_context(nc.sbuf_tensor("pog_%d" % li, [128, D_MODEL], F32))
            pgres = Res()
            sc.dma("sp", out=pgain[:], in_=bcast_row(P["post_norm_gain"][li], D_MODEL), w=[pgres])
            bufs = post_bufs(cx, es2, li)
            for i in range(NT):
                ops, opsr = cx.pbig[i % 3], cx.pbigr[i % 3]
                for half in range(2):
                    for kc in range(8):
                        sc.op("pe", lambda: nc.tensor.matmul(
                            ops[:, half * 512:(half + 1) * 512], lhsT=yT[:, kc, i * 128:(i + 1) * 128],
                            rhs=wo[:, kc, half * 512:(half + 1) * 512], start=(kc == 0), stop=(kc == 7)),
                            r=[yTres[i], wor],
                            **({"w": opsr} if (kc == 0 and half == 0) else {"pw": opsr}))
                emit_postnorm_residual(cx, es2, li, i, ops, opsr, pgain, pgres, h_in, hres_in, h_out, hres_out, bufs)
    sc.fence()


def _needed_params(layers):
    need = {"pre_norm_gain": None, "post_norm_gain": None}
    nsa = sorted({li // 2 for li in layers if li % 2 == 0})
    ret = sorted({li // 2 for li in layers if li % 2 == 1})
    if nsa:
        need["rel_bias_table"] = None
        for n in ("nsa_w_in", "nsa_w_out", "nsa_cmp_k_pos", "nsa_cmp_k_w1", "nsa_cmp_k_w2",
                  "nsa_cmp_v_pos", "nsa_cmp_v_w1", "nsa_cmp_v_w2"):
            need[n] = nsa
    if ret:
        for n in ("ret_w_in", "ret_w_out", "ret_gn_gain"):
            need[n] = ret
    return need


PARAM_SHAPES = {
    "pre_norm_gain": [4, 1024], "post_norm_gain": [4, 1024], "rel_bias_table": [32, 16],
    "nsa_w_in": [2, 1024, NSA_PROJ], "nsa_w_out": [2, 1024, 1024],
    "nsa_cmp_k_pos": [2, 32, 64], "nsa_cmp_k_w1": [2, 2048, 256], "nsa_cmp_k_w2": [2, 256, 64],
    "nsa_cmp_v_pos": [2, 32, 64], "nsa_cmp_v_w1": [2, 2048, 256], "nsa_cmp_v_w2": [2, 256, 64],
    "ret_w_in": [2, 1024, RET_PROJ], "ret_w_out": [2, 2048, 1024], "ret_gn_gain": [2, 2048],
}


def host_consts():
    rc = _ret_consts()
    c = {
        "c_cosT": rc["cosT"], "c_sinT": rc["sinT"],
        "c_innerT": rc["innerT"], "c_zeta": rc["zeta"], "c_epsp": rc["epsp"],
        "c_ident": np.eye(128, dtype=np.float32),
        "c_anti": np.ascontiguousarray(np.fliplr(np.eye(128, dtype=np.float32))),
    }
    c.update(_nsa_consts())
    return c, rc


def build_program(layers, first_from_x=True):
    consts, rc = host_consts()
    nc = bass.Bass("TRN2", target_bir_lowering=False, dynamic_dma_scratch_size=8192)
    P = {}
    x = nc.dram_tensor("x", [SEQ, D_MODEL], F32, kind="ExternalInput").ap()
    out = nc.dram_tensor("out", [SEQ, D_MODEL], F32, kind="ExternalOutput").ap()
    need = _needed_params(layers)
    slotmap = {}
    for name, shp in PARAM_SHAPES.items():
        if name not in need:
            continue
        shp = list(shp)
        if need[name] is not None:
            shp[0] = len(need[name])
            slotmap[name] = {s_: k_ for k_, s_ in enumerate(need[name])}
        P[name] = nc.dram_tensor(name, shp, F32, kind="ExternalInput").ap()
    has_nsa = any(li % 2 == 0 for li in layers)
    has_ret = any(li % 2 == 1 for li in layers)
    nsa_c = ("c_ohw", "c_ohc", "c_E", "c_ov", "c_forced")
    ret_c = ("c_cosT", "c_sinT")
    consts = {k: v for k, v in consts.items()
              if not ((k in nsa_c and not has_nsa) or (k in ret_c and not has_ret))}
    for name, arr in consts.items():
        P[name] = nc.dram_tensor(name, list(arr.shape), F32, kind="ExternalInput").ap()
    scratch = [nc.dram_tensor("hs%d" % i, [SEQ, D_MODEL], F32, kind="Internal").ap() for i in range(2)]
    Res.base = {}
    with ExitStack() as es:
        sc = Sched(nc, es)
        cx = Ctx()
        cx.nc, cx.sc, cx.P = nc, sc, P
        cx.dumps, cx.dump_res = [], []
        cx.pbig = [es.enter_context(nc.psum_tensor("pbig%d" % i, [128, 1024], F32)) for i in range(3)]
        cx.bres = [Res() for _ in range(6)]
        cx.pbigr = [[cx.bres[2 * i], cx.bres[2 * i + 1]] for i in range(3)]
        cx.bank = lambda k: cx.pbig[k // 2][:, (k % 2) * 512:(k % 2 + 1) * 512]
        cx.psT = [es.enter_context(nc.psum_tensor("psT%d" % i, [128, 1024], BF16)) for i in range(2)]
        cx.psTr = [Res(), Res()]
        cx.cres = Res()
        identf = es.enter_context(nc.sbuf_tensor("identf", [128, 128], F32))
        cx.identb = es.enter_context(nc.sbuf_tensor("identb", [128, 128], BF16))
        cx.eps_rms = es.enter_context(nc.sbuf_tensor("eps_rms", [128, 1], F32))
        sc.dma("sp", out=identf[:], in_=P["c_ident"][:, :], w=[cx.cres])
        sc.op("dve", lambda: nc.vector.tensor_copy(out=cx.identb[:], in_=identf[:]), r=[cx.cres], w=[cx.cres])
        sc.op("dve", lambda: nc.vector.memset(cx.eps_rms[:], RMS_EPS), pw=[cx.cres])
        antif = es.enter_context(nc.sbuf_tensor("antif", [128, 128], F32))
        cx.antib = es.enter_context(nc.sbuf_tensor("antib", [128, 128], BF16))
        ares = Res()
        sc.dma("sp", out=antif[:], in_=P["c_anti"][:, :], w=[ares])
        sc.op("dve", lambda: nc.vector.tensor_copy(out=cx.antib[:], in_=antif[:]), r=[ares], pw=[cx.cres])
        RC = {"gC": rc["gC"]}
        RC["innerT"] = es.enter_context(nc.sbuf_tensor("innerT", [128, 512], F32))
        RC["zeta"] = es.enter_context(nc.sbuf_tensor("zeta", [128, 4], F32))
        RC["epsp"] = es.enter_context(nc.sbuf_tensor("epsp", [128, 4], F32))
        sc.dma("sp", out=RC["innerT"][:], in_=P["c_innerT"][:, :], pw=[cx.cres])
        sc.dma("sp", out=RC["zeta"][:], in_=P["c_zeta"][:, :], pw=[cx.cres])
        sc.dma("sp", out=RC["epsp"][:], in_=P["c_epsp"][:, :], pw=[cx.cres])
        cx.RC = RC
        if any(li % 2 == 0 for li in layers):
            emit_nsa_prologue(cx, es)
        xres = [Res() for _ in range(NT)]
        ores = [Res() for _ in range(NT)]
        sres = [[Res() for _ in range(NT)] for _ in range(2)]
        cur, curres = x, xres
        for n, li in enumerate(layers):
            last = (n == len(layers) - 1)
            dst, dstres = (out, ores) if last else (scratch[n % 2], sres[n % 2])
            if li % 2 == 0:
                emit_nsa_layer(cx, li, slotmap["nsa_w_in"][li // 2], cur, curres, dst, dstres)
            else:
                emit_ret_layer(cx, li, slotmap["ret_w_in"][li // 2], cur, curres, dst, dstres)
            if STAGE < 99:
                sc.fence()
                sc._wait("sp", Res.base, skip_own=False)
                break
            cur, curres = dst, dstres
        sc.finish(ores + cx.dump_res)
        nc.all_engine_barrier()
        sc.clear_all()
    nc._dumps = cx.dumps
    return nc, consts


_PROG_CACHE = {}


def run_layers(layers, x, params):
    key = tuple(layers)
    if key not in _PROG_CACHE:
        _PROG_CACHE[key] = build_program(list(layers))
    nc, consts = _PROG_CACHE[key]
    B = x.shape[0]
    need = _needed_params(list(layers))
    shared = {}
    for name, sl in need.items():
        a = np.asarray(params[name], dtype=np.float32)
        shared[name] = np.ascontiguousarray(a if sl is None else a[sl])
    in_maps = []
    for b in range(B):
        m = {"x": np.ascontiguousarray(x[b])}
        m.update(shared)
        m.update(consts)
        in_maps.append(m)
    res = run_bass_kernel_spmd(nc, in_maps, core_ids=list(range(B)))
    global LAST_RESULTS
    LAST_RESULTS = res.results
    return np.stack([r["out"] for r in res.results], axis=0)


def kernel(x, **params):
    x = np.asarray(x, dtype=np.float32)
    h = run_layers(list(range(DEPTH)), x, params)
    return h.astype(np.float32)
```
